# Optimizing a Trainium2 kernel written in Bass

```python
import math
import jax, jax.numpy as jnp
from jax import lax
import numpy as np

D_MODEL = 2048
BATCH = 1
SEQ = 8192
DEPTH = 2

HEAD_DIM = 128
ATTN_WIDTH = 3 * D_MODEL // 4
ATTN_HEADS = ATTN_WIDTH // HEAD_DIM
LRU_WIDTH = 3 * D_MODEL // 4
LRU_BLOCKS = 12
LRU_BLOCK_W = LRU_WIDTH // LRU_BLOCKS
MIX_WIDTH = ATTN_WIDTH + LRU_WIDTH
IN_WIDTH = 3 * ATTN_WIDTH + 2 * LRU_WIDTH
CONV_WIDTH = 4
LRU_C = 8.0
ROPE_THETA = 10000.0
DILATED_BRANCHES = ((128, 1), (512, 4), (2048, 16))
Q_BLOCK = 128
NEG_INF = -1e30
NORM_EPS = 1e-6
N_EXPERTS = 32
TOP_K = 4
EXPERT_FF = D_MODEL
SWIGLU_LIMIT = 7.0
SWIGLU_ALPHA = 1.702
MOE_BLOCK = 256

kernel_name = 'hybrid_dilated_attn_rglru_moe_block'


def rmsnorm(x, g):
    x32 = x.astype(jnp.float32)
    y = x32 * lax.rsqrt(jnp.mean(x32 * x32, axis=-1, keepdims=True) + NORM_EPS)
    return (y * g.astype(jnp.float32)).astype(x.dtype)


def rope_tables(positions):
    inv_freq = ROPE_THETA ** (-jnp.arange(0, HEAD_DIM, 2, dtype=jnp.float32) / HEAD_DIM)
    ang = positions.astype(jnp.float32)[..., None] * inv_freq
    ang = jnp.concatenate([ang, ang], axis=-1)[:, :, None, :]
    return jnp.cos(ang), jnp.sin(ang)


def apply_rope(t, cos, sin):
    t32 = t.astype(jnp.float32)
    half = HEAD_DIM // 2
    rot = jnp.concatenate([-t32[..., half:], t32[..., :half]], axis=-1)
    return (t32 * cos + rot * sin).astype(t.dtype)


def dilated_branch(q, k, v, window, dilation):
    B, S, H, E = q.shape
    d = dilation
    w = window // d
    L = S // d
    bq = math.gcd(L, Q_BLOCK)
    nb = L // bq

    def to_res(t):
        return t.reshape(B, L, d, H, E).transpose(0, 3, 2, 1, 4)

    qb = to_res(q).reshape(B, H, d, nb, bq, E)
    pad = ((0, 0), (0, 0), (0, 0), (w, 0), (0, 0))
    idx = jnp.arange(nb)[:, None] * bq + jnp.arange(bq + w)[None, :]
    kb = jnp.pad(to_res(k), pad)[:, :, :, idx]
    vb = jnp.pad(to_res(v), pad)[:, :, :, idx]
    s = jnp.einsum('bhrnqe,bhrnke->bhrnqk', qb, kb,
                   preferred_element_type=jnp.float32) * (E ** -0.5)
    qi = jnp.arange(bq)[:, None]
    kk = jnp.arange(bq + w)[None, :]
    band = (kk >= qi) & (kk <= qi + w)
    valid = idx >= w
    mask = band[None, :, :] & valid[:, None, :]
    s = jnp.where(mask, s, NEG_INF)
    m = jnp.max(s, axis=-1, keepdims=True)
    p = jnp.exp(s - m)
    den = jnp.sum(p, axis=-1, keepdims=True)
    o = jnp.einsum('bhrnqk,bhrnke->bhrnqe', p, vb.astype(jnp.float32)) / den
    lse = (m + jnp.log(den))[..., 0]
    o = o.reshape(B, H, d, L, E).transpose(0, 3, 2, 1, 4).reshape(B, S, H, E)
    lse = lse.reshape(B, H, d, L).transpose(0, 3, 2, 1).reshape(B, S, H)
    return o, lse


def mixture_of_dilated_attention(q, k, v):
    outs, lses = [], []
    for window, dilation in DILATED_BRANCHES:
        o, lse = dilated_branch(q, k, v, window, dilation)
        outs.append(o)
        lses.append(lse)
    wts = jax.nn.softmax(jnp.stack(lses, axis=0), axis=0)
    out = jnp.sum(wts[..., None] * jnp.stack(outs, axis=0), axis=0)
    return out.astype(q.dtype)


def causal_depthwise_conv(x, w, b):
    C = x.shape[-1]
    y = lax.conv_general_dilated(x, w[:, None, :].astype(x.dtype), window_strides=(1,),
                                 padding=[(CONV_WIDTH - 1, 0)],
                                 dimension_numbers=('NWC', 'WIO', 'NWC'),
                                 feature_group_count=C)
    return y + b.astype(x.dtype)


def _lin_rec_combine(left, right):
    a_l, b_l = left
    a_r, b_r = right
    return a_l * a_r, a_r * b_l + b_r


def rg_lru(xc, positions, wa, ba, wx, bx, lam):
    B, S, C = xc.shape
    xb = xc.reshape(B, S, LRU_BLOCKS, LRU_BLOCK_W)
    r = jax.nn.sigmoid(jnp.einsum('bshi,hij->bshj', xb, wa).reshape(B, S, C) + ba)
    i = jax.nn.sigmoid(jnp.einsum('bshi,hij->bshj', xb, wx).reshape(B, S, C) + bx)
    log_a = -LRU_C * r.astype(jnp.float32) * jax.nn.softplus(-lam.astype(jnp.float32))
    reset = (positions == 0)[..., None]
    a = jnp.where(reset, 0.0, jnp.exp(log_a))
    mult = jnp.where(reset, 1.0, jnp.sqrt(-jnp.expm1(2.0 * log_a)))
    bterm = xc.astype(jnp.float32) * i.astype(jnp.float32) * mult
    _, h = lax.associative_scan(_lin_rec_combine, (a, bterm), axis=1)
    return h.astype(xc.dtype)


def hybrid_mixer(h, positions, w_in, conv_w, conv_b, wa, ba, wx, bx, lam, attn_g, lru_g, w_out):
    B, S, _ = h.shape
    z = h @ w_in
    q, k, v, xr, gr = jnp.split(
        z, [ATTN_WIDTH, 2 * ATTN_WIDTH, 3 * ATTN_WIDTH, 3 * ATTN_WIDTH + LRU_WIDTH], axis=-1)
    q = q.reshape(B, S, ATTN_HEADS, HEAD_DIM)
    k = k.reshape(B, S, ATTN_HEADS, HEAD_DIM)
    v = v.reshape(B, S, ATTN_HEADS, HEAD_DIM)
    cos, sin = rope_tables(positions)
    q = apply_rope(q, cos, sin)
    k = apply_rope(k, cos, sin)
    attn = mixture_of_dilated_attention(q, k, v).reshape(B, S, ATTN_WIDTH)
    xc = causal_depthwise_conv(xr, conv_w, conv_b)
    lru = rg_lru(xc, positions, wa, ba, wx, bx, lam) * jax.nn.gelu(gr, approximate=True)
    y = jnp.concatenate([rmsnorm(attn, attn_g), rmsnorm(lru, lru_g)], axis=-1)
    return y @ w_out


def moe(h, router_w, router_b, w1, b1, w2, b2, layer):
    B, S, D = h.shape
    T = B * S
    hf = h.reshape(T, D)
    logits = (hf @ router_w).astype(jnp.float32) + router_b.astype(jnp.float32)
    top_vals, top_idx = lax.top_k(logits, TOP_K)
    gates = jax.nn.softmax(top_vals, axis=-1)
    A = T * TOP_K
    e_flat = top_idx.reshape(A)
    tok_flat = jnp.repeat(jnp.arange(T, dtype=jnp.int32), TOP_K)
    g_flat = gates.reshape(A)
    order = jnp.argsort(e_flat)
    e_sorted = e_flat[order]
    counts = jnp.bincount(e_flat, length=N_EXPERTS)
    padded = ((counts + MOE_BLOCK - 1) // MOE_BLOCK) * MOE_BLOCK
    pad_end = jnp.cumsum(padded)
    pad_start = pad_end - padded
    start = jnp.cumsum(counts) - counts
    dest = pad_start[e_sorted] + (jnp.arange(A) - start[e_sorted])
    n_blocks = -(-A // MOE_BLOCK) + N_EXPERTS
    P = n_blocks * MOE_BLOCK
    tok_buf = jnp.zeros((P,), jnp.int32).at[dest].set(tok_flat[order])
    gate_buf = jnp.zeros((P,), jnp.float32).at[dest].set(g_flat[order])
    block_expert = jnp.clip(
        jnp.searchsorted(pad_end, jnp.arange(n_blocks) * MOE_BLOCK, side='right'),
        0, N_EXPERTS - 1)

    def block_fn(args):
        tok, g, e = args
        xb = hf[tok]
        u = xb @ w1[layer, e] + b1[layer, e]
        x_glu = jnp.minimum(u[:, ::2], SWIGLU_LIMIT)
        x_lin = jnp.clip(u[:, 1::2], -SWIGLU_LIMIT, SWIGLU_LIMIT)
        act = x_glu * jax.nn.sigmoid(SWIGLU_ALPHA * x_glu) * (x_lin + 1.0)
        y = act @ w2[layer, e] + b2[layer, e]
        return y.astype(jnp.float32) * g[:, None]

    ys = lax.map(block_fn, (tok_buf.reshape(n_blocks, MOE_BLOCK),
                            gate_buf.reshape(n_blocks, MOE_BLOCK), block_expert))
    out = jnp.zeros((T, D), jnp.float32).at[tok_buf].add(ys.reshape(P, D))
    return out.astype(h.dtype).reshape(B, S, D)


def setup_inputs(seed: int = 0) -> dict:
    key = jax.random.key(seed)
    ks = jax.random.split(key, 24)
    f32 = jnp.float32
    L = DEPTH

    def nrm(k, shape, scale):
        return jax.random.normal(k, shape, f32) * scale

    x = nrm(ks[0], (BATCH, SEQ, D_MODEL), 1.0)
    c = nrm(ks[1], (BATCH, D_MODEL), 1.0)
    positions = jnp.broadcast_to(jnp.arange(SEQ, dtype=jnp.int32), (BATCH, SEQ))
    ada_w = nrm(ks[2], (L, D_MODEL, 6 * D_MODEL), 0.5 * D_MODEL ** -0.5)
    ada_b = nrm(ks[3], (L, 6 * D_MODEL), 0.02)
    norm1_g = 1.0 + nrm(ks[4], (L, D_MODEL), 0.02)
    norm2_g = 1.0 + nrm(ks[5], (L, D_MODEL), 0.02)
    w_in = nrm(ks[6], (L, D_MODEL, IN_WIDTH), D_MODEL ** -0.5)
    conv_w = nrm(ks[7], (L, CONV_WIDTH, LRU_WIDTH), CONV_WIDTH ** -0.5)
    conv_b = nrm(ks[8], (L, LRU_WIDTH), 0.01)
    lru_wa = nrm(ks[9], (L, LRU_BLOCKS, LRU_BLOCK_W, LRU_BLOCK_W), LRU_BLOCK_W ** -0.5)
    lru_ba = nrm(ks[10], (L, LRU_WIDTH), 0.01)
    lru_wx = nrm(ks[11], (L, LRU_BLOCKS, LRU_BLOCK_W, LRU_BLOCK_W), LRU_BLOCK_W ** -0.5)
    lru_bx = nrm(ks[12], (L, LRU_WIDTH), 0.01)
    a_c = jax.random.uniform(ks[13], (L, LRU_WIDTH), f32, 0.9, 0.999)
    s = a_c ** (1.0 / LRU_C)
    lru_lambda = jnp.log(s) - jnp.log1p(-s)
    attn_out_g = 1.0 + nrm(ks[14], (L, ATTN_WIDTH), 0.02)
    lru_out_g = 1.0 + nrm(ks[15], (L, LRU_WIDTH), 0.02)
    w_out = nrm(ks[16], (L, MIX_WIDTH, D_MODEL), MIX_WIDTH ** -0.5)
    router_w = nrm(ks[17], (L, D_MODEL, N_EXPERTS), D_MODEL ** -0.5)
    router_b = nrm(ks[18], (L, N_EXPERTS), 0.01)
    w1 = nrm(ks[19], (L, N_EXPERTS, D_MODEL, 2 * EXPERT_FF), D_MODEL ** -0.5)
    b1 = nrm(ks[20], (L, N_EXPERTS, 2 * EXPERT_FF), 0.01)
    w2 = nrm(ks[21], (L, N_EXPERTS, EXPERT_FF, D_MODEL), EXPERT_FF ** -0.5)
    b2 = nrm(ks[22], (L, N_EXPERTS, D_MODEL), 0.01)
    final_g = 1.0 + nrm(ks[23], (D_MODEL,), 0.02)
    return {'x': x, 'c': c, 'positions': positions, 'ada_w': ada_w, 'ada_b': ada_b,
            'norm1_g': norm1_g, 'norm2_g': norm2_g, 'w_in': w_in, 'conv_w': conv_w,
            'conv_b': conv_b, 'lru_wa': lru_wa, 'lru_ba': lru_ba, 'lru_wx': lru_wx,
            'lru_bx': lru_bx, 'lru_lambda': lru_lambda, 'attn_out_g': attn_out_g,
            'lru_out_g': lru_out_g, 'w_out': w_out, 'router_w': router_w,
            'router_b': router_b, 'w1': w1, 'b1': b1, 'w2': w2, 'b2': b2,
            'final_g': final_g}


def reference(x, c, positions, ada_w, ada_b, norm1_g, norm2_g, w_in, conv_w, conv_b,
              lru_wa, lru_ba, lru_wx, lru_bx, lru_lambda, attn_out_g, lru_out_g, w_out,
              router_w, router_b, w1, b1, w2, b2, final_g):
    cond = jax.nn.silu(c)
    for l in range(DEPTH):
        mod = cond @ ada_w[l] + ada_b[l]
        sh1, sc1, g1, sh2, sc2, g2 = jnp.split(mod, 6, axis=-1)
        h = rmsnorm(x, norm1_g[l]) * (1.0 + sc1[:, None, :]) + sh1[:, None, :]
        y = hybrid_mixer(h, positions, w_in[l], conv_w[l], conv_b[l], lru_wa[l], lru_ba[l],
                         lru_wx[l], lru_bx[l], lru_lambda[l], attn_out_g[l], lru_out_g[l],
                         w_out[l])
        x = x + g1[:, None, :] * y
        h = rmsnorm(x, norm2_g[l]) * (1.0 + sc2[:, None, :]) + sh2[:, None, :]
        y = moe(h, router_w[l], router_b[l], w1, b1, w2, b2, l)
        x = x + g2[:, None, :] * y
    return rmsnorm(x, final_g)
```

```python
import contextlib
import numpy as np
import concourse.bass as bass
import concourse.mybir as mybir
from concourse.bass_utils import run_bass_kernel_spmd

F32 = mybir.dt.float32
BF16 = mybir.dt.bfloat16
I32 = mybir.dt.int32
AF = mybir.ActivationFunctionType
ALU = mybir.AluOpType


class Prog:
    ENG = ['pe', 'act', 'dve', 'pool', 'sp']

    def __init__(self):
        self.nc = bass.Bass("TRN2", target_bir_lowering=False)
        self.es = contextlib.ExitStack()
        self.ops = {e: [] for e in self.ENG}
        self.dma_slots = {}
        self.out_toks = []

    def din(self, name, shape, dt=F32):
        return self.nc.dram_tensor(name, list(shape), dt, kind="ExternalInput").ap()

    def dout(self, name, shape, dt=F32):
        return self.nc.dram_tensor(name, list(shape), dt, kind="ExternalOutput").ap()

    def sb(self, name, shape, dt=F32):
        return self.es.enter_context(self.nc.sbuf_tensor('sb_' + name, list(shape), dt))

    def ps(self, name, shape=(128, 512), dt=F32):
        return self.es.enter_context(self.nc.psum_tensor('ps_' + name, list(shape), dt))

    def op(self, eng, fn, deps=()):
        o = dict(fn=fn, deps=[d for d in deps if d is not None], sig=False, dma=None)
        self.ops[eng].append(o)
        return ('c', eng, len(self.ops[eng]) - 1)

    def dma(self, eng, out, in_, deps=(), slot=None, is_out=False, **kw):
        if slot is None:
            slot = '_a%d' % len(self.dma_slots)
        cnt = self.dma_slots.get(slot, 0) + 1
        self.dma_slots[slot] = cnt
        o = dict(fn=lambda e: e.dma_start(out=out, in_=in_, **kw),
                 deps=[d for d in deps if d is not None], sig=False, dma=(slot, cnt))
        self.ops[eng].append(o)
        tok = ('d', slot, cnt)
        if is_out:
            self.out_toks.append(tok)
        return tok

    def mm(self, out, lhsT, rhs, start=True, stop=True, deps=()):
        return self.op('pe', lambda e: e.matmul(out, lhsT, rhs, start=start, stop=stop), deps)

    def tr(self, out, in_, ident, deps=()):
        return self.op('pe', lambda e: e.transpose(out, in_, ident), deps)

    def act(self, out, in_, func, bias=None, scale=None, deps=(), accum_out=None):
        kw = {}
        if bias is not None:
            kw['bias'] = bias
        if scale is not None:
            kw['scale'] = scale
        if accum_out is not None:
            kw['accum_out'] = accum_out
        return self.op('act', lambda e: e.activation(out, in_, func, **kw), deps)

    def tt(self, out, in0, in1, op, deps=(), eng='dve'):
        return self.op(eng, lambda e: e.tensor_tensor(out, in0, in1, op), deps)

    def ts(self, out, in0, s1, s2, op0, op1=None, deps=(), eng='dve'):
        if op1 is None:
            return self.op(eng, lambda e: e.tensor_scalar(out, in0, s1, None, op0), deps)
        return self.op(eng, lambda e: e.tensor_scalar(out, in0, s1, s2, op0, op1), deps)

    def stt(self, out, in0, scalar, in1, op0, op1, deps=()):
        return self.op('dve', lambda e: e.scalar_tensor_tensor(out, in0, scalar, in1, op0, op1), deps)

    def cp(self, out, in_, deps=(), eng='dve'):
        if eng == 'act':
            return self.op('act', lambda e: e.copy(out, in_), deps)
        return self.op(eng, lambda e: e.tensor_copy(out, in_), deps)

    def recip(self, out, in_, deps=()):
        return self.op('dve', lambda e: e.reciprocal(out, in_), deps)

    def memset(self, ap, val, deps=(), eng='dve'):
        return self.op(eng, lambda e: e.memset(ap, val), deps)

    def build(self):
        nc = self.nc
        for eng in self.ENG:
            for o in self.ops[eng]:
                for d in o['deps']:
                    if d[0] == 'c':
                        self.ops[d[1]][d[2]]['sig'] = True
        for eng in self.ENG:
            c = 0
            for o in self.ops[eng]:
                if o['sig'] and o['dma'] is None:
                    c += 1
                    o['cnt'] = c
        if self.out_toks:
            self.ops['sp'].append(dict(fn=None, deps=list(self.out_toks), sig=False, dma=None))
        self.check_deadlock()
        self.sem = {e: self.es.enter_context(nc.semaphore('s_' + e)) for e in self.ENG}
        self.dsem = {s: self.es.enter_context(nc.semaphore('d_' + s)) for s in self.dma_slots}
        block = self.es.enter_context(nc.Block())

        def replay(name, e):
            waited = {}
            for o in self.ops[name]:
                for d in o['deps']:
                    if d[0] == 'c':
                        key = ('c', d[1]); sem = self.sem[d[1]]; val = self.ops[d[1]][d[2]]['cnt']
                    else:
                        key = ('d', d[1]); sem = self.dsem[d[1]]; val = 16 * d[2]
                    if waited.get(key, 0) < val:
                        e.wait_ge(sem, val)
                        waited[key] = val
                if o['fn'] is None:
                    continue
                try:
                    ins = o['fn'](e)
                except Exception:
                    print('EMIT FAIL engine', name, 'op index', self.ops[name].index(o), 'dma', o['dma'])
                    raise
                if o['dma'] is not None:
                    ins.then_inc(self.dsem[o['dma'][0]], 16)
                elif o['sig']:
                    ins.then_inc(self.sem[name], 1)

        @block.tensor
        def _(e):
            replay('pe', e)

        @block.scalar
        def _(e):
            replay('act', e)

        @block.vector
        def _(e):
            replay('dve', e)

        @block.gpsimd
        def _(e):
            replay('pool', e)

        @block.sync
        def _(e):
            replay('sp', e)

        self.es.close()
        return nc

    def check_deadlock(self):
        ptr = {e: 0 for e in self.ENG}
        done_c = {e: -1 for e in self.ENG}
        dcnt = {s: 0 for s in self.dma_slots}
        total = sum(len(v) for v in self.ops.values())
        ndone = 0
        while ndone < total:
            prog = False
            for e in self.ENG:
                while ptr[e] < len(self.ops[e]):
                    o = self.ops[e][ptr[e]]
                    ok = True
                    for d in o['deps']:
                        if d[0] == 'c':
                            if done_c[d[1]] < d[2]:
                                ok = False; break
                        else:
                            if dcnt[d[1]] < d[2]:
                                ok = False; break
                    if not ok:
                        break
                    if o['dma'] is not None:
                        dcnt[o['dma'][0]] += 1
                    done_c[e] = ptr[e]
                    ptr[e] += 1; ndone += 1; prog = True
            if not prog:
                msg = []
                for e in self.ENG:
                    if ptr[e] < len(self.ops[e]):
                        o = self.ops[e][ptr[e]]
                        msg.append('%s blocked at op %d deps=%s' % (e, ptr[e], o['deps']))
                raise RuntimeError('DEADLOCK: ' + ' | '.join(msg))

    def run(self, in_maps, trace=False):
        nc = self.build()
        n = len(in_maps)
        res = run_bass_kernel_spmd(nc, in_maps, core_ids=list(range(n)), trace=trace)
        return res

import math
import ml_dtypes


def build_M():
    P = Prog()
    c_in = P.din('c', [128, 16])
    w_in = P.din('w', [2048, 3072])
    b_in = P.din('b', [1, 3072])
    o = P.dout('mod', [1, 3072])
    cs = P.sb('cs', [128, 16]); cond = P.sb('cond', [128, 16])
    wb = [P.sb('wb%d' % i, [128, 16, 512]) for i in range(2)]
    bb = P.sb('bb', [1, 3072]); ob = P.sb('ob', [1, 3072])
    ps = [P.ps('ps%d' % i) for i in range(2)]
    t_c = P.dma('sp', cs[:], c_in[:, :])
    t_b = P.dma('sp', bb[:], b_in[:, :])
    t_cond = P.act(cond[:], cs[:], AF.Silu, deps=[t_c])
    wv = w_in.rearrange("(kc p) n -> p kc n", p=128)
    ev = [None, None]
    outs = []
    for j in range(6):
        t_w = P.dma('sp' if j % 2 == 0 else 'act', wb[j % 2][:], wv[:, :, j * 512:(j + 1) * 512], deps=[ev[j % 2]], slot='w%d' % (j % 2))
        last = None
        for kc in range(16):
            last = P.mm(ps[j % 2][0:1, :], cond[:, kc:kc + 1], wb[j % 2][:, kc, :], start=(kc == 0), stop=(kc == 15),
                        deps=[t_w, t_cond, ev[j % 2]] if kc == 0 else [])
        ev[j % 2] = P.tt(ob[:, j * 512:(j + 1) * 512], ps[j % 2][0:1, :], bb[:, j * 512:(j + 1) * 512], ALU.add, deps=[last, t_b])
        outs.append(ev[j % 2])
    P.dma('sp', o[:, :], ob[:], deps=outs, is_out=True)
    return P


TWO_PI = 2 * math.pi
MAGIC = 12582912.0

def consts_A():
    invf = (10000.0 ** (-np.arange(0, 128, 2, dtype=np.float32) / 128)).astype(np.float32)
    invf = np.concatenate([invf, invf])[None, :].astype(np.float32)
    sign = np.concatenate([-np.ones(64), np.ones(64)]).astype(np.float32)[:, None]
    swap = np.zeros((128, 128), np.float32)
    for e in range(128):
        swap[(e + 64) % 128, e] = 1.0
    return dict(invf=invf, sign=sign, swap=swap)

def build_A(cc_list=None):
    P = Prog()
    xT = P.din('xT', [2048, 1024]); modv = P.din('modv', [128, 6, 16]); g1n = P.din('gn', [128, 16])
    w = P.din('w', [2048, 7680]); pos = P.din('pos', [1, 1024], I32)
    invf_d = P.din('invf', [1, 128]); sign_d = P.din('sign', [128, 1]); swap_d = P.din('swap', [128, 128])
    qT = P.dout('qT', [1536, 1024], BF16); kT = P.dout('kT', [1536, 1024], BF16); vT = P.dout('vT', [1536, 1024], BF16)
    xrT = P.dout('xrT', [1536, 1024]); grT = P.dout('grT', [1536, 1024])

    x = P.sb('x', [128, 16, 1024]); h = P.sb('h', [128, 16, 1024], BF16); sq = h
    mods = P.sb('mods', [128, 6, 16]); gn = P.sb('gns', [128, 16]); G = P.sb('G', [128, 16])
    ones = P.sb('ones', [128, 128], BF16); swp = P.sb('swp', [128, 128], BF16)
    invf = P.sb('invfs', [1, 128]); sign = P.sb('signs', [128, 1]); posi = P.sb('posi', [1, 1024], I32); posf = P.sb('posf', [1, 1024])
    rstd = P.sb('rstd', [128, 1024]); tmp = [P.sb('tmp%d' % i, [128, 1024]) for i in range(2)]
    cosT = P.sb('cosT', [128, 1024]); sinT = P.sb('sinT', [128, 1024])
    r1 = tmp[0]; r2 = tmp[1]
    wb = [P.sb('wb%d' % i, [128, 16, 512], BF16) for i in range(2)]
    tb = [P.sb('tb%d' % i, [128, 512], BF16) for i in range(2)]
    ta = [P.sb('ta%d' % i, [128, 512]) for i in range(2)]
    tb2 = [P.sb('tbb%d' % i, [128, 512]) for i in range(2)]
    ost = [P.sb('ost%d' % i, [128, 1024], BF16) for i in range(2)]
    ost32 = [P.sb('ost32_%d' % i, [128, 1024]) for i in range(2)]
    pm = [P.ps('pm%d' % i) for i in range(4)]
    pr = [P.ps('pr%d' % i) for i in range(2)]
    pz = [P.ps('pz%d' % i) for i in range(2)]

    t_x = [P.dma('sp', x[:, kc * 4:(kc + 1) * 4, :], xT.rearrange("(kc p) t -> p kc t", p=128)[:, kc * 4:(kc + 1) * 4, :]) for kc in range(4)]
    t_m = P.dma('act', mods[:], modv[:, :, :]); t_g = P.dma('act', gn[:], g1n[:, :])
    t_if = P.dma('act', invf[:], invf_d[:, :]); t_sg = P.dma('act', sign[:], sign_d[:, :])
    t_pos = P.dma('act', posi[:], pos[:, :])
    t_sw = P.dma('pool', swp[:], swap_d[:, :])
    t_ones = P.memset(ones[:], 1.0)
    wv = w.rearrange("(kc p) n -> p kc n", p=128)
    NWB = 15
    t_w = [None] * NWB
    wfree = [None, None]
    def load_w(j, deps):
        t_w[j] = P.dma('pool', wb[j % 2][:], wv[:, :, j * 512:(j + 1) * 512], deps=deps, slot='w%d' % (j % 2))
    if cc_list is None:
        load_w(0, []); load_w(1, [])
    else:
        for jj in sorted(set(c // 4 for c in cc_list)):
            load_w(jj, [wfree[jj % 2]])
    t_G = P.stt(G[:], mods[:, 1, :], 1.0, gn[:], ALU.add, ALU.mult, deps=[t_m, t_g])
    t_pf = P.cp(posf[:], posi[:], deps=[t_pos])
    t_ang = []
    for hf in range(2):
        t_ang.append(P.mm(pz[hf][:], invf[:], posf[:, hf * 512:(hf + 1) * 512], deps=[t_if, t_pf]))
    def reduce_sin(dst, off, deps_extra):
        toks = []
        for hf in range(2):
            sl = slice(hf * 512, (hf + 1) * 512)
            a0 = P.ts(r1[:, sl], pz[hf][:], 1.0 / TWO_PI, off / TWO_PI, ALU.mult, ALU.add, deps=[t_ang[hf]] + deps_extra)
            a = P.ts(r1[:, sl], r1[:, sl], MAGIC, None, ALU.add, deps=[a0])
            b = P.ts(r1[:, sl], r1[:, sl], MAGIC, None, ALU.subtract, deps=[a])
            c = P.stt(r2[:, sl], r1[:, sl], -TWO_PI, pz[hf][:], ALU.mult, ALU.add, deps=[b])
            d = P.ts(r2[:, sl], r2[:, sl], off + math.pi, TWO_PI - 1e-5, ALU.add, ALU.min, deps=[c])
            e = P.ts(r2[:, sl], r2[:, sl], 1e-5, -math.pi, ALU.max, ALU.add, deps=[d])
            toks.append(e)
        return toks
    tk = reduce_sin(sinT, 0.0, [])
    t_sin = P.act(sinT[:], r2[:], AF.Sin, scale=sign[:], deps=tk + [t_sg])
    tk = reduce_sin(cosT, math.pi / 2, [t_sin])
    t_cos = P.act(cosT[:], r2[:], AF.Sin, deps=tk)
    t_sq = [P.act(sq[:, kc, :], x[:, kc, :], AF.Square, deps=[t_x[kc // 4]]) for kc in range(16)]
    t_ss = []
    for hf in range(2):
        last = None
        for kc in range(16):
            last = P.mm(pm[hf][:], ones[:], sq[:, kc, hf * 512:(hf + 1) * 512], start=(kc == 0), stop=(kc == 15), deps=[t_sq[kc], t_ones])
        t_ss.append(last)
    t_rs = []
    for hf in range(2):
        sl = slice(hf * 512, (hf + 1) * 512)
        a = P.act(rstd[:, sl], pm[hf][:], AF.Sqrt, bias=1e-6, scale=1.0 / 2048, deps=[t_ss[hf]])
        t_rs.append(P.recip(rstd[:, sl], rstd[:, sl], deps=[a]))
    t_h = []
    tfree = [t_cos, t_cos]
    for kc in range(16):
        a = P.tt(tmp[kc % 2][:], x[:, kc, :], rstd[:], ALU.mult, deps=t_rs + [t_x[kc // 4], tfree[kc % 2]])
        b = P.act(h[:, kc, :], tmp[kc % 2][:], AF.Identity, bias=mods[:, 0, kc:kc + 1], scale=G[:, kc:kc + 1], deps=[a, t_G])
        tfree[kc % 2] = b
        t_h.append(b)
    bank_free = [[t_rs[0]], [t_rs[1]], [], []]
    tb_free = [None, None]; pr_free = [None, None]; ta_free = [None, None]
    ost_free = [None, None]; ost32_free = [None, None]
    pending = None
    u = 0
    outs = [qT, kT, vT, xrT, grT]
    for cc in (cc_list if cc_list is not None else range(60)):
        j = cc // 4
        sec = cc // 12; row0 = (cc % 12) * 128
        half_toks = []
        for hf in range(2):
            sl = slice(hf * 512, (hf + 1) * 512)
            mb = u % 4
            last = None
            for kc in range(16):
                deps = [t_h[kc]]
                if kc == 0:
                    deps += [t_w[j]] + bank_free[mb]
                last = P.mm(pm[mb][:], wb[j % 2][:, kc, (cc % 4) * 128:(cc % 4 + 1) * 128], h[:, kc, sl], start=(kc == 0), stop=(kc == 15), deps=deps)
            if cc % 4 == 3 and hf == 1:
                if j + 2 < NWB and cc_list is None:
                    load_w(j + 2, [last])
            if sec < 2:
                i2 = u % 2
                e1 = P.cp(tb[i2][:], pm[mb][:], deps=[last, tb_free[i2]], eng='act')
                e3 = P.tt(ta[i2][:], pm[mb][:], cosT[:, sl], ALU.mult, deps=[last, t_cos, ta_free[i2], e1])
                bank_free[mb] = [e1, e3]
                if pending is not None:
                    pending()
                def mk(i2=i2, e1=e1, e3=e3, sl=sl, slot=cc % 2, hf=hf):
                    def f():
                        e2 = P.mm(pr[i2][:], swp[:], tb[i2][:], deps=[e1, t_sw, pr_free[i2]])
                        tb_free[i2] = e2
                        e4 = P.tt(tb2[i2][:], pr[i2][:], sinT[:, sl], ALU.mult, deps=[e2, t_sin])
                        pr_free[i2] = e4
                        e5 = P.tt(ost[slot][:, sl], ta[i2][:], tb2[i2][:], ALU.add, deps=[e3, e4, ost_free[slot]])
                        ta_free[i2] = e5
                        return e5
                    return f
                g = mk()
                res = {}
                def pend(g=g, res=res):
                    res['t'] = g()
                pending = pend
                half_toks.append(res)
            else:
                if pending is not None:
                    pending(); pending = None
                if sec == 2:
                    slot = cc % 2
                    e = P.cp(ost[slot][:, sl], pm[mb][:], deps=[last, ost_free[slot]], eng='act')
                else:
                    slot = cc % 2
                    e = P.cp(ost32[slot][:, sl], pm[mb][:], deps=[last, ost32_free[slot]], eng=('act' if hf == 0 else 'dve'))
                bank_free[mb] = [e]
                half_toks.append({'t': e})
            u += 1
        def mkout(cc=cc, sec=sec, row0=row0, half_toks=half_toks):
            def f():
                slot = cc % 2
                deps = [r['t'] for r in half_toks]
                if sec < 3:
                    t = P.dma('sp', outs[sec][row0:row0 + 128, :], ost[slot][:], deps=deps, slot='o%d' % slot, is_out=True)
                    ost_free[slot] = t
                else:
                    t = P.dma('sp', outs[sec][row0:row0 + 128, :], ost32[slot][:], deps=deps, slot='o32_%d' % slot, is_out=True)
                    ost32_free[slot] = t
            return f
        if sec < 2:
            prev_pending = pending
            outf = mkout()
            def pend2(prev_pending=prev_pending, outf=outf):
                prev_pending(); outf()
            pending = pend2
        else:
            mkout()()
    if pending is not None:
        pending()
    return P

def prep_A(core, x, mod_l, norm_g, w_in_l, positions):
    T0 = core * 1024
    d = dict(xT=np.ascontiguousarray(x[0, T0:T0 + 1024, :].T),
             modv=np.ascontiguousarray(mod_l.reshape(6, 16, 128).transpose(2, 0, 1)),
             gn=np.ascontiguousarray(norm_g.reshape(16, 128).T),
             w=w_in_l, pos=np.ascontiguousarray(positions[:, T0:T0 + 1024]))
    d.update(consts_A())
    return d


NEG = -1e30
NT = 53

def attn_tiles():
    groups = []
    for gi in range(4):
        g = []
        for i in (2 * gi, 2 * gi + 1):
            bank = i // 4; c0 = (i % 4) * 128
            g.append(dict(ks=2048 + 128 * (i - 1), kst=1, kp=128, qs=128 * i, qst=1, nq=128, m=i, vt=i, outs=[(bank, c0, 1, 128, 0)]))
            g.append(dict(ks=2048 + 128 * i, kst=1, kp=128, qs=128 * i, qst=1, nq=128, m=11, vt=i + 1, outs=[(bank, c0, 1, 128, 0)]))
        groups.append(g)
    for r in range(4):
        g = []
        for i in range(2):
            g.append(dict(ks=2048 + 512 * (i - 1) + r, kst=4, kp=128, qs=512 * i + r, qst=4, nq=128, m=8 + i, vt=9 + r * 3 + i, outs=[(i, r, 4, 128, 0)]))
            g.append(dict(ks=2048 + 512 * i + r, kst=4, kp=128, qs=512 * i + r, qst=4, nq=128, m=11, vt=9 + r * 3 + i + 1, outs=[(i, r, 4, 128, 0)]))
        groups.append(g)
    for r4 in range(4):
        g = []
        for r in range(4 * r4, 4 * r4 + 4):
            outs = [(0, r, 16, 32, 0), (1, r, 16, 32, 32)]
            g.append(dict(ks=r, kst=16, kp=128, qs=r, qst=16, nq=64, m=10, vt=21 + 2 * r, outs=outs))
            g.append(dict(ks=2048 + r, kst=16, kp=64, qs=r, qst=16, nq=64, m=11, vt=21 + 2 * r + 1, outs=outs))
        groups.append(g)
    return groups

def sl(start, step, n):
    return slice(start, start + step * (n - 1) + 1, step)

def emit_attn(P, q_d, k_d, v_d, mask_d, ident_d, out_d, psS, psN, psD):
    qb = [P.sb('qb%d' % i, [128, 1024], BF16) for i in range(2)]
    kb = [P.sb('kb%d' % i, [128, 3072], BF16) for i in range(2)]
    vb = [P.sb('vb%d' % i, [128, NT, 128], BF16) for i in range(2)]
    masks = P.sb('masks', [128, 12, 128], BF16); ident = P.sb('ident', [128, 128], BF16); ones = P.sb('onesb', [128, 128], BF16)
    pb = [P.sb('pb%d' % i, [128, 512], BF16) for i in range(2)]
    rden = P.sb('rden', [128, 1024]); ao = [P.sb('ao%d' % i, [128, 1024]) for i in range(2)]
    t_mask = P.dma('act', masks[:], mask_d[:, :, :]); t_id = P.dma('act', ident[:], ident_d[:, :])
    t_ones = P.memset(ones[:], 1.0)
    groups = attn_tiles()
    scale = 1.0 / math.sqrt(128.0)
    load_tok = [None, None]; buf_free = [[], []]
    def load(h):
        b = h % 2
        t1 = P.dma('sp', qb[b][:], q_d[:, h, :], deps=buf_free[b], slot='q%d' % b)
        t2 = P.dma('sp', kb[b][:], k_d[:, h, :], deps=buf_free[b], slot='k%d' % b)
        t3 = P.dma('sp', vb[b][:], v_d[:, h, :, :], deps=buf_free[b], slot='v%d' % b)
        load_tok[b] = [t1, t2, t3]
    load(0); load(1)
    S_free = [None, None]; pb_free = [None, None]
    nd_free = []
    ao_free = [None, None]
    gcount = 0
    for h in range(12):
        b = h % 2
        first_in_bank = {('N', 0): True, ('N', 1): True, ('D', 0): True, ('D', 1): True}
        pend = None
        last_pv = None
        def do_pv(g, sb_i, t_exp):
            nonlocal last_pv
            c0 = 0
            for t in g:
                for (bank, st, step, n, pc0) in t['outs']:
                    for kind, ps, lhs in (('N', psN, vb[b][0:t['kp'], t['vt'], :]), ('D', psD, ones[0:t['kp'], :])):
                        fst = first_in_bank[(kind, bank)]
                        first_in_bank[(kind, bank)] = False
                        last_pv = P.mm(ps[bank][:, sl(st, step, n)], lhs, pb[sb_i][0:t['kp'], c0 + pc0:c0 + pc0 + n],
                                       start=fst, stop=False, deps=[t_exp, t_ones] + (nd_free if fst else []))
                c0 += t['nq']
            return last_pv
        for g in groups:
            si = gcount % 2
            c0 = 0
            last = None
            for ti, t in enumerate(g):
                deps = load_tok[b] + [S_free[si], t_mask, t_id] if ti == 0 else []
                P.mm(psS[si][0:t['kp'], c0:c0 + t['nq']], kb[b][:, sl(t['ks'], t['kst'], t['kp'])], qb[b][:, sl(t['qs'], t['qst'], t['nq'])],
                     start=True, stop=False, deps=deps)
                mslice = masks[0:t['kp'], t['m'], 0:t['nq']]
                last = P.mm(psS[si][0:t['kp'], c0:c0 + t['nq']], ident[0:t['kp'], 0:t['kp']], mslice, start=False, stop=True)
                c0 += t['nq']
            t_exp = P.act(pb[si][:, 0:c0], psS[si][:, 0:c0], AF.Exp, scale=scale, deps=[last, pb_free[si]])
            S_free[si] = t_exp
            if pend is not None:
                pg, psi, ptexp = pend
                pb_free[psi] = do_pv(pg, psi, ptexp)
            pend = (g, si, t_exp)
            gcount += 1
        pg, psi, ptexp = pend
        pb_free[psi] = do_pv(pg, psi, ptexp)
        buf_free[b] = [last_pv]
        if h + 2 < 12:
            load(h + 2)
        oi = h % 2
        evs = []
        for bank in range(2):
            cs = slice(bank * 512, (bank + 1) * 512)
            a = P.recip(rden[:, cs], psD[bank][:], deps=[last_pv])
            e = P.tt(ao[oi][:, cs], psN[bank][:], rden[:, cs], ALU.mult, deps=[a, ao_free[oi]])
            evs.append(e)
        nd_free = evs
        ao_free[oi] = P.dma('act', out_d[h * 128:(h + 1) * 128, :], ao[oi][:], deps=evs, slot='ao%d' % oi, is_out=True)

def build_B(do_lru=True):
    P = Prog()
    q_d = P.din('q', [128, 12, 1024], BF16); k_d = P.din('k', [128, 12, 3072], BF16); v_d = P.din('v', [128, 12, NT, 128], BF16)
    mask_d = P.din('masks', [128, 12, 128], BF16); ident_d = P.din('ident', [128, 128], BF16)
    out_d = P.dout('attnT', [1536, 1024])
    psS = [P.ps('psS%d' % i) for i in range(2)]; psN = [P.ps('psN%d' % i) for i in range(2)]; psD = [P.ps('psD%d' % i) for i in range(2)]
    emit_attn(P, q_d, k_d, v_d, mask_d, ident_d, out_d, psS, psN, psD)
    if do_lru:
        psG = [P.ps('psG%d' % i) for i in range(2)]
        emit_lru(P, psG)
    return P

def emit_lru(P, psG):
    xr_d = P.din('xr', [128, 12, 1027]); cw_d = P.din('cw', [128, 12, 4]); vec_d = P.din('vecs', [128, 4, 12])
    wa_d = P.din('wa', [128, 12, 128]); wx_d = P.din('wx', [128, 12, 128]); pos_d = P.din('pos', [1, 1024], I32)
    hl_d = P.dout('hloc', [1536, 1024]); pp_d = P.dout('pprod', [1536, 1024])
    xr = P.sb('xr', [128, 12, 1027]); cw = P.sb('cw', [128, 12, 4]); vecs = P.sb('vecs', [128, 4, 12])
    wa = P.sb('wa', [128, 12, 128], BF16); wx = P.sb('wx', [128, 12, 128], BF16)
    posi = P.sb('lposi', [1, 1024], I32); nzr = P.sb('nzr', [1, 1024]); nz = P.sb('nz', [128, 1024]); ones1 = P.sb('ones1', [1, 128])
    sca = P.sb('sca', [128, 12]); sca2 = P.sb('sca2', [128, 12]); zeros = P.sb('zeros', [128, 1024])
    xc = P.sb('xc', [128, 1024]); xcb = P.sb('xcb', [128, 1024], BF16)
    rb = P.sb('rb', [128, 1024]); ib = P.sb('ib', [128, 1024]); ab = P.sb('ab', [128, 1024]); mb_ = P.sb('mb', [128, 1024])
    ho = [P.sb('ho%d' % i, [128, 1024]) for i in range(2)]; po = [P.sb('po%d' % i, [128, 1024]) for i in range(2)]
    t_xr = P.dma('sp', xr[:], xr_d[:, :, :]); t_cw = P.dma('sp', cw[:], cw_d[:, :, :]); t_v = P.dma('sp', vecs[:], vec_d[:, :, :])
    t_wa = P.dma('pool', wa[:], wa_d[:, :, :]); t_wx = P.dma('pool', wx[:], wx_d[:, :, :]); t_pos = P.dma('sp', posi[:], pos_d[:, :])
    t_z = P.memset(zeros[:], 0.0, eng='pool'); t_o1 = P.memset(ones1[:], 1.0, eng='pool')
    a = P.cp(nzr[:], posi[:], deps=[t_pos])
    a = P.ts(nzr[:], nzr[:], 0.0, None, ALU.not_equal, deps=[a])
    t_nz = []
    for hf in range(2):
        cs = slice(hf * 512, (hf + 1) * 512)
        m = P.mm(psG[hf][:], ones1[:], nzr[:, cs], deps=[a, t_o1])
        t_nz.append(P.cp(nz[:, cs], psG[hf][:], deps=[m]))
    s1 = P.act(sca[:], vecs[:, 3, :], AF.Exp, scale=-1.0, deps=[t_v])
    s2 = P.act(sca[:], sca[:], AF.Ln, bias=1.0, deps=[s1])
    s3 = P.ts(sca2[:], sca[:], -16.0, None, ALU.mult, deps=[s2])
    s4 = P.ts(sca[:], sca[:], -8.0, None, ALU.mult, deps=[s3])
    g_free = t_nz
    prev = []
    ho_free = [None, None]; po_free = [None, None]
    for b in range(12):
        c = P.act(xc[:], xr[:, b, 3:1027], AF.Identity, bias=vecs[:, 0, b:b + 1], scale=cw[:, b, 3:4], deps=[t_xr, t_cw, t_v] + prev)
        for k in range(3):
            c = P.stt(xc[:], xr[:, b, k:k + 1024], cw[:, b, k:k + 1], xc[:], ALU.mult, ALU.add, deps=[c])
        cb = P.cp(xcb[:], xc[:], deps=[c] + prev, eng='pool')
        gr_ = []
        for gi, (wt, bias_i, dst, tw) in enumerate(((wa, 1, rb, t_wa), (wx, 2, ib, t_wx))):
            evs = []
            for hf in range(2):
                cs = slice(hf * 512, (hf + 1) * 512)
                m = P.mm(psG[hf][:], wt[:, b, :], xcb[:, cs], deps=[cb, tw] + (g_free if isinstance(g_free, list) else [g_free]))
                evs.append(P.act(dst[:, cs], psG[hf][:], AF.Sigmoid, bias=vecs[:, bias_i, b:b + 1], deps=[m] + prev))
            g_free = evs
            gr_.append(evs)
        ta_ = P.act(ab[:], rb[:], AF.Exp, scale=sca[:, b:b + 1], deps=gr_[0] + [s4] + prev)
        tm = P.act(mb_[:], rb[:], AF.Exp, scale=sca2[:, b:b + 1], deps=gr_[0] + [s4] + prev)
        tm = P.act(mb_[:], mb_[:], AF.Sqrt, scale=-1.0, bias=1.0, deps=[tm])
        ta2 = P.tt(ab[:], ab[:], nz[:], ALU.mult, deps=[ta_] + t_nz)
        tm = P.stt(mb_[:], mb_[:], -1.0, nz[:], ALU.add, ALU.mult, deps=[tm] + t_nz)
        tm = P.ts(mb_[:], mb_[:], 1.0, None, ALU.add, deps=[tm])
        tb_ = P.tt(ib[:], ib[:], xc[:], ALU.mult, deps=gr_[1] + [c])
        tb_ = P.tt(ib[:], ib[:], mb_[:], ALU.mult, deps=[tb_, tm])
        oi = b % 2
        sc1 = P.op('dve', lambda e, oi=oi: e.tensor_tensor_scan(ho[oi][:], ab[:], ib[:], 0.0, ALU.mult, ALU.add), deps=[ta2, tb_, ho_free[oi]])
        sc2 = P.op('dve', lambda e, oi=oi: e.tensor_tensor_scan(po[oi][:], ab[:], zeros[:], 1.0, ALU.mult, ALU.add), deps=[ta2, t_z, po_free[oi]])
        ho_free[oi] = P.dma('sp', hl_d[b * 128:(b + 1) * 128, :], ho[oi][:], deps=[sc1], slot='ho%d' % oi, is_out=True)
        po_free[oi] = P.dma('sp', pp_d[b * 128:(b + 1) * 128, :], po[oi][:], deps=[sc2], slot='po%d' % oi, is_out=True)
        prev = [sc1, sc2, cb]

def bf(a):
    return np.ascontiguousarray(a).astype(ml_dtypes.bfloat16)

def consts_B(core):
    kk = np.arange(128)[:, None]; qi = np.arange(128)[None, :]
    A = np.where(kk >= qi, 0.0, NEG).astype(np.float32)
    Bm = np.where(kk <= qi, 0.0, NEG).astype(np.float32)
    allneg = np.full((128, 128), NEG, np.float32)
    m = np.zeros((128, 12, 128), np.float32)
    for i in range(8):
        m[:, i, :] = allneg if (core == 0 and i == 0) else A
    for i in range(2):
        m[:, 8 + i, :] = allneg if (core == 0 and i == 0) else A
    if core == 0:
        m[:, 10, :] = allneg
    elif core == 1:
        mm_ = A.copy(); mm_[:64, :] = NEG
        m[:, 10, :] = mm_
    else:
        m[:, 10, :] = A
    m[:, 11, :] = Bm
    return dict(masks=bf(m), ident=bf(np.eye(128, dtype=np.float32)))

def prep_B_attn(core, qT_all, kT_all, vT_all):
    T0 = core * 1024
    q = qT_all[:, T0:T0 + 1024].reshape(12, 128, 1024).transpose(1, 0, 2)
    kpad = np.zeros((1536, 2048 + 8192), dtype=kT_all.dtype); kpad[:, 2048:] = kT_all
    k = kpad[:, T0:T0 + 3072].reshape(12, 128, 3072).transpose(1, 0, 2)
    vtok = np.zeros((2048 + 8192, 1536), dtype=vT_all.dtype); vtok[2048:] = vT_all.T
    base = T0 + 2048
    idx = np.zeros((NT, 128), np.int64); valid = np.ones((NT, 128), bool)
    for j in range(9):
        idx[j] = base + 128 * (j - 1) + np.arange(128)
    for r in range(4):
        for j in range(3):
            idx[9 + r * 3 + j] = base + 512 * (j - 1) + 4 * np.arange(128) + r
    for r in range(16):
        idx[21 + 2 * r] = base - 2048 + 16 * np.arange(128) + r
        ii = base + 16 * np.arange(128) + r
        valid[21 + 2 * r + 1, 64:] = False
        ii[64:] = 0
        idx[21 + 2 * r + 1] = ii
    vt = vtok[idx]
    vt[~valid] = 0
    v = vt.reshape(NT, 128, 12, 128).transpose(1, 2, 0, 3)
    d = dict(q=np.ascontiguousarray(q), k=np.ascontiguousarray(k), v=np.ascontiguousarray(v))
    d.update(consts_B(core))
    return d

def prep_B_lru(core, xrT_all, conv_w, conv_b, wa, ba, wx, bx, lam, positions):
    T0 = core * 1024
    xpad = np.zeros((1536, 3 + 8192), np.float32); xpad[:, 3:] = xrT_all
    xr = xpad[:, T0:T0 + 1027].reshape(12, 128, 1027).transpose(1, 0, 2)
    cw = conv_w.T.reshape(12, 128, 4).transpose(1, 0, 2)
    f = lambda v: v.reshape(12, 128).T
    vecs = np.stack([f(conv_b), f(ba), f(bx), f(lam)], axis=1)
    return dict(xr=np.ascontiguousarray(xr), cw=np.ascontiguousarray(cw), vecs=np.ascontiguousarray(vecs),
                wa=np.ascontiguousarray(wa.transpose(1, 0, 2)), wx=np.ascontiguousarray(wx.transpose(1, 0, 2)),
                pos=np.ascontiguousarray(positions[:, T0:T0 + 1024]))


def build_C1():
    P = Prog()
    xT_d = P.din('xT', [2048, 1024]); at_d = P.din('attnT', [1536, 1024]); hl_d = P.din('hloc', [1536, 1024])
    pp_d = P.din('pprod', [1536, 1024]); gr_d = P.din('grT', [1536, 1024])
    summ_d = P.din('summ', [128, 8, 2, 12]); sel_d = P.din('sel', [128, 8])
    modv_d = P.din('modv', [128, 6, 16]); g2n_d = P.din('g2n', [128, 16]); og_d = P.din('og', [128, 24])
    wo_d = P.din('wo', [3072, 2048]); rw_d = P.din('rw', [128, 16, 32]); rb_d = P.din('rb', [1, 32])
    utri_d = P.din('utri', [128, 128], BF16)
    x1_o = P.dout('x1T', [2048, 1024]); h2_o = P.dout('h2T', [2048, 1024], BF16)
    G_o = P.dout('G', [128, 8, 32]); pm_o = P.dout('pm', [128, 8, 32]); cnt_o = P.dout('cnt', [128, 32])

    x = P.sb('x', [128, 16, 1024])
    y = P.sb('y', [128, 24, 1024], BF16)
    st = [P.sb('st%d' % i, [128, 1024]) for i in range(2)]
    st2 = [P.sb('st2_%d' % i, [128, 1024]) for i in range(2)]
    st3 = [P.sb('st3_%d' % i, [128, 1024]) for i in range(2)]
    sq = [P.sb('sq%d' % i, [128, 1024], BF16) for i in range(2)]
    summ = P.sb('summ', [128, 8, 2, 12]); sel = P.sb('sel', [128, 8]); Hc = P.sb('Hc', [128, 12]); Hs = P.sb('Hs', [128, 12])
    mods = P.sb('mods', [128, 6, 16]); g2n = P.sb('g2n', [128, 16]); og = P.sb('og', [128, 24]); G2 = P.sb('G2', [128, 16])
    ones = P.sb('ones', [128, 128], BF16); utri = P.sb('utri', [128, 128], BF16); ones1 = P.sb('ones1', [1, 128])
    rw = P.sb('rw', [128, 16, 32]); rb = P.sb('rb', [1, 32])
    rstdA = P.sb('rstdA', [128, 1024]); rstdL = P.sb('rstdL', [128, 1024])
    wb = [P.sb('wb%d' % i, [128, 24, 256], BF16) for i in range(2)]
    t1 = [P.sb('t1_%d' % i, [128, 512]) for i in range(2)]; t2 = [P.sb('t2_%d' % i, [128, 512]) for i in range(2)]
    hb = [P.sb('hb%d' % i, [128, 1024], BF16) for i in range(2)]
    lg = P.sb('lg', [128, 8, 32]); top8 = P.sb('top8', [128, 8, 8]); nmax = P.sb('nmax', [128, 8]); maskf = P.sb('maskf', [128, 8, 32])
    maskb = P.sb('maskb', [128, 8, 32], BF16); ex = P.sb('ex', [128, 8, 32]); den = P.sb('den', [128, 8]); Gs = P.sb('Gs', [128, 8, 32]); pm = P.sb('pm', [128, 8, 32])
    ps = [P.ps('b%d' % i) for i in range(8)]

    t_x = [P.dma('sp', x[:, kc * 4:(kc + 1) * 4, :], xT_d.rearrange("(kc p) t -> p kc t", p=128)[:, kc * 4:(kc + 1) * 4, :]) for kc in range(4)]
    t_su = P.dma('act', summ[:], summ_d[:, :, :, :]); t_sel = P.dma('act', sel[:], sel_d[:, :])
    t_m = P.dma('act', mods[:], modv_d[:, :, :]); t_g2 = P.dma('act', g2n[:], g2n_d[:, :]); t_og = P.dma('act', og[:], og_d[:, :])
    t_rw = P.dma('act', rw[:], rw_d[:, :, :]); t_rb = P.dma('act', rb[:], rb_d[:, :]); t_ut = P.dma('act', utri[:], utri_d[:, :])
    t_ones = P.memset(ones[:], 1.0); t_o1 = P.memset(ones1[:], 1.0)
    wv = wo_d.rearrange("(kc p) n -> p kc n", p=128)
    t_w = [None] * 8
    def load_w(j, deps):
        t_w[j] = P.dma('pool', wb[j % 2][:], wv[:, :, j * 256:(j + 1) * 256], deps=deps, slot='w%d' % (j % 2))
    load_w(0, []); load_w(1, [])
    a = P.memset(Hc[:], 0.0, deps=[]); b = P.memset(Hs[:], 0.0)
    tH = [a, b]
    for c in range(8):
        s = P.stt(Hs[:], Hc[:], sel[:, c:c + 1], Hs[:], ALU.mult, ALU.add, deps=tH + [t_sel, t_su])
        u = P.tt(Hc[:], Hc[:], summ[:, c, 0, :], ALU.mult, deps=[s])
        u = P.tt(Hc[:], Hc[:], summ[:, c, 1, :], ALU.add, deps=[u])
        tH = [u]
    t_Hs = tH
    G2t = P.stt(G2[:], mods[:, 4, :], 1.0, g2n[:], ALU.add, ALU.mult, deps=[t_m, t_g2])
    st_free = [None, None]; st2_free = [None, None]; st3_free = [None, None]; sq_free = [None, None]
    lastA = [None, None]; lastL = [None, None]
    t_y = []
    for ci in range(24):
        i = ci % 2
        if ci < 12:
            b_ = ci
            ld = P.dma('sp', st[i][:], at_d[b_ * 128:(b_ + 1) * 128, :], deps=[st_free[i]], slot='st%d' % i)
            src_ready = [ld]
        else:
            b_ = ci - 12
            l1 = P.dma('sp', st[i][:], hl_d[b_ * 128:(b_ + 1) * 128, :], deps=[st_free[i]], slot='st%d' % i)
            l2 = P.dma('sp', st2[i][:], pp_d[b_ * 128:(b_ + 1) * 128, :], deps=[st2_free[i]], slot='st2_%d' % i)
            l3 = P.dma('sp', st3[i][:], gr_d[b_ * 128:(b_ + 1) * 128, :], deps=[st3_free[i]], slot='st3_%d' % i)
            hf_ = P.stt(st[i][:], st2[i][:], Hs[:, b_:b_ + 1], st[i][:], ALU.mult, ALU.add, deps=[l1, l2] + t_Hs)
            ge = P.act(st2[i][:], st3[i][:], AF.Gelu_apprx_tanh, deps=[l3, hf_])
            st3_free[i] = ge
            lr = P.tt(st[i][:], st[i][:], st2[i][:], ALU.mult, deps=[hf_, ge])
            st2_free[i] = lr
            src_ready = [lr]
        s_ = P.act(sq[i][:], st[i][:], AF.Square, deps=src_ready + [sq_free[i]])
        mmt = None
        for hf in range(2):
            bank = (0 if ci < 12 else 2) + hf
            mmt = P.mm(ps[bank][:], ones[:], sq[i][:, hf * 512:(hf + 1) * 512], start=(ci % 12 == 0), stop=(ci % 12 == 11), deps=[s_, t_ones])
            if ci < 12: lastA[hf] = mmt
            else: lastL[hf] = mmt
        sq_free[i] = mmt
        yy = P.ts(y[:, ci, :], st[i][:], og[:, ci:ci + 1], None, ALU.mult, deps=src_ready + [t_og], eng='pool')
        st_free[i] = [yy, s_]
        st_free[i] = yy
        st_free[i] = P.op('pool', lambda e: e.memset(ones1[0:1, 0:1], 1.0), deps=[yy, s_])
        t_y.append(yy)
    t_rA = []; t_rL = []
    for hf in range(2):
        cs = slice(hf * 512, (hf + 1) * 512)
        a = P.act(rstdA[:, cs], ps[hf][:], AF.Sqrt, bias=1e-6, scale=1.0 / 1536, deps=[lastA[hf]])
        t_rA.append(P.recip(rstdA[:, cs], rstdA[:, cs], deps=[a]))
        a = P.act(rstdL[:, cs], ps[2 + hf][:], AF.Sqrt, bias=1e-6, scale=1.0 / 1536, deps=[lastL[hf]])
        t_rL.append(P.recip(rstdL[:, cs], rstdL[:, cs], deps=[a]))
    bank_free = {4: None, 5: None, 6: None, 7: None}
    tfree = [None, None]
    t_x1 = []
    u = 0
    for dmc in range(16):
        j = dmc // 2
        for hf in range(2):
            cs = slice(hf * 512, (hf + 1) * 512)
            bA = 4 + (u % 2) * 2; bL = bA + 1
            la = None; ll = None
            for ci in range(12):
                la = P.mm(ps[bA][:], wb[j % 2][:, ci, (dmc % 2) * 128:(dmc % 2 + 1) * 128], y[:, ci, cs], start=(ci == 0), stop=(ci == 11),
                          deps=[t_y[ci]] + ([t_w[j], bank_free[bA]] if ci == 0 else []))
            for ci in range(12, 24):
                ll = P.mm(ps[bL][:], wb[j % 2][:, ci, (dmc % 2) * 128:(dmc % 2 + 1) * 128], y[:, ci, cs], start=(ci == 12), stop=(ci == 23),
                          deps=[t_y[ci]] + ([bank_free[bL]] if ci == 12 else []))
            if dmc % 2 == 1 and hf == 1 and j + 2 < 8:
                load_w(j + 2, [ll])
            i2 = u % 2
            e1 = P.tt(t1[i2][:], ps[bA][:], rstdA[:, cs], ALU.mult, deps=[la, t_rA[hf], tfree[i2]])
            e2 = P.tt(t2[i2][:], ps[bL][:], rstdL[:, cs], ALU.mult, deps=[ll, t_rL[hf], tfree[i2]])
            bank_free[bA] = e1; bank_free[bL] = e2
            e3 = P.tt(t1[i2][:], t1[i2][:], t2[i2][:], ALU.add, deps=[e1, e2], eng='pool')
            e4 = P.stt(x[:, dmc, cs], t1[i2][:], mods[:, 2, dmc:dmc + 1], x[:, dmc, cs], ALU.mult, ALU.add, deps=[e3, t_m, t_x[dmc // 4]])
            tfree[i2] = e4
            t_x1.append(e4)
            u += 1
    for kc in range(4):
        P.dma('sp', x1_o.rearrange("(kc p) t -> p kc t", p=128)[:, kc * 4:(kc + 1) * 4, :], x[:, kc * 4:(kc + 1) * 4, :], deps=t_x1[kc * 8:(kc + 1) * 8], is_out=True)
    sq_free = [t_y[-1], t_y[-1]]
    lastN = [None, None]
    for kc in range(16):
        i = kc % 2
        s_ = P.act(sq[i][:], x[:, kc, :], AF.Square, deps=t_x1[2 * kc:2 * kc + 2] + [sq_free[i]])
        for hf in range(2):
            lastN[hf] = P.mm(ps[hf][:], ones[:], sq[i][:, hf * 512:(hf + 1) * 512], start=(kc == 0), stop=(kc == 15), deps=[s_] + (t_rA + t_rL if kc == 0 else []))
        sq_free[i] = lastN[1]
    rstd2 = rstdA
    t_r2 = []
    for hf in range(2):
        cs = slice(hf * 512, (hf + 1) * 512)
        a = P.act(rstd2[:, cs], ps[hf][:], AF.Sqrt, bias=1e-6, scale=1.0 / 2048, deps=[lastN[hf]] + t_x1)
        t_r2.append(P.recip(rstd2[:, cs], rstd2[:, cs], deps=[a]))
    LG = ps[2]
    st_free = [None, None]; st2_free = [None, None]; hb_free = [None, None]
    last_r = None
    for kc in range(16):
        i = kc % 2
        a = P.tt(st[i][:], x[:, kc, :], rstd2[:], ALU.mult, deps=t_r2 + [st_free[i]])
        hh = P.act(st2[i][:], st[i][:], AF.Identity, bias=mods[:, 3, kc:kc + 1], scale=G2[:, kc:kc + 1], deps=[a, G2t, st2_free[i]])
        st_free[i] = hh
        for g in range(8):
            last_r = P.mm(LG[:, g * 32:(g + 1) * 32], st2[i][:, g * 128:(g + 1) * 128], rw[:, kc, :], start=(kc == 0 and g == 0), stop=False,
                          deps=[hh, t_rw] + t_rL)
        cb = P.cp(hb[i][:], st2[i][:], deps=[hh, hb_free[i]], eng='pool')
        st2_free[i] = P.op('pool', lambda e: e.memset(ones1[0:1, 0:1], 1.0), deps=[cb, last_r])
        hb_free[i] = P.dma('sp', h2_o[kc * 128:(kc + 1) * 128, :], hb[i][:], deps=[cb], slot='hb%d' % i, is_out=True)
    for g in range(8):
        last_r = P.mm(LG[:, g * 32:(g + 1) * 32], ones1[:, :], rb[:, :], start=False, stop=(g == 7), deps=[t_rb, t_o1])
    c0 = P.cp(lg[:].rearrange("p g e -> p (g e)"), LG[:, 0:256], deps=[last_r])
    tk = []
    for g in range(8):
        m = P.op('dve', lambda e, g=g: e.max(top8[:, g, :], lg[:, g, :]), deps=[c0])
        tk.append(m)
    n1 = P.ts(nmax[:], top8[:, :, 0], -1.0, None, ALU.mult, deps=tk)
    last = n1
    for g in range(8):
        mk = P.ts(maskf[:, g, :], lg[:, g, :], top8[:, g, 3:4], None, ALU.is_ge, deps=[last])
        e_ = P.act(ex[:, g, :], lg[:, g, :], AF.Exp, bias=nmax[:, g:g + 1], deps=[n1])
        em = P.tt(ex[:, g, :], ex[:, g, :], maskf[:, g, :], ALU.mult, deps=[mk, e_])
        dn = P.op('dve', lambda e, g=g: e.tensor_reduce(den[:, g:g + 1], ex[:, g, :], mybir.AxisListType.X, ALU.add), deps=[em])
        last = dn
    rd = P.recip(den[:], den[:], deps=[last])
    for g in range(8):
        last = P.ts(Gs[:, g, :], ex[:, g, :], den[:, g:g + 1], None, ALU.mult, deps=[rd])
    P.dma('sp', G_o[:, :, :], Gs[:], deps=[last], is_out=True)
    mb = P.cp(maskb[:], maskf[:], deps=[last])
    PO = ps[3]
    lp = None
    for g in range(8):
        lp = P.mm(PO[:, g * 32:(g + 1) * 32], utri[:], maskb[:, g, :], start=(g == 0), stop=False, deps=[mb, t_ut] + t_r2)
        for g2 in range(g):
            lp = P.mm(PO[:, g * 32:(g + 1) * 32], ones[:], maskb[:, g2, :], start=False, stop=False)
    lc_ = None
    for g in range(8):
        lc_ = P.mm(PO[:, 256:288], ones[:], maskb[:, g, :], start=False, stop=False, deps=[mb])
    cnts = P.sb('cnts', [128, 32])
    cc_ = P.cp(cnts[:], PO[:, 256:288], deps=[lc_])
    P.dma('sp', cnt_o[:, :], cnts[:], deps=[cc_], is_out=True)
    pmf = pm[:].rearrange("p g e -> p (g e)")
    a = P.stt(pmf, PO[:, 0:256], 1.0, maskf[:].rearrange("p g e -> p (g e)"), ALU.add, ALU.mult, deps=[lp, cc_])
    a = P.ts(pmf, pmf, -1.0, None, ALU.add, deps=[a])
    P.dma('sp', pm_o[:, :, :], pm[:], deps=[a], is_out=True)
    return P

def prep_C1(core, xT_core, attnT, hloc_all, pprod_all, grT_core, mod_l, norm2_g, attn_g, lru_g, w_out_l, router_w_l, router_b_l):
    summ = np.zeros((128, 8, 2, 12), np.float32)
    for c in range(8):
        summ[:, c, 0, :] = pprod_all[c][:, -1].reshape(12, 128).T
        summ[:, c, 1, :] = hloc_all[c][:, -1].reshape(12, 128).T
    sel = np.zeros((128, 8), np.float32); sel[:, core] = 1.0
    og = np.concatenate([attn_g.reshape(12, 128).T, lru_g.reshape(12, 128).T], axis=1)
    utri = (np.arange(128)[:, None] < np.arange(128)[None, :]).astype(np.float32)
    return dict(xT=xT_core, attnT=attnT, hloc=hloc_all[core], pprod=pprod_all[core], grT=grT_core, summ=summ, sel=sel,
                modv=np.ascontiguousarray(mod_l.reshape(6, 16, 128).transpose(2, 0, 1)), g2n=np.ascontiguousarray(norm2_g.reshape(16, 128).T),
                og=np.ascontiguousarray(og), wo=w_out_l, rw=np.ascontiguousarray(router_w_l.reshape(16, 128, 32).transpose(1, 0, 2)),
                rb=np.ascontiguousarray(router_b_l[None, :]), utri=utri.astype(ml_dtypes.bfloat16))


SCS = 1792
NBS = SCS // 128
NSC = 2
CAPT = SCS * NSC
NBT = CAPT // 128
BIG = 1000000.0
SUBS = [(0, 512), (512, 512), (1024, 512), (1536, 256)]

def _ind(P, kind, out, in_, idx_ap, deps, slot, **kw):
    P.dma_slots[slot] = P.dma_slots.get(slot, 0) + 1
    cnt = P.dma_slots[slot]
    if not hasattr(P, 'regcache'):
        P.regcache = {}
    if 'bounds_check' in kw:
        bval = kw.pop('bounds_check')
        okw = dict(kw)
        def fix(e, okw=okw, bval=bval):
            if bval not in P.regcache:
                P.regcache[bval] = e.to_reg(bval)
            d = dict(okw); d['bounds_check'] = P.regcache[bval]
            return d
    else:
        okw = dict(kw)
        def fix(e, okw=okw):
            return okw
    if kind == 'g':
        fn = lambda e: e.indirect_dma_start(out=out, out_offset=None, in_=in_, in_offset=bass.IndirectOffsetOnAxis(ap=idx_ap, axis=0), **fix(e))
    else:
        fn = lambda e: e.indirect_dma_start(out=out, out_offset=bass.IndirectOffsetOnAxis(ap=idx_ap, axis=0), in_=in_, in_offset=None, **fix(e))
    P.ops['pool'].append(dict(fn=fn, deps=[d for d in deps if d is not None], sig=False, dma=(slot, cnt)))
    return ('d', slot, cnt)

def _ind_old(P, kind, out, in_, idx_ap, deps, slot, **kw):
    cnt = 0
    if kind == 'g':
        fn = lambda e: e.indirect_dma_start(out=out, out_offset=None, in_=in_, in_offset=bass.IndirectOffsetOnAxis(ap=idx_ap, axis=0), **kw)
    else:
        fn = lambda e: e.indirect_dma_start(out=out, out_offset=bass.IndirectOffsetOnAxis(ap=idx_ap, axis=0), in_=in_, in_offset=None, **kw)
    P.ops['pool'].append(dict(fn=fn, deps=[d for d in deps if d is not None], sig=False, dma=(slot, cnt)))
    return ('d', slot, cnt)

def build_C3(NE=4, nsc=NSC):
    P = Prog(); nc = P.nc
    h_d = P.din('h2all', [8192, 2048], BF16); pm_d = P.din('pm', [128, 64, NE]); cnt_d = P.din('cnt', [128, 8, NE]); G_d = P.din('Grows', [8192, NE])
    tid_d = P.din('tid', [128, 64], I32); dum_d = P.din('dumrow', [128, 1]); id_d = P.din('ident', [128, 128], BF16)
    w1_d = P.din('w1', [NE, 2048, 4096]); w2_d = P.din('w2', [NE, 2048, 2048])
    b1g_d = P.din('b1g', [128, NE, 16]); b1l_d = P.din('b1l', [128, NE, 16]); b2_d = P.din('b2', [1, NE, 2048])
    y_o = P.dout('ypart', [8192 + 128, 2048])
    lists = [nc.dram_tensor("lists%d" % e, [CAPT, 1], I32, kind="Internal").ap() for e in range(NE)]

    pm = P.sb('pm', [128, 64, NE]); cnt = P.sb('cnt', [128, 8, NE]); start = P.sb('start', [128, 8, NE]); valid = P.sb('valid', [128, 64, NE])
    posg = P.sb('posg', [128, 64, NE]); inv = P.sb('inv', [128, 64, NE]); posi = P.sb('posi', [128, 64, NE], I32)
    tid = P.sb('tid', [128, 64], I32); dum = P.sb('dum', [128, 1]); ident = P.sb('ident', [128, 128], BF16)
    bigt = P.sb('bigt', [128, NBT], I32)
    idxt = P.sb('idxt', [128, NE, NBT], I32); idxf = P.sb('idxf', [128, NE, NBT]); v2 = P.sb('v2', [128, NE, NBT]); isc = P.sb('isc', [128, 4, NE, NBT], I32); iscf = P.sb('iscf', [128, NE, NBT])
    gsl = P.sb('gsl', [128, NBT, NE])
    b1g = P.sb('b1g', [128, NE, 16]); b1l = P.sb('b1l', [128, NE, 16]); b2 = P.sb('b2', [1, 2048]); ones1 = P.sb('ones1', [1, 128])
    XeT = P.sb('XeT', [128, 16, SCS], BF16); actT = P.sb('actT', [128, 16, SCS], BF16)
    Xs = [P.sb('Xs%d' % i, [128, 2048], BF16) for i in range(3)]
    w1b = [P.sb('w1b%d' % i, [128, 16, 256], BF16) for i in range(2)]
    w2b = [P.sb('w2b%d' % i, [128, 16, 512], BF16) for i in range(2)]
    Yst = [P.sb('Yst%d' % i, [128, 512]) for i in range(4)]
    gc = P.sb('gc', [128, 512]); sgm = P.sb('sgm', [128, 512]); lc = P.sb('lc', [128, 512])
    zer = Yst
    banks = [P.ps('b%d' % i) for i in range(6)]
    tbank = [P.ps('tb%d' % i, [128, 1024], BF16) for i in range(2)]
    bfree = [[] for _ in range(6)]; bptr = [0]
    def alloc():
        i = bptr[0] % 6; bptr[0] += 1
        return i, list(bfree[i])

    t_pm = P.dma('sp', pm[:], pm_d[:, :, :]); t_cnt = P.dma('sp', cnt[:], cnt_d[:, :, :]); t_tid = P.dma('sp', tid[:], tid_d[:, :])
    t_c = [P.dma('act', b1g[:], b1g_d[:, :, :]), P.dma('act', b1l[:], b1l_d[:, :, :]), P.dma('act', dum[:], dum_d[:, :]), P.dma('act', ident[:], id_d[:, :])]
    t_o1 = P.memset(ones1[:], 1.0)
    zt = [P.memset(Yst[i][:], 0.0, eng='pool') for i in range(4)]
    yv = y_o.rearrange("(b p) (c n) -> b p c n", p=128, n=512)
    t_zero = []
    for b in range(65):
        t_zero.append(P.dma('sp' if b % 2 == 0 else 'act', yv[b, :, :, :], Yst[0][:].rearrange("p (o n) -> p o n", o=1).broadcast(1, 4) if False else Yst[b % 4][:, None, :].to_broadcast([128, 4, 512]) if False else Yst[b % 4][:], deps=zt, slot='z%d' % (b % 4))) if False else None
    t_zero = []
    for b in range(65):
        for c4 in range(4):
            t_zero.append(P.dma('sp' if (b + c4) % 2 == 0 else 'act', y_o[b * 128:(b + 1) * 128, c4 * 512:(c4 + 1) * 512], Yst[c4][:], deps=zt, slot='z%d' % c4))
    t_zero_last = t_zero[-4:]
    w1v = w1_d.rearrange("e (kc p) n -> e p kc n", p=128); w2v = w2_d.rearrange("e (kc p) n -> e p kc n", p=128)
    w1_free = [None, None]; w2_free = [None, None]; w1_tok = {}; w2_tok = {}
    w1_seq = [(e, s, c) for e in range(NE) for s in range(nsc) for c in range(16)]
    w2_seq = [(e, s, c) for e in range(NE) for s in range(nsc) for c in range(4)]
    w1_i = [0]; w2_i = [0]
    def issue_w1():
        if w1_i[0] >= len(w1_seq): return
        k = w1_i[0]; e, s, c = w1_seq[k]
        w1_tok[(e, s, c)] = (P.dma('pool', w1b[k % 2][:], w1v[e, :, :, c * 256:(c + 1) * 256], deps=[w1_free[k % 2]], slot='w1_%d' % (k % 2)), k % 2)
        w1_i[0] += 1
    def issue_w2():
        if w2_i[0] >= len(w2_seq): return
        k = w2_i[0]; e, s, c = w2_seq[k]
        w2_tok[(e, s, c)] = (P.dma('pool', w2b[k % 2][:], w2v[e, :, :, c * 512:(c + 1) * 512], deps=[w2_free[k % 2]], slot='w2_%d' % (k % 2)), k % 2)
        w2_i[0] += 1
    a = P.memset(start[:, 0, :], 0.0, deps=[t_cnt])
    for s in range(1, 8):
        a = P.tt(start[:, s, :], start[:, s - 1, :], cnt[:, s - 1, :], ALU.add, deps=[a, t_cnt])
    t_start = a
    tv = P.ts(valid[:], pm[:], 0.0, None, ALU.is_ge, deps=[t_pm])
    last = tv
    for s in range(8):
        for e in range(NE):
            last = P.ts(posg[:, 8 * s:8 * s + 8, e], pm[:, 8 * s:8 * s + 8, e], start[:, s, e:e + 1], None, ALU.add, deps=[t_start, t_pm])
    a = P.tt(posg[:], posg[:], valid[:], ALU.mult, deps=[last, tv])
    b = P.ts(inv[:], valid[:], -1.0, -BIG, ALU.add, ALU.mult, deps=[tv])
    a = P.tt(posg[:], posg[:], inv[:], ALU.add, deps=[a, b])
    t_posi = P.cp(posi[:], posg[:], deps=[a])
    tb_ = P.memset(bigt[:], int(BIG))
    t_li = [P.dma('sp', lists[e].rearrange("(p b) o -> p (b o)", p=128), bigt[:], deps=[tb_]) for e in range(NE)]
    t_sc = []
    for e in range(NE):
        lastsc = None
        hist = []
        for G in range(64):
            lastsc = _ind(P, 's', lists[e], tid[:, G:G + 1], posi[:, G, e:e + 1], [t_posi, t_tid, t_li[e]] + ([hist[-8]] if len(hist) >= 8 else []), 'ls%d' % e, bounds_check=CAPT - 1, oob_is_err=False)
            hist.append(lastsc)
        t_sc.append(lastsc)
    t_idx = [P.dma('sp', idxt[:, e, :], lists[e].rearrange("(p b) o -> p (b o)", p=128), deps=[t_sc[e]]) for e in range(NE)]
    a = P.cp(idxf[:], idxt[:], deps=t_idx)
    a2 = P.ts(v2[:], idxf[:], 8192.0, None, ALU.is_lt, deps=[a])
    a3 = P.tt(idxf[:], idxf[:], v2[:], ALU.mult, deps=[a2])
    a4 = P.ts(v2[:], v2[:], -1.0, -1.0, ALU.add, ALU.mult, deps=[a3])
    a5 = P.ts(v2[:], v2[:], dum[:, 0:1], None, ALU.mult, deps=[a4, t_c[2]])
    a6 = P.tt(idxf[:], idxf[:], v2[:], ALU.add, deps=[a5])
    t_isc_l = []
    for c4 in range(4):
        a7 = P.ts(iscf[:], idxf[:], 4.0, float(c4), ALU.mult, ALU.add, deps=[a6] + t_isc_l)
        t_isc_l.append(P.cp(isc[:, c4, :, :], iscf[:], deps=[a7]))
    t_isc = t_isc_l[-1]
    y4 = y_o.rearrange("t (c n) -> (t c) n", n=512)
    issue_w1(); issue_w1(); issue_w2(); issue_w2()

    Xs_free = [[] for _ in range(3)]; Yst_free = [list(t_zero) if False else [] for _ in range(4)]
    XeT_free = []; actT_free = []; tmp_free = []; b2_free = []; gsl_free = []
    prev_scatter = list(t_zero_last) + t_zero
    tfree = [[], []]
    for e in range(NE):
        t_b2 = P.dma('sp', b2[:], b2_d[:, e, :], deps=b2_free, slot='b2')
        exp_scatter = []
        for sc in range(nsc):
            blk0 = sc * NBS
            t_g = None
            for bl in range(NBS):
                t_g = _ind(P, 'g', gsl[:, blk0 + bl, :], G_d[:, :], idxt[:, e, blk0 + bl:blk0 + bl + 1], [t_idx[e]] + gsl_free, 'gg', bounds_check=8191, oob_is_err=False)
            gsl_free = []
            xe_t = [[None] * NBS for _ in range(16)]
            lasttr = None
            for bl in range(NBS):
                r = bl % 3
                tg = _ind(P, 'g', Xs[r][:], h_d[:, :], idxt[:, e, blk0 + bl:blk0 + bl + 1], [t_idx[e]] + Xs_free[r], 'xg%d' % r, bounds_check=8191, oob_is_err=False)
                for q in range(4):
                    ti = (bl * 4 + q) % 2
                    for k4 in range(4):
                        kc = q * 4 + k4
                        lasttr = P.tr(tbank[ti][:, k4 * 128:(k4 + 1) * 128], Xs[r][:, kc * 128:(kc + 1) * 128], ident[:], deps=[tg, t_c[3]] + (tfree[ti] if k4 == 0 else []))
                    ev = P.cp(XeT[:, q * 4:(q + 1) * 4, bl * 128:(bl + 1) * 128], tbank[ti][:, 0:512].rearrange("p (k n) -> p k n", k=4), deps=[lasttr] + XeT_free,
                              eng=('act' if q % 2 == 0 else 'dve'))
                    tfree[ti] = [ev]
                    for k4 in range(4):
                        xe_t[q * 4 + k4][bl] = ev
                Xs_free[r] = [lasttr]
            XeT_free = []
            act_t = [[None] * len(SUBS) for _ in range(16)]
            lastw1 = None
            for ic in range(16):
                tw, wi = w1_tok[(e, sc, ic)]
                for si, (s0, sn) in enumerate(SUBS):
                    bG, dG = alloc(); bL, dL = alloc()
                    xdeps = [xe_t[kc_][bb] for kc_ in range(16) for bb in range(s0 // 128, (s0 + sn) // 128)]
                    for kc in range(16):
                        P.mm(banks[bG][:, 0:sn], w1b[wi][:, kc, 0:256:2], XeT[:, kc, s0:s0 + sn], start=(kc == 0), stop=(kc == 15), deps=(dG + [tw] + xdeps if kc == 0 else []))
                    mg = ('c', 'pe', len(P.ops['pe']) - 1)
                    for kc in range(16):
                        lastw1 = P.mm(banks[bL][:, 0:sn], w1b[wi][:, kc, 1:256:2], XeT[:, kc, s0:s0 + sn], start=(kc == 0), stop=(kc == 15), deps=(dL if kc == 0 else []))
                    e1 = P.ts(gc[:, 0:sn], banks[bG][:, 0:sn], b1g[:, e, ic:ic + 1], 7.0, ALU.add, ALU.min, deps=[mg] + tmp_free + t_c)
                    e2 = P.ts(lc[:, 0:sn], banks[bL][:, 0:sn], b1l[:, e, ic:ic + 1], 7.0, ALU.add, ALU.min, deps=[lastw1] + tmp_free)
                    bfree[bG] = [e1]; bfree[bL] = [e2]
                    e3 = P.act(sgm[:, 0:sn], gc[:, 0:sn], AF.Sigmoid, scale=1.702, deps=[e1] + tmp_free)
                    e4 = P.ts(lc[:, 0:sn], lc[:, 0:sn], -7.0, 1.0, ALU.max, ALU.add, deps=[e2], eng='pool')
                    e5 = P.tt(gc[:, 0:sn], gc[:, 0:sn], sgm[:, 0:sn], ALU.mult, deps=[e3, e1], eng='pool')
                    e6 = P.tt(actT[:, ic, s0:s0 + sn], gc[:, 0:sn], lc[:, 0:sn], ALU.mult, deps=[e5, e4] + actT_free, eng='pool')
                    tmp_free = [e6]
                    act_t[ic][si] = e6
                w1_free[wi] = lastw1
                issue_w1()
            actT_free = []
            XeT_free = [lastw1]
            lastw2 = None
            yk = 0
            for dmc in range(4):
                tw, wi = w2_tok[(e, sc, dmc)]
                for bl in range(NBS):
                    si = [i for i, (s0, sn) in enumerate(SUBS) if s0 <= bl * 128 < s0 + sn][0]
                    bk, dps = alloc()
                    for ic in range(16):
                        P.mm(banks[bk][:, 0:512], actT[:, ic, bl * 128:(bl + 1) * 128], w2b[wi][:, ic, :], start=(ic == 0), stop=False,
                             deps=(dps + [tw] if ic == 0 else []) + [act_t[ic][si]])
                    lastw2 = P.mm(banks[bk][:, 0:512], ones1[:, :], b2[:, dmc * 512:(dmc + 1) * 512], start=False, stop=True, deps=[t_o1, t_b2])
                    r = yk % 4; yk += 1
                    ev = P.act(Yst[r][:], banks[bk][:, 0:512], AF.Copy, scale=gsl[:, blk0 + bl, e:e + 1], deps=[lastw2, t_g] + Yst_free[r] + zt)
                    bfree[bk] = [ev]
                    sct = _ind(P, 's', y4, Yst[r][:], isc[:, dmc, e, blk0 + bl:blk0 + bl + 1], [ev, t_isc] + prev_scatter, 'ys%d' % r, compute_op=ALU.add)
                    Yst_free[r] = [sct]
                    exp_scatter.append(sct)
                w2_free[wi] = lastw2
                issue_w2()
            actT_free = [lastw2]
            gsl_free = [lastw2]
            b2_free = [lastw2]
            prev_scatter = prev_scatter if sc < nsc - 1 else []
        prev_scatter = exp_scatter[-4:] + [t for t in exp_scatter]
    for t in prev_scatter:
        P.out_toks.append(t)
    return P

def prep_C3(core, h2all, pm_all, cnt_all, G_all, w1_l, b1_l, w2_l, b2_l, NE=4):
    e0 = core * NE
    pm = np.concatenate([pm_all[s][:, :, e0:e0 + NE] for s in range(8)], axis=1)
    cnt = np.stack([np.tile(cnt_all[s][None, e0:e0 + NE], (128, 1)) for s in range(8)], axis=1)
    Grows = np.concatenate([G_all[s][:, :, e0:e0 + NE].transpose(1, 0, 2).reshape(1024, NE) for s in range(8)], axis=0)
    tid = (np.arange(64, dtype=np.int32)[None, :] * 128 + np.arange(128, dtype=np.int32)[:, None]).astype(np.int32)
    b1 = b1_l[e0:e0 + NE]
    b1g = b1[:, 0::2].reshape(NE, 16, 128).transpose(2, 0, 1); b1l = b1[:, 1::2].reshape(NE, 16, 128).transpose(2, 0, 1)
    return dict(h2all=h2all, pm=np.ascontiguousarray(pm), cnt=np.ascontiguousarray(cnt.astype(np.float32)), Grows=np.ascontiguousarray(Grows),
                tid=tid, dumrow=(8192 + np.arange(128, dtype=np.float32))[:, None], ident=np.eye(128, dtype=np.float32).astype(ml_dtypes.bfloat16),
                w1=np.ascontiguousarray(w1_l[e0:e0 + NE]), w2=np.ascontiguousarray(w2_l[e0:e0 + NE]),
                b1g=np.ascontiguousarray(b1g), b1l=np.ascontiguousarray(b1l), b2=np.ascontiguousarray(b2_l[None, e0:e0 + NE]))


def build_D(final=False):
    P = Prog()
    x_d = P.din('x1T', [2048, 1024]); y_d = P.din('yparts', [8, 2048, 1024]); modv_d = P.din('modv', [128, 6, 16]); fg_d = P.din('fg', [128, 16])
    o_d = P.dout('x2T', [2048, 1024])
    x = P.sb('x', [128, 16, 1024]); mods = P.sb('mods', [128, 6, 16]); fg = P.sb('fg', [128, 16])
    yb = [[P.sb('yb%d_%d' % (i, c), [128, 1024]) for c in range(8)] for i in range(2)]
    sq = [P.sb('sq%d' % i, [128, 1024], BF16) for i in range(2)]; ones = P.sb('ones', [128, 128], BF16); rstd = P.sb('rstd', [128, 1024])
    ps = [P.ps('b%d' % i) for i in range(2)]
    t_m = P.dma('act', mods[:], modv_d[:, :, :]); t_fg = P.dma('act', fg[:], fg_d[:, :])
    t_x = [P.dma('act', x[:, kc * 4:(kc + 1) * 4, :], x_d.rearrange("(kc p) t -> p kc t", p=128)[:, kc * 4:(kc + 1) * 4, :]) for kc in range(4)]
    t_ones = P.memset(ones[:], 1.0)
    yfree = [[None] * 8, [None] * 8]
    sq_free = [None, None]
    t_x2 = []
    lastN = [None, None]
    for kc in range(16):
        i = kc % 2
        lt = [P.dma('sp', yb[i][c][:], y_d[c, kc * 128:(kc + 1) * 128, :], deps=[yfree[i][c]], slot='y%d_%d' % (i, c)) for c in range(8)]
        a01 = P.tt(yb[i][0][:], yb[i][0][:], yb[i][1][:], ALU.add, deps=[lt[0], lt[1]])
        a23 = P.tt(yb[i][2][:], yb[i][2][:], yb[i][3][:], ALU.add, deps=[lt[2], lt[3]], eng='pool')
        a45 = P.tt(yb[i][4][:], yb[i][4][:], yb[i][5][:], ALU.add, deps=[lt[4], lt[5]])
        a67 = P.tt(yb[i][6][:], yb[i][6][:], yb[i][7][:], ALU.add, deps=[lt[6], lt[7]], eng='pool')
        b0 = P.tt(yb[i][0][:], yb[i][0][:], yb[i][2][:], ALU.add, deps=[a01, a23])
        b1 = P.tt(yb[i][4][:], yb[i][4][:], yb[i][6][:], ALU.add, deps=[a45, a67], eng='pool')
        c0 = P.tt(yb[i][0][:], yb[i][0][:], yb[i][4][:], ALU.add, deps=[b0, b1])
        xx = P.stt(x[:, kc, :], yb[i][0][:], mods[:, 5, kc:kc + 1], x[:, kc, :], ALU.mult, ALU.add, deps=[c0, t_m, t_x[kc // 4]])
        for c in range(8):
            yfree[i][c] = xx
        t_x2.append(xx)
        if final:
            s_ = P.act(sq[i][:], x[:, kc, :], AF.Square, deps=[xx, sq_free[i]])
            for hf in range(2):
                lastN[hf] = P.mm(ps[hf][:], ones[:], sq[i][:, hf * 512:(hf + 1) * 512], start=(kc == 0), stop=(kc == 15), deps=[s_, t_ones])
            sq_free[i] = lastN[1]
    if final:
        t_r = []
        for hf in range(2):
            cs = slice(hf * 512, (hf + 1) * 512)
            a = P.act(rstd[:, cs], ps[hf][:], AF.Sqrt, bias=1e-6, scale=1.0 / 2048, deps=[lastN[hf]])
            t_r.append(P.recip(rstd[:, cs], rstd[:, cs], deps=[a]))
        t_f = []
        for kc in range(16):
            t_f.append(P.stt(x[:, kc, :], x[:, kc, :], fg[:, kc:kc + 1], rstd[:], ALU.mult, ALU.mult, deps=t_r + [t_fg, t_x2[kc]]))
        t_x2 = t_f
    for kc in range(4):
        P.dma('sp', o_d.rearrange("(kc p) t -> p kc t", p=128)[:, kc * 4:(kc + 1) * 4, :], x[:, kc * 4:(kc + 1) * 4, :], deps=t_x2[kc * 4:(kc + 1) * 4], is_out=True)
    return P


def _np(a):
    return np.asarray(a)

def _to_bf16(a):
    a = np.asarray(a)
    if a.dtype == ml_dtypes.bfloat16:
        return a
    if a.dtype.itemsize == 2:
        return a.view(ml_dtypes.bfloat16)
    raise ValueError('unexpected dtype %s' % a.dtype)

def kernel(x, c, positions, ada_w, ada_b, norm1_g, norm2_g, w_in, conv_w, conv_b, lru_wa, lru_ba, lru_wx, lru_bx,
           lru_lambda, attn_out_g, lru_out_g, w_out, router_w, router_b, w1, b1, w2, b2, final_g):
    x = _np(x); c = _np(c); positions = _np(positions); ada_w = _np(ada_w); ada_b = _np(ada_b)
    norm1_g = _np(norm1_g); norm2_g = _np(norm2_g); w_in = _np(w_in); conv_w = _np(conv_w); conv_b = _np(conv_b)
    lru_wa = _np(lru_wa); lru_ba = _np(lru_ba); lru_wx = _np(lru_wx); lru_bx = _np(lru_bx); lru_lambda = _np(lru_lambda)
    attn_out_g = _np(attn_out_g); lru_out_g = _np(lru_out_g); w_out = _np(w_out); router_w = _np(router_w); router_b = _np(router_b)
    w1 = _np(w1); b1 = _np(b1); w2 = _np(w2); b2 = _np(b2); final_g = _np(final_g)
    NC = 8
    ims = []
    for core in range(NC):
        l = core // 4; cs = (core % 4) * 3072
        ims.append({'c': np.ascontiguousarray(c.reshape(16, 128).T), 'w': np.ascontiguousarray(ada_w[l][:, cs:cs + 3072]),
                    'b': np.ascontiguousarray(ada_b[l][None, cs:cs + 3072])})
    res = build_M().run(ims)
    mod = np.concatenate([np.asarray(r['mod'])[0] for r in res.results]).reshape(2, 12288)
    xT = [np.ascontiguousarray(x[0, k * 1024:(k + 1) * 1024, :].T) for k in range(NC)]
    fgl = np.ascontiguousarray(final_g.reshape(16, 128).T)
    for l in range(2):
        modv = np.ascontiguousarray(mod[l].reshape(6, 16, 128).transpose(2, 0, 1))
        w_in_l = np.ascontiguousarray(w_in[l])
        cA = consts_A()
        ims = []
        for k in range(NC):
            d = dict(xT=xT[k], modv=modv, gn=np.ascontiguousarray(norm1_g[l].reshape(16, 128).T), w=w_in_l,
                     pos=np.ascontiguousarray(positions[:, k * 1024:(k + 1) * 1024]))
            d.update(cA)
            ims.append(d)
        rA = build_A().run(ims).results
        qT = np.concatenate([_to_bf16(r['qT']) for r in rA], axis=1)
        kT = np.concatenate([_to_bf16(r['kT']) for r in rA], axis=1)
        vT = np.concatenate([_to_bf16(r['vT']) for r in rA], axis=1)
        xrT = np.concatenate([np.asarray(r['xrT']) for r in rA], axis=1)
        grT = [np.asarray(r['grT']) for r in rA]
        ims = []
        for k in range(NC):
            m = prep_B_attn(k, qT, kT, vT)
            m.update(prep_B_lru(k, xrT, conv_w[l], conv_b[l], lru_wa[l], lru_ba[l], lru_wx[l], lru_bx[l], lru_lambda[l], positions))
            ims.append(m)
        rB = build_B(True).run(ims).results
        hloc = [np.asarray(r['hloc']) for r in rB]; pprod = [np.asarray(r['pprod']) for r in rB]
        wo_l = np.ascontiguousarray(w_out[l])
        ims = [prep_C1(k, xT[k], np.asarray(rB[k]['attnT']), hloc, pprod, grT[k], mod[l], norm2_g[l], attn_out_g[l], lru_out_g[l],
                       wo_l, router_w[l], router_b[l]) for k in range(NC)]
        rC = build_C1().run(ims).results
        x1T = [np.asarray(r['x1T']) for r in rC]
        h2all = np.ascontiguousarray(np.concatenate([_to_bf16(r['h2T']).T for r in rC], axis=0))
        pm = [np.asarray(r['pm']) for r in rC]; G = [np.asarray(r['G']) for r in rC]
        cnt_all = [np.asarray(r['cnt'])[0] for r in rC]
        ims = [prep_C3(k, h2all, pm, cnt_all, G, w1[l], b1[l], w2[l], b2[l]) for k in range(NC)]
        rE = build_C3().run(ims).results
        yp = [np.asarray(r['ypart']) for r in rE]
        final = (l == 1)
        ims = [dict(x1T=x1T[k], yparts=np.ascontiguousarray(np.stack([yp[cc][k * 1024:(k + 1) * 1024].T for cc in range(NC)])), modv=modv, fg=fgl)
               for k in range(NC)]
        rD = build_D(final=final).run(ims).results
        xT = [np.asarray(r['x2T']) for r in rD]
    out = np.concatenate([t.T for t in xT], axis=0)[None].astype(np.float32)
    return out
```

```python
import contextlib
import numpy as np
import concourse.bass as bass
import concourse.mybir as mybir
from concourse.bass_utils import run_bass_kernel_spmd

F32 = mybir.dt.float32
BF16 = mybir.dt.bfloat16
I32 = mybir.dt.int32
AF = mybir.ActivationFunctionType
ALU = mybir.AluOpType


class Prog:
    ENG = ['pe', 'act', 'dve', 'pool', 'sp']

    def __init__(self):
        self.nc = bass.Bass("TRN2", target_bir_lowering=False)
        self.es = contextlib.ExitStack()
        self.ops = {e: [] for e in self.ENG}
        self.dma_slots = {}
        self.out_toks = []

    def din(self, name, shape, dt=F32):
        return self.nc.dram_tensor(name, list(shape), dt, kind="ExternalInput").ap()

    def dout(self, name, shape, dt=F32):
        return self.nc.dram_tensor(name, list(shape), dt, kind="ExternalOutput").ap()

    def sb(self, name, shape, dt=F32):
        return self.es.enter_context(self.nc.sbuf_tensor('sb_' + name, list(shape), dt))

    def ps(self, name, shape=(128, 512), dt=F32):
        return self.es.enter_context(self.nc.psum_tensor('ps_' + name, list(shape), dt))

    def op(self, eng, fn, deps=()):
        o = dict(fn=fn, deps=[d for d in deps if d is not None], sig=False, dma=None)
        self.ops[eng].append(o)
        return ('c', eng, len(self.ops[eng]) - 1)

    def dma(self, eng, out, in_, deps=(), slot=None, is_out=False, **kw):
        if slot is None:
            slot = '_a%d' % len(self.dma_slots)
        cnt = self.dma_slots.get(slot, 0) + 1
        self.dma_slots[slot] = cnt
        o = dict(fn=lambda e: e.dma_start(out=out, in_=in_, **kw),
                 deps=[d for d in deps if d is not None], sig=False, dma=(slot, cnt))
        self.ops[eng].append(o)
        tok = ('d', slot, cnt)
        if is_out:
            self.out_toks.append(tok)
        return tok

    def mm(self, out, lhsT, rhs, start=True, stop=True, deps=()):
        return self.op('pe', lambda e: e.matmul(out, lhsT, rhs, start=start, stop=stop), deps)

    def tr(self, out, in_, ident, deps=()):
        return self.op('pe', lambda e: e.transpose(out, in_, ident), deps)

    def act(self, out, in_, func, bias=None, scale=None, deps=(), accum_out=None):
        kw = {}
        if bias is not None:
            kw['bias'] = bias
        if scale is not None:
            kw['scale'] = scale
        if accum_out is not None:
            kw['accum_out'] = accum_out
        return self.op('act', lambda e: e.activation(out, in_, func, **kw), deps)

    def tt(self, out, in0, in1, op, deps=(), eng='dve'):
        return self.op(eng, lambda e: e.tensor_tensor(out, in0, in1, op), deps)

    def ts(self, out, in0, s1, s2, op0, op1=None, deps=(), eng='dve'):
        if op1 is None:
            return self.op(eng, lambda e: e.tensor_scalar(out, in0, s1, None, op0), deps)
        return self.op(eng, lambda e: e.tensor_scalar(out, in0, s1, s2, op0, op1), deps)

    def stt(self, out, in0, scalar, in1, op0, op1, deps=()):
        return self.op('dve', lambda e: e.scalar_tensor_tensor(out, in0, scalar, in1, op0, op1), deps)

    def cp(self, out, in_, deps=(), eng='dve'):
        if eng == 'act':
            return self.op('act', lambda e: e.copy(out, in_), deps)
        return self.op(eng, lambda e: e.tensor_copy(out, in_), deps)

    def recip(self, out, in_, deps=()):
        return self.op('dve', lambda e: e.reciprocal(out, in_), deps)

    def memset(self, ap, val, deps=(), eng='dve'):
        return self.op(eng, lambda e: e.memset(ap, val), deps)

    def build(self):
        nc = self.nc
        for eng in self.ENG:
            for o in self.ops[eng]:
                for d in o['deps']:
                    if d[0] == 'c':
                        self.ops[d[1]][d[2]]['sig'] = True
        for eng in self.ENG:
            c = 0
            for o in self.ops[eng]:
                if o['sig'] and o['dma'] is None:
                    c += 1
                    o['cnt'] = c
        if self.out_toks:
            self.ops['sp'].append(dict(fn=None, deps=list(self.out_toks), sig=False, dma=None))
        self.check_deadlock()
        self.sem = {e: self.es.enter_context(nc.semaphore('s_' + e)) for e in self.ENG}
        self.dsem = {s: self.es.enter_context(nc.semaphore('d_' + s)) for s in self.dma_slots}
        block = self.es.enter_context(nc.Block())

        def replay(name, e):
            waited = {}
            for o in self.ops[name]:
                for d in o['deps']:
                    if d[0] == 'c':
                        key = ('c', d[1]); sem = self.sem[d[1]]; val = self.ops[d[1]][d[2]]['cnt']
                    else:
                        key = ('d', d[1]); sem = self.dsem[d[1]]; val = 16 * d[2]
                    if waited.get(key, 0) < val:
                        e.wait_ge(sem, val)
                        waited[key] = val
                if o['fn'] is None:
                    continue
                try:
                    ins = o['fn'](e)
                except Exception:
                    print('EMIT FAIL engine', name, 'op index', self.ops[name].index(o), 'dma', o['dma'])
                    raise
                if o['dma'] is not None:
                    ins.then_inc(self.dsem[o['dma'][0]], 16)
                elif o['sig']:
                    ins.then_inc(self.sem[name], 1)

        @block.tensor
        def _(e):
            replay('pe', e)

        @block.scalar
        def _(e):
            replay('act', e)

        @block.vector
        def _(e):
            replay('dve', e)

        @block.gpsimd
        def _(e):
            replay('pool', e)

        @block.sync
        def _(e):
            replay('sp', e)

        self.es.close()
        return nc

    def check_deadlock(self):
        ptr = {e: 0 for e in self.ENG}
        done_c = {e: -1 for e in self.ENG}
        dcnt = {s: 0 for s in self.dma_slots}
        total = sum(len(v) for v in self.ops.values())
        ndone = 0
        while ndone < total:
            prog = False
            for e in self.ENG:
                while ptr[e] < len(self.ops[e]):
                    o = self.ops[e][ptr[e]]
                    ok = True
                    for d in o['deps']:
                        if d[0] == 'c':
                            if done_c[d[1]] < d[2]:
                                ok = False; break
                        else:
                            if dcnt[d[1]] < d[2]:
                                ok = False; break
                    if not ok:
                        break
                    if o['dma'] is not None:
                        dcnt[o['dma'][0]] += 1
                    done_c[e] = ptr[e]
                    ptr[e] += 1; ndone += 1; prog = True
            if not prog:
                msg = []
                for e in self.ENG:
                    if ptr[e] < len(self.ops[e]):
                        o = self.ops[e][ptr[e]]
                        msg.append('%s blocked at op %d deps=%s' % (e, ptr[e], o['deps']))
                raise RuntimeError('DEADLOCK: ' + ' | '.join(msg))

    def run(self, in_maps, trace=False):
        nc = self.build()
        n = len(in_maps)
        res = run_bass_kernel_spmd(nc, in_maps, core_ids=list(range(n)), trace=trace)
        return res

import math
import ml_dtypes


def build_M():
    P = Prog()
    c_in = P.din('c', [128, 16])
    w_in = P.din('w', [2048, 3072])
    b_in = P.din('b', [1, 3072])
    o = P.dout('mod', [1, 3072])
    cs = P.sb('cs', [128, 16]); cond = P.sb('cond', [128, 16])
    wb = [P.sb('wb%d' % i, [128, 16, 512]) for i in range(2)]
    bb = P.sb('bb', [1, 3072]); ob = P.sb('ob', [1, 3072])
    ps = [P.ps('ps%d' % i) for i in range(2)]
    t_c = P.dma('sp', cs[:], c_in[:, :])
    t_b = P.dma('sp', bb[:], b_in[:, :])
    t_cond = P.act(cond[:], cs[:], AF.Silu, deps=[t_c])
    wv = w_in.rearrange("(kc p) n -> p kc n", p=128)
    ev = [None, None]
    outs = []
    for j in range(6):
        t_w = P.dma('sp' if j % 2 == 0 else 'act', wb[j % 2][:], wv[:, :, j * 512:(j + 1) * 512], deps=[ev[j % 2]], slot='w%d' % (j % 2))
        last = None
        for kc in range(16):
            last = P.mm(ps[j % 2][0:1, :], cond[:, kc:kc + 1], wb[j % 2][:, kc, :], start=(kc == 0), stop=(kc == 15),
                        deps=[t_w, t_cond, ev[j % 2]] if kc == 0 else [])
        ev[j % 2] = P.tt(ob[:, j * 512:(j + 1) * 512], ps[j % 2][0:1, :], bb[:, j * 512:(j + 1) * 512], ALU.add, deps=[last, t_b])
        outs.append(ev[j % 2])
    P.dma('sp', o[:, :], ob[:], deps=outs, is_out=True)
    return P


TWO_PI = 2 * math.pi
MAGIC = 12582912.0

def consts_A():
    invf = (10000.0 ** (-np.arange(0, 128, 2, dtype=np.float32) / 128)).astype(np.float32)
    invf = np.concatenate([invf, invf])[None, :].astype(np.float32)
    sign = np.concatenate([-np.ones(64), np.ones(64)]).astype(np.float32)[:, None]
    swap = np.zeros((128, 128), np.float32)
    for e in range(128):
        swap[(e + 64) % 128, e] = 1.0
    return dict(invf=invf, sign=sign, swap=swap)

def build_A(cc_list=None):
    P = Prog()
    xT = P.din('xT', [2048, 1024]); modv = P.din('modv', [128, 6, 16]); g1n = P.din('gn', [128, 16])
    w = P.din('w', [2048, 7680]); pos = P.din('pos', [1, 1024], I32)
    invf_d = P.din('invf', [1, 128]); sign_d = P.din('sign', [128, 1]); swap_d = P.din('swap', [128, 128])
    qT = P.dout('qT', [1536, 1024], BF16); kT = P.dout('kT', [1536, 1024], BF16); vT = P.dout('vT', [1536, 1024], BF16)
    xrT = P.dout('xrT', [1536, 1024]); grT = P.dout('grT', [1536, 1024])

    x = P.sb('x', [128, 16, 1024]); h = P.sb('h', [128, 16, 1024], BF16); sq = h
    mods = P.sb('mods', [128, 6, 16]); gn = P.sb('gns', [128, 16]); G = P.sb('G', [128, 16])
    ones = P.sb('ones', [128, 128], BF16); swp = P.sb('swp', [128, 128], BF16)
    invf = P.sb('invfs', [1, 128]); sign = P.sb('signs', [128, 1]); posi = P.sb('posi', [1, 1024], I32); posf = P.sb('posf', [1, 1024])
    rstd = P.sb('rstd', [128, 1024]); tmp = [P.sb('tmp%d' % i, [128, 1024]) for i in range(2)]
    cosT = P.sb('cosT', [128, 1024]); sinT = P.sb('sinT', [128, 1024])
    r1 = tmp[0]; r2 = tmp[1]
    wb = [P.sb('wb%d' % i, [128, 16, 512], BF16) for i in range(2)]
    tb = [P.sb('tb%d' % i, [128, 512], BF16) for i in range(2)]
    ta = [P.sb('ta%d' % i, [128, 512]) for i in range(2)]
    tb2 = [P.sb('tbb%d' % i, [128, 512]) for i in range(2)]
    ost = [P.sb('ost%d' % i, [128, 1024], BF16) for i in range(2)]
    ost32 = [P.sb('ost32_%d' % i, [128, 1024]) for i in range(2)]
    pm = [P.ps('pm%d' % i) for i in range(4)]
    pr = [P.ps('pr%d' % i) for i in range(2)]
    pz = [P.ps('pz%d' % i) for i in range(2)]

    t_x = [P.dma('sp', x[:, kc * 4:(kc + 1) * 4, :], xT.rearrange("(kc p) t -> p kc t", p=128)[:, kc * 4:(kc + 1) * 4, :]) for kc in range(4)]
    t_m = P.dma('act', mods[:], modv[:, :, :]); t_g = P.dma('act', gn[:], g1n[:, :])
    t_if = P.dma('act', invf[:], invf_d[:, :]); t_sg = P.dma('act', sign[:], sign_d[:, :])
    t_pos = P.dma('act', posi[:], pos[:, :])
    t_sw = P.dma('pool', swp[:], swap_d[:, :])
    t_ones = P.memset(ones[:], 1.0)
    wv = w.rearrange("(kc p) n -> p kc n", p=128)
    NWB = 15
    t_w = [None] * NWB
    wfree = [None, None]
    def load_w(j, deps):
        t_w[j] = P.dma('pool', wb[j % 2][:], wv[:, :, j * 512:(j + 1) * 512], deps=deps, slot='w%d' % (j % 2))
    if cc_list is None:
        load_w(0, []); load_w(1, [])
    else:
        for jj in sorted(set(c // 4 for c in cc_list)):
            load_w(jj, [wfree[jj % 2]])
    t_G = P.stt(G[:], mods[:, 1, :], 1.0, gn[:], ALU.add, ALU.mult, deps=[t_m, t_g])
    t_pf = P.cp(posf[:], posi[:], deps=[t_pos])
    t_ang = []
    for hf in range(2):
        t_ang.append(P.mm(pz[hf][:], invf[:], posf[:, hf * 512:(hf + 1) * 512], deps=[t_if, t_pf]))
    def reduce_sin(dst, off, deps_extra):
        toks = []
        for hf in range(2):
            sl = slice(hf * 512, (hf + 1) * 512)
            a0 = P.ts(r1[:, sl], pz[hf][:], 1.0 / TWO_PI, off / TWO_PI, ALU.mult, ALU.add, deps=[t_ang[hf]] + deps_extra)
            a = P.ts(r1[:, sl], r1[:, sl], MAGIC, None, ALU.add, deps=[a0])
            b = P.ts(r1[:, sl], r1[:, sl], MAGIC, None, ALU.subtract, deps=[a])
            c = P.stt(r2[:, sl], r1[:, sl], -TWO_PI, pz[hf][:], ALU.mult, ALU.add, deps=[b])
            d = P.ts(r2[:, sl], r2[:, sl], off + math.pi, TWO_PI - 1e-5, ALU.add, ALU.min, deps=[c])
            e = P.ts(r2[:, sl], r2[:, sl], 1e-5, -math.pi, ALU.max, ALU.add, deps=[d])
            toks.append(e)
        return toks
    tk = reduce_sin(sinT, 0.0, [])
    t_sin = P.act(sinT[:], r2[:], AF.Sin, scale=sign[:], deps=tk + [t_sg])
    tk = reduce_sin(cosT, math.pi / 2, [t_sin])
    t_cos = P.act(cosT[:], r2[:], AF.Sin, deps=tk)
    t_sq = [P.act(sq[:, kc, :], x[:, kc, :], AF.Square, deps=[t_x[kc // 4]]) for kc in range(16)]
    t_ss = []
    for hf in range(2):
        last = None
        for kc in range(16):
            last = P.mm(pm[hf][:], ones[:], sq[:, kc, hf * 512:(hf + 1) * 512], start=(kc == 0), stop=(kc == 15), deps=[t_sq[kc], t_ones])
        t_ss.append(last)
    t_rs = []
    for hf in range(2):
        sl = slice(hf * 512, (hf + 1) * 512)
        a = P.act(rstd[:, sl], pm[hf][:], AF.Sqrt, bias=1e-6, scale=1.0 / 2048, deps=[t_ss[hf]])
        t_rs.append(P.recip(rstd[:, sl], rstd[:, sl], deps=[a]))
    t_h = []
    tfree = [t_cos, t_cos]
    for kc in range(16):
        a = P.tt(tmp[kc % 2][:], x[:, kc, :], rstd[:], ALU.mult, deps=t_rs + [t_x[kc // 4], tfree[kc % 2]])
        b = P.act(h[:, kc, :], tmp[kc % 2][:], AF.Identity, bias=mods[:, 0, kc:kc + 1], scale=G[:, kc:kc + 1], deps=[a, t_G])
        tfree[kc % 2] = b
        t_h.append(b)
    bank_free = [[t_rs[0]], [t_rs[1]], [], []]
    tb_free = [None, None]; pr_free = [None, None]; ta_free = [None, None]
    ost_free = [None, None]; ost32_free = [None, None]
    pending = None
    u = 0
    outs = [qT, kT, vT, xrT, grT]
    for cc in (cc_list if cc_list is not None else range(60)):
        j = cc // 4
        sec = cc // 12; row0 = (cc % 12) * 128
        half_toks = []
        for hf in range(2):
            sl = slice(hf * 512, (hf + 1) * 512)
            mb = u % 4
            last = None
            for kc in range(16):
                deps = [t_h[kc]]
                if kc == 0:
                    deps += [t_w[j]] + bank_free[mb]
                last = P.mm(pm[mb][:], wb[j % 2][:, kc, (cc % 4) * 128:(cc % 4 + 1) * 128], h[:, kc, sl], start=(kc == 0), stop=(kc == 15), deps=deps)
            if cc % 4 == 3 and hf == 1:
                if j + 2 < NWB and cc_list is None:
                    load_w(j + 2, [last])
            if sec < 2:
                i2 = u % 2
                e1 = P.cp(tb[i2][:], pm[mb][:], deps=[last, tb_free[i2]], eng='act')
                e3 = P.tt(ta[i2][:], pm[mb][:], cosT[:, sl], ALU.mult, deps=[last, t_cos, ta_free[i2], e1])
                bank_free[mb] = [e1, e3]
                if pending is not None:
                    pending()
                def mk(i2=i2, e1=e1, e3=e3, sl=sl, slot=cc % 2, hf=hf):
                    def f():
                        e2 = P.mm(pr[i2][:], swp[:], tb[i2][:], deps=[e1, t_sw, pr_free[i2]])
                        tb_free[i2] = e2
                        e4 = P.tt(tb2[i2][:], pr[i2][:], sinT[:, sl], ALU.mult, deps=[e2, t_sin])
                        pr_free[i2] = e4
                        e5 = P.tt(ost[slot][:, sl], ta[i2][:], tb2[i2][:], ALU.add, deps=[e3, e4, ost_free[slot]])
                        ta_free[i2] = e5
                        return e5
                    return f
                g = mk()
                res = {}
                def pend(g=g, res=res):
                    res['t'] = g()
                pending = pend
                half_toks.append(res)
            else:
                if pending is not None:
                    pending(); pending = None
                if sec == 2:
                    slot = cc % 2
                    e = P.cp(ost[slot][:, sl], pm[mb][:], deps=[last, ost_free[slot]], eng='act')
                else:
                    slot = cc % 2
                    e = P.cp(ost32[slot][:, sl], pm[mb][:], deps=[last, ost32_free[slot]], eng=('act' if hf == 0 else 'dve'))
                bank_free[mb] = [e]
                half_toks.append({'t': e})
            u += 1
        def mkout(cc=cc, sec=sec, row0=row0, half_toks=half_toks):
            def f():
                slot = cc % 2
                deps = [r['t'] for r in half_toks]
                if sec < 3:
                    t = P.dma('sp', outs[sec][row0:row0 + 128, :], ost[slot][:], deps=deps, slot='o%d' % slot, is_out=True)
                    ost_free[slot] = t
                else:
                    t = P.dma('sp', outs[sec][row0:row0 + 128, :], ost32[slot][:], deps=deps, slot='o32_%d' % slot, is_out=True)
                    ost32_free[slot] = t
            return f
        if sec < 2:
            prev_pending = pending
            outf = mkout()
            def pend2(prev_pending=prev_pending, outf=outf):
                prev_pending(); outf()
            pending = pend2
        else:
            mkout()()
    if pending is not None:
        pending()
    return P

def prep_A(core, x, mod_l, norm_g, w_in_l, positions):
    T0 = core * 1024
    d = dict(xT=np.ascontiguousarray(x[0, T0:T0 + 1024, :].T),
             modv=np.ascontiguousarray(mod_l.reshape(6, 16, 128).transpose(2, 0, 1)),
             gn=np.ascontiguousarray(norm_g.reshape(16, 128).T),
             w=w_in_l, pos=np.ascontiguousarray(positions[:, T0:T0 + 1024]))
    d.update(consts_A())
    return d


NEG = -1e30
NT = 53

def attn_tiles():
    groups = []
    for gi in range(4):
        g = []
        for i in (2 * gi, 2 * gi + 1):
            bank = i // 4; c0 = (i % 4) * 128
            g.append(dict(ks=2048 + 128 * (i - 1), kst=1, kp=128, qs=128 * i, qst=1, nq=128, m=i, vt=i, outs=[(bank, c0, 1, 128, 0)]))
            g.append(dict(ks=2048 + 128 * i, kst=1, kp=128, qs=128 * i, qst=1, nq=128, m=11, vt=i + 1, outs=[(bank, c0, 1, 128, 0)]))
        groups.append(g)
    for r in range(4):
        g = []
        for i in range(2):
            g.append(dict(ks=2048 + 512 * (i - 1) + r, kst=4, kp=128, qs=512 * i + r, qst=4, nq=128, m=8 + i, vt=9 + r * 3 + i, outs=[(i, r, 4, 128, 0)]))
            g.append(dict(ks=2048 + 512 * i + r, kst=4, kp=128, qs=512 * i + r, qst=4, nq=128, m=11, vt=9 + r * 3 + i + 1, outs=[(i, r, 4, 128, 0)]))
        groups.append(g)
    for r4 in range(4):
        g = []
        for r in range(4 * r4, 4 * r4 + 4):
            outs = [(0, r, 16, 32, 0), (1, r, 16, 32, 32)]
            g.append(dict(ks=r, kst=16, kp=128, qs=r, qst=16, nq=64, m=10, vt=21 + 2 * r, outs=outs))
            g.append(dict(ks=2048 + r, kst=16, kp=64, qs=r, qst=16, nq=64, m=11, vt=21 + 2 * r + 1, outs=outs))
        groups.append(g)
    return groups

def sl(start, step, n):
    return slice(start, start + step * (n - 1) + 1, step)

def emit_attn(P, q_d, k_d, v_d, mask_d, ident_d, out_d, psS, psN, psD):
    qb = [P.sb('qb%d' % i, [128, 1024], BF16) for i in range(2)]
    kb = [P.sb('kb%d' % i, [128, 3072], BF16) for i in range(2)]
    vb = [P.sb('vb%d' % i, [128, NT, 128], BF16) for i in range(2)]
    masks = P.sb('masks', [128, 12, 128], BF16); ident = P.sb('ident', [128, 128], BF16); ones = P.sb('onesb', [128, 128], BF16)
    pb = [P.sb('pb%d' % i, [128, 512], BF16) for i in range(2)]
    rden = P.sb('rden', [128, 1024]); ao = [P.sb('ao%d' % i, [128, 1024]) for i in range(2)]
    t_mask = P.dma('act', masks[:], mask_d[:, :, :]); t_id = P.dma('act', ident[:], ident_d[:, :])
    t_ones = P.memset(ones[:], 1.0)
    groups = attn_tiles()
    scale = 1.0 / math.sqrt(128.0)
    load_tok = [None, None]; buf_free = [[], []]
    def load(h):
        b = h % 2
        t1 = P.dma('sp', qb[b][:], q_d[:, h, :], deps=buf_free[b], slot='q%d' % b)
        t2 = P.dma('sp', kb[b][:], k_d[:, h, :], deps=buf_free[b], slot='k%d' % b)
        t3 = P.dma('sp', vb[b][:], v_d[:, h, :, :], deps=buf_free[b], slot='v%d' % b)
        load_tok[b] = [t1, t2, t3]
    load(0); load(1)
    S_free = [None, None]; pb_free = [None, None]
    nd_free = []
    ao_free = [None, None]
    gcount = 0
    for h in range(12):
        b = h % 2
        first_in_bank = {('N', 0): True, ('N', 1): True, ('D', 0): True, ('D', 1): True}
        pend = None
        last_pv = None
        def do_pv(g, sb_i, t_exp):
            nonlocal last_pv
            c0 = 0
            for t in g:
                for (bank, st, step, n, pc0) in t['outs']:
                    for kind, ps, lhs in (('N', psN, vb[b][0:t['kp'], t['vt'], :]), ('D', psD, ones[0:t['kp'], :])):
                        fst = first_in_bank[(kind, bank)]
                        first_in_bank[(kind, bank)] = False
                        last_pv = P.mm(ps[bank][:, sl(st, step, n)], lhs, pb[sb_i][0:t['kp'], c0 + pc0:c0 + pc0 + n],
                                       start=fst, stop=False, deps=[t_exp, t_ones] + (nd_free if fst else []))
                c0 += t['nq']
            return last_pv
        for g in groups:
            si = gcount % 2
            c0 = 0
            last = None
            for ti, t in enumerate(g):
                deps = load_tok[b] + [S_free[si], t_mask, t_id] if ti == 0 else []
                P.mm(psS[si][0:t['kp'], c0:c0 + t['nq']], kb[b][:, sl(t['ks'], t['kst'], t['kp'])], qb[b][:, sl(t['qs'], t['qst'], t['nq'])],
                     start=True, stop=False, deps=deps)
                mslice = masks[0:t['kp'], t['m'], 0:t['nq']]
                last = P.mm(psS[si][0:t['kp'], c0:c0 + t['nq']], ident[0:t['kp'], 0:t['kp']], mslice, start=False, stop=True)
                c0 += t['nq']
            t_exp = P.act(pb[si][:, 0:c0], psS[si][:, 0:c0], AF.Exp, scale=scale, deps=[last, pb_free[si]])
            S_free[si] = t_exp
            if pend is not None:
                pg, psi, ptexp = pend
                pb_free[psi] = do_pv(pg, psi, ptexp)
            pend = (g, si, t_exp)
            gcount += 1
        pg, psi, ptexp = pend
        pb_free[psi] = do_pv(pg, psi, ptexp)
        buf_free[b] = [last_pv]
        if h + 2 < 12:
            load(h + 2)
        oi = h % 2
        evs = []
        for bank in range(2):
            cs = slice(bank * 512, (bank + 1) * 512)
            a = P.recip(rden[:, cs], psD[bank][:], deps=[last_pv])
            e = P.tt(ao[oi][:, cs], psN[bank][:], rden[:, cs], ALU.mult, deps=[a, ao_free[oi]])
            evs.append(e)
        nd_free = evs
        ao_free[oi] = P.dma('act', out_d[h * 128:(h + 1) * 128, :], ao[oi][:], deps=evs, slot='ao%d' % oi, is_out=True)

def build_B(do_lru=True):
    P = Prog()
    q_d = P.din('q', [128, 12, 1024], BF16); k_d = P.din('k', [128, 12, 3072], BF16); v_d = P.din('v', [128, 12, NT, 128], BF16)
    mask_d = P.din('masks', [128, 12, 128], BF16); ident_d = P.din('ident', [128, 128], BF16)
    out_d = P.dout('attnT', [1536, 1024])
    psS = [P.ps('psS%d' % i) for i in range(2)]; psN = [P.ps('psN%d' % i) for i in range(2)]; psD = [P.ps('psD%d' % i) for i in range(2)]
    emit_attn(P, q_d, k_d, v_d, mask_d, ident_d, out_d, psS, psN, psD)
    if do_lru:
        psG = [P.ps('psG%d' % i) for i in range(2)]
        emit_lru(P, psG)
    return P

def emit_lru(P, psG):
    xr_d = P.din('xr', [128, 12, 1027]); cw_d = P.din('cw', [128, 12, 4]); vec_d = P.din('vecs', [128, 4, 12])
    wa_d = P.din('wa', [128, 12, 128]); wx_d = P.din('wx', [128, 12, 128]); pos_d = P.din('pos', [1, 1024], I32)
    hl_d = P.dout('hloc', [1536, 1024]); pp_d = P.dout('pprod', [1536, 1024])
    xr = P.sb('xr', [128, 12, 1027]); cw = P.sb('cw', [128, 12, 4]); vecs = P.sb('vecs', [128, 4, 12])
    wa = P.sb('wa', [128, 12, 128], BF16); wx = P.sb('wx', [128, 12, 128], BF16)
    posi = P.sb('lposi', [1, 1024], I32); nzr = P.sb('nzr', [1, 1024]); nz = P.sb('nz', [128, 1024]); ones1 = P.sb('ones1', [1, 128])
    sca = P.sb('sca', [128, 12]); sca2 = P.sb('sca2', [128, 12]); zeros = P.sb('zeros', [128, 1024])
    xc = P.sb('xc', [128, 1024]); xcb = P.sb('xcb', [128, 1024], BF16)
    rb = P.sb('rb', [128, 1024]); ib = P.sb('ib', [128, 1024]); ab = P.sb('ab', [128, 1024]); mb_ = P.sb('mb', [128, 1024])
    ho = [P.sb('ho%d' % i, [128, 1024]) for i in range(2)]; po = [P.sb('po%d' % i, [128, 1024]) for i in range(2)]
    t_xr = P.dma('sp', xr[:], xr_d[:, :, :]); t_cw = P.dma('sp', cw[:], cw_d[:, :, :]); t_v = P.dma('sp', vecs[:], vec_d[:, :, :])
    t_wa = P.dma('pool', wa[:], wa_d[:, :, :]); t_wx = P.dma('pool', wx[:], wx_d[:, :, :]); t_pos = P.dma('sp', posi[:], pos_d[:, :])
    t_z = P.memset(zeros[:], 0.0, eng='pool'); t_o1 = P.memset(ones1[:], 1.0, eng='pool')
    a = P.cp(nzr[:], posi[:], deps=[t_pos])
    a = P.ts(nzr[:], nzr[:], 0.0, None, ALU.not_equal, deps=[a])
    t_nz = []
    for hf in range(2):
        cs = slice(hf * 512, (hf + 1) * 512)
        m = P.mm(psG[hf][:], ones1[:], nzr[:, cs], deps=[a, t_o1])
        t_nz.append(P.cp(nz[:, cs], psG[hf][:], deps=[m]))
    s1 = P.act(sca[:], vecs[:, 3, :], AF.Exp, scale=-1.0, deps=[t_v])
    s2 = P.act(sca[:], sca[:], AF.Ln, bias=1.0, deps=[s1])
    s3 = P.ts(sca2[:], sca[:], -16.0, None, ALU.mult, deps=[s2])
    s4 = P.ts(sca[:], sca[:], -8.0, None, ALU.mult, deps=[s3])
    g_free = t_nz
    prev = []
    ho_free = [None, None]; po_free = [None, None]
    for b in range(12):
        c = P.act(xc[:], xr[:, b, 3:1027], AF.Identity, bias=vecs[:, 0, b:b + 1], scale=cw[:, b, 3:4], deps=[t_xr, t_cw, t_v] + prev)
        for k in range(3):
            c = P.stt(xc[:], xr[:, b, k:k + 1024], cw[:, b, k:k + 1], xc[:], ALU.mult, ALU.add, deps=[c])
        cb = P.cp(xcb[:], xc[:], deps=[c] + prev, eng='pool')
        gr_ = []
        for gi, (wt, bias_i, dst, tw) in enumerate(((wa, 1, rb, t_wa), (wx, 2, ib, t_wx))):
            evs = []
            for hf in range(2):
                cs = slice(hf * 512, (hf + 1) * 512)
                m = P.mm(psG[hf][:], wt[:, b, :], xcb[:, cs], deps=[cb, tw] + (g_free if isinstance(g_free, list) else [g_free]))
                evs.append(P.act(dst[:, cs], psG[hf][:], AF.Sigmoid, bias=vecs[:, bias_i, b:b + 1], deps=[m] + prev))
            g_free = evs
            gr_.append(evs)
        ta_ = P.act(ab[:], rb[:], AF.Exp, scale=sca[:, b:b + 1], deps=gr_[0] + [s4] + prev)
        tm = P.act(mb_[:], rb[:], AF.Exp, scale=sca2[:, b:b + 1], deps=gr_[0] + [s4] + prev)
        tm = P.act(mb_[:], mb_[:], AF.Sqrt, scale=-1.0, bias=1.0, deps=[tm])
        ta2 = P.tt(ab[:], ab[:], nz[:], ALU.mult, deps=[ta_] + t_nz)
        tm = P.stt(mb_[:], mb_[:], -1.0, nz[:], ALU.add, ALU.mult, deps=[tm] + t_nz)
        tm = P.ts(mb_[:], mb_[:], 1.0, None, ALU.add, deps=[tm])
        tb_ = P.tt(ib[:], ib[:], xc[:], ALU.mult, deps=gr_[1] + [c])
        tb_ = P.tt(ib[:], ib[:], mb_[:], ALU.mult, deps=[tb_, tm])
        oi = b % 2
        sc1 = P.op('dve', lambda e, oi=oi: e.tensor_tensor_scan(ho[oi][:], ab[:], ib[:], 0.0, ALU.mult, ALU.add), deps=[ta2, tb_, ho_free[oi]])
        sc2 = P.op('dve', lambda e, oi=oi: e.tensor_tensor_scan(po[oi][:], ab[:], zeros[:], 1.0, ALU.mult, ALU.add), deps=[ta2, t_z, po_free[oi]])
        ho_free[oi] = P.dma('sp', hl_d[b * 128:(b + 1) * 128, :], ho[oi][:], deps=[sc1], slot='ho%d' % oi, is_out=True)
        po_free[oi] = P.dma('sp', pp_d[b * 128:(b + 1) * 128, :], po[oi][:], deps=[sc2], slot='po%d' % oi, is_out=True)
        prev = [sc1, sc2, cb]

def bf(a):
    return np.ascontiguousarray(a).astype(ml_dtypes.bfloat16)

def consts_B(core):
    kk = np.arange(128)[:, None]; qi = np.arange(128)[None, :]
    A = np.where(kk >= qi, 0.0, NEG).astype(np.float32)
    Bm = np.where(kk <= qi, 0.0, NEG).astype(np.float32)
    allneg = np.full((128, 128), NEG, np.float32)
    m = np.zeros((128, 12, 128), np.float32)
    for i in range(8):
        m[:, i, :] = allneg if (core == 0 and i == 0) else A
    for i in range(2):
        m[:, 8 + i, :] = allneg if (core == 0 and i == 0) else A
    if core == 0:
        m[:, 10, :] = allneg
    elif core == 1:
        mm_ = A.copy(); mm_[:64, :] = NEG
        m[:, 10, :] = mm_
    else:
        m[:, 10, :] = A
    m[:, 11, :] = Bm
    return dict(masks=bf(m), ident=bf(np.eye(128, dtype=np.float32)))

def prep_B_attn(core, qT_all, kT_all, vT_all):
    T0 = core * 1024
    q = qT_all[:, T0:T0 + 1024].reshape(12, 128, 1024).transpose(1, 0, 2)
    kpad = np.zeros((1536, 2048 + 8192), dtype=kT_all.dtype); kpad[:, 2048:] = kT_all
    k = kpad[:, T0:T0 + 3072].reshape(12, 128, 3072).transpose(1, 0, 2)
    vtok = np.zeros((2048 + 8192, 1536), dtype=vT_all.dtype); vtok[2048:] = vT_all.T
    base = T0 + 2048
    idx = np.zeros((NT, 128), np.int64); valid = np.ones((NT, 128), bool)
    for j in range(9):
        idx[j] = base + 128 * (j - 1) + np.arange(128)
    for r in range(4):
        for j in range(3):
            idx[9 + r * 3 + j] = base + 512 * (j - 1) + 4 * np.arange(128) + r
    for r in range(16):
        idx[21 + 2 * r] = base - 2048 + 16 * np.arange(128) + r
        ii = base + 16 * np.arange(128) + r
        valid[21 + 2 * r + 1, 64:] = False
        ii[64:] = 0
        idx[21 + 2 * r + 1] = ii
    vt = vtok[idx]
    vt[~valid] = 0
    v = vt.reshape(NT, 128, 12, 128).transpose(1, 2, 0, 3)
    d = dict(q=np.ascontiguousarray(q), k=np.ascontiguousarray(k), v=np.ascontiguousarray(v))
    d.update(consts_B(core))
    return d

def prep_B_lru(core, xrT_all, conv_w, conv_b, wa, ba, wx, bx, lam, positions):
    T0 = core * 1024
    xpad = np.zeros((1536, 3 + 8192), np.float32); xpad[:, 3:] = xrT_all
    xr = xpad[:, T0:T0 + 1027].reshape(12, 128, 1027).transpose(1, 0, 2)
    cw = conv_w.T.reshape(12, 128, 4).transpose(1, 0, 2)
    f = lambda v: v.reshape(12, 128).T
    vecs = np.stack([f(conv_b), f(ba), f(bx), f(lam)], axis=1)
    return dict(xr=np.ascontiguousarray(xr), cw=np.ascontiguousarray(cw), vecs=np.ascontiguousarray(vecs),
                wa=np.ascontiguousarray(wa.transpose(1, 0, 2)), wx=np.ascontiguousarray(wx.transpose(1, 0, 2)),
                pos=np.ascontiguousarray(positions[:, T0:T0 + 1024]))


def build_C1():
    P = Prog()
    xT_d = P.din('xT', [2048, 1024]); at_d = P.din('attnT', [1536, 1024]); hl_d = P.din('hloc', [1536, 1024])
    pp_d = P.din('pprod', [1536, 1024]); gr_d = P.din('grT', [1536, 1024])
    summ_d = P.din('summ', [128, 8, 2, 12]); sel_d = P.din('sel', [128, 8])
    modv_d = P.din('modv', [128, 6, 16]); g2n_d = P.din('g2n', [128, 16]); og_d = P.din('og', [128, 24])
    wo_d = P.din('wo', [3072, 2048]); rw_d = P.din('rw', [128, 16, 32]); rb_d = P.din('rb', [1, 32])
    utri_d = P.din('utri', [128, 128], BF16)
    x1_o = P.dout('x1T', [2048, 1024]); h2_o = P.dout('h2T', [2048, 1024], BF16)
    G_o = P.dout('G', [128, 8, 32]); pm_o = P.dout('pm', [128, 8, 32]); cnt_o = P.dout('cnt', [128, 32])

    x = P.sb('x', [128, 16, 1024])
    y = P.sb('y', [128, 24, 1024], BF16)
    st = [P.sb('st%d' % i, [128, 1024]) for i in range(2)]
    st2 = [P.sb('st2_%d' % i, [128, 1024]) for i in range(2)]
    st3 = [P.sb('st3_%d' % i, [128, 1024]) for i in range(2)]
    sq = [P.sb('sq%d' % i, [128, 1024], BF16) for i in range(2)]
    summ = P.sb('summ', [128, 8, 2, 12]); sel = P.sb('sel', [128, 8]); Hc = P.sb('Hc', [128, 12]); Hs = P.sb('Hs', [128, 12])
    mods = P.sb('mods', [128, 6, 16]); g2n = P.sb('g2n', [128, 16]); og = P.sb('og', [128, 24]); G2 = P.sb('G2', [128, 16])
    ones = P.sb('ones', [128, 128], BF16); utri = P.sb('utri', [128, 128], BF16); ones1 = P.sb('ones1', [1, 128])
    rw = P.sb('rw', [128, 16, 32]); rb = P.sb('rb', [1, 32])
    rstdA = P.sb('rstdA', [128, 1024]); rstdL = P.sb('rstdL', [128, 1024])
    wb = [P.sb('wb%d' % i, [128, 24, 256], BF16) for i in range(2)]
    t1 = [P.sb('t1_%d' % i, [128, 512]) for i in range(2)]; t2 = [P.sb('t2_%d' % i, [128, 512]) for i in range(2)]
    hb = [P.sb('hb%d' % i, [128, 1024], BF16) for i in range(2)]
    lg = P.sb('lg', [128, 8, 32]); top8 = P.sb('top8', [128, 8, 8]); nmax = P.sb('nmax', [128, 8]); maskf = P.sb('maskf', [128, 8, 32])
    maskb = P.sb('maskb', [128, 8, 32], BF16); ex = P.sb('ex', [128, 8, 32]); den = P.sb('den', [128, 8]); Gs = P.sb('Gs', [128, 8, 32]); pm = P.sb('pm', [128, 8, 32])
    ps = [P.ps('b%d' % i) for i in range(8)]

    t_x = [P.dma('sp', x[:, kc * 4:(kc + 1) * 4, :], xT_d.rearrange("(kc p) t -> p kc t", p=128)[:, kc * 4:(kc + 1) * 4, :]) for kc in range(4)]
    t_su = P.dma('act', summ[:], summ_d[:, :, :, :]); t_sel = P.dma('act', sel[:], sel_d[:, :])
    t_m = P.dma('act', mods[:], modv_d[:, :, :]); t_g2 = P.dma('act', g2n[:], g2n_d[:, :]); t_og = P.dma('act', og[:], og_d[:, :])
    t_rw = P.dma('act', rw[:], rw_d[:, :, :]); t_rb = P.dma('act', rb[:], rb_d[:, :]); t_ut = P.dma('act', utri[:], utri_d[:, :])
    t_ones = P.memset(ones[:], 1.0); t_o1 = P.memset(ones1[:], 1.0)
    wv = wo_d.rearrange("(kc p) n -> p kc n", p=128)
    t_w = [None] * 8
    def load_w(j, deps):
        t_w[j] = P.dma('pool', wb[j % 2][:], wv[:, :, j * 256:(j + 1) * 256], deps=deps, slot='w%d' % (j % 2))
    load_w(0, []); load_w(1, [])
    a = P.memset(Hc[:], 0.0, deps=[]); b = P.memset(Hs[:], 0.0)
    tH = [a, b]
    for c in range(8):
        s = P.stt(Hs[:], Hc[:], sel[:, c:c + 1], Hs[:], ALU.mult, ALU.add, deps=tH + [t_sel, t_su])
        u = P.tt(Hc[:], Hc[:], summ[:, c, 0, :], ALU.mult, deps=[s])
        u = P.tt(Hc[:], Hc[:], summ[:, c, 1, :], ALU.add, deps=[u])
        tH = [u]
    t_Hs = tH
    G2t = P.stt(G2[:], mods[:, 4, :], 1.0, g2n[:], ALU.add, ALU.mult, deps=[t_m, t_g2])
    st_free = [None, None]; st2_free = [None, None]; st3_free = [None, None]; sq_free = [None, None]
    lastA = [None, None]; lastL = [None, None]
    t_y = []
    for ci in range(24):
        i = ci % 2
        if ci < 12:
            b_ = ci
            ld = P.dma('sp', st[i][:], at_d[b_ * 128:(b_ + 1) * 128, :], deps=[st_free[i]], slot='st%d' % i)
            src_ready = [ld]
        else:
            b_ = ci - 12
            l1 = P.dma('sp', st[i][:], hl_d[b_ * 128:(b_ + 1) * 128, :], deps=[st_free[i]], slot='st%d' % i)
            l2 = P.dma('sp', st2[i][:], pp_d[b_ * 128:(b_ + 1) * 128, :], deps=[st2_free[i]], slot='st2_%d' % i)
            l3 = P.dma('sp', st3[i][:], gr_d[b_ * 128:(b_ + 1) * 128, :], deps=[st3_free[i]], slot='st3_%d' % i)
            hf_ = P.stt(st[i][:], st2[i][:], Hs[:, b_:b_ + 1], st[i][:], ALU.mult, ALU.add, deps=[l1, l2] + t_Hs)
            ge = P.act(st2[i][:], st3[i][:], AF.Gelu_apprx_tanh, deps=[l3, hf_])
            st3_free[i] = ge
            lr = P.tt(st[i][:], st[i][:], st2[i][:], ALU.mult, deps=[hf_, ge])
            st2_free[i] = lr
            src_ready = [lr]
        s_ = P.act(sq[i][:], st[i][:], AF.Square, deps=src_ready + [sq_free[i]])
        mmt = None
        for hf in range(2):
            bank = (0 if ci < 12 else 2) + hf
            mmt = P.mm(ps[bank][:], ones[:], sq[i][:, hf * 512:(hf + 1) * 512], start=(ci % 12 == 0), stop=(ci % 12 == 11), deps=[s_, t_ones])
            if ci < 12: lastA[hf] = mmt
            else: lastL[hf] = mmt
        sq_free[i] = mmt
        yy = P.ts(y[:, ci, :], st[i][:], og[:, ci:ci + 1], None, ALU.mult, deps=src_ready + [t_og], eng='pool')
        st_free[i] = [yy, s_]
        st_free[i] = yy
        st_free[i] = P.op('pool', lambda e: e.memset(ones1[0:1, 0:1], 1.0), deps=[yy, s_])
        t_y.append(yy)
    t_rA = []; t_rL = []
    for hf in range(2):
        cs = slice(hf * 512, (hf + 1) * 512)
        a = P.act(rstdA[:, cs], ps[hf][:], AF.Sqrt, bias=1e-6, scale=1.0 / 1536, deps=[lastA[hf]])
        t_rA.append(P.recip(rstdA[:, cs], rstdA[:, cs], deps=[a]))
        a = P.act(rstdL[:, cs], ps[2 + hf][:], AF.Sqrt, bias=1e-6, scale=1.0 / 1536, deps=[lastL[hf]])
        t_rL.append(P.recip(rstdL[:, cs], rstdL[:, cs], deps=[a]))
    bank_free = {4: None, 5: None, 6: None, 7: None}
    tfree = [None, None]
    t_x1 = []
    u = 0
    for dmc in range(16):
        j = dmc // 2
        for hf in range(2):
            cs = slice(hf * 512, (hf + 1) * 512)
            bA = 4 + (u % 2) * 2; bL = bA + 1
            la = None; ll = None
            for ci in range(12):
                la = P.mm(ps[bA][:], wb[j % 2][:, ci, (dmc % 2) * 128:(dmc % 2 + 1) * 128], y[:, ci, cs], start=(ci == 0), stop=(ci == 11),
                          deps=[t_y[ci]] + ([t_w[j], bank_free[bA]] if ci == 0 else []))
            for ci in range(12, 24):
                ll = P.mm(ps[bL][:], wb[j % 2][:, ci, (dmc % 2) * 128:(dmc % 2 + 1) * 128], y[:, ci, cs], start=(ci == 12), stop=(ci == 23),
                          deps=[t_y[ci]] + ([bank_free[bL]] if ci == 12 else []))
            if dmc % 2 == 1 and hf == 1 and j + 2 < 8:
                load_w(j + 2, [ll])
            i2 = u % 2
            e1 = P.tt(t1[i2][:], ps[bA][:], rstdA[:, cs], ALU.mult, deps=[la, t_rA[hf], tfree[i2]])
            e2 = P.tt(t2[i2][:], ps[bL][:], rstdL[:, cs], ALU.mult, deps=[ll, t_rL[hf], tfree[i2]])
            bank_free[bA] = e1; bank_free[bL] = e2
            e3 = P.tt(t1[i2][:], t1[i2][:], t2[i2][:], ALU.add, deps=[e1, e2], eng='pool')
            e4 = P.stt(x[:, dmc, cs], t1[i2][:], mods[:, 2, dmc:dmc + 1], x[:, dmc, cs], ALU.mult, ALU.add, deps=[e3, t_m, t_x[dmc // 4]])
            tfree[i2] = e4
            t_x1.append(e4)
            u += 1
    for kc in range(4):
        P.dma('sp', x1_o.rearrange("(kc p) t -> p kc t", p=128)[:, kc * 4:(kc + 1) * 4, :], x[:, kc * 4:(kc + 1) * 4, :], deps=t_x1[kc * 8:(kc + 1) * 8], is_out=True)
    sq_free = [t_y[-1], t_y[-1]]
    lastN = [None, None]
    for kc in range(16):
        i = kc % 2
        s_ = P.act(sq[i][:], x[:, kc, :], AF.Square, deps=t_x1[2 * kc:2 * kc + 2] + [sq_free[i]])
        for hf in range(2):
            lastN[hf] = P.mm(ps[hf][:], ones[:], sq[i][:, hf * 512:(hf + 1) * 512], start=(kc == 0), stop=(kc == 15), deps=[s_] + (t_rA + t_rL if kc == 0 else []))
        sq_free[i] = lastN[1]
    rstd2 = rstdA
    t_r2 = []
    for hf in range(2):
        cs = slice(hf * 512, (hf + 1) * 512)
        a = P.act(rstd2[:, cs], ps[hf][:], AF.Sqrt, bias=1e-6, scale=1.0 / 2048, deps=[lastN[hf]] + t_x1)
        t_r2.append(P.recip(rstd2[:, cs], rstd2[:, cs], deps=[a]))
    LG = ps[2]
    st_free = [None, None]; st2_free = [None, None]; hb_free = [None, None]
    last_r = None
    for kc in range(16):
        i = kc % 2
        a = P.tt(st[i][:], x[:, kc, :], rstd2[:], ALU.mult, deps=t_r2 + [st_free[i]])
        hh = P.act(st2[i][:], st[i][:], AF.Identity, bias=mods[:, 3, kc:kc + 1], scale=G2[:, kc:kc + 1], deps=[a, G2t, st2_free[i]])
        st_free[i] = hh
        for g in range(8):
            last_r = P.mm(LG[:, g * 32:(g + 1) * 32], st2[i][:, g * 128:(g + 1) * 128], rw[:, kc, :], start=(kc == 0 and g == 0), stop=False,
                          deps=[hh, t_rw] + t_rL)
        cb = P.cp(hb[i][:], st2[i][:], deps=[hh, hb_free[i]], eng='pool')
        st2_free[i] = P.op('pool', lambda e: e.memset(ones1[0:1, 0:1], 1.0), deps=[cb, last_r])
        hb_free[i] = P.dma('sp', h2_o[kc * 128:(kc + 1) * 128, :], hb[i][:], deps=[cb], slot='hb%d' % i, is_out=True)
    for g in range(8):
        last_r = P.mm(LG[:, g * 32:(g + 1) * 32], ones1[:, :], rb[:, :], start=False, stop=(g == 7), deps=[t_rb, t_o1])
    c0 = P.cp(lg[:].rearrange("p g e -> p (g e)"), LG[:, 0:256], deps=[last_r])
    tk = []
    for g in range(8):
        m = P.op('dve', lambda e, g=g: e.max(top8[:, g, :], lg[:, g, :]), deps=[c0])
        tk.append(m)
    n1 = P.ts(nmax[:], top8[:, :, 0], -1.0, None, ALU.mult, deps=tk)
    last = n1
    for g in range(8):
        mk = P.ts(maskf[:, g, :], lg[:, g, :], top8[:, g, 3:4], None, ALU.is_ge, deps=[last])
        e_ = P.act(ex[:, g, :], lg[:, g, :], AF.Exp, bias=nmax[:, g:g + 1], deps=[n1])
        em = P.tt(ex[:, g, :], ex[:, g, :], maskf[:, g, :], ALU.mult, deps=[mk, e_])
        dn = P.op('dve', lambda e, g=g: e.tensor_reduce(den[:, g:g + 1], ex[:, g, :], mybir.AxisListType.X, ALU.add), deps=[em])
        last = dn
    rd = P.recip(den[:], den[:], deps=[last])
    for g in range(8):
        last = P.ts(Gs[:, g, :], ex[:, g, :], den[:, g:g + 1], None, ALU.mult, deps=[rd])
    P.dma('sp', G_o[:, :, :], Gs[:], deps=[last], is_out=True)
    mb = P.cp(maskb[:], maskf[:], deps=[last])
    PO = ps[3]
    lp = None
    for g in range(8):
        lp = P.mm(PO[:, g * 32:(g + 1) * 32], utri[:], maskb[:, g, :], start=(g == 0), stop=False, deps=[mb, t_ut] + t_r2)
        for g2 in range(g):
            lp = P.mm(PO[:, g * 32:(g + 1) * 32], ones[:], maskb[:, g2, :], start=False, stop=False)
    lc_ = None
    for g in range(8):
        lc_ = P.mm(PO[:, 256:288], ones[:], maskb[:, g, :], start=False, stop=False, deps=[mb])
    cnts = P.sb('cnts', [128, 32])
    cc_ = P.cp(cnts[:], PO[:, 256:288], deps=[lc_])
    P.dma('sp', cnt_o[:, :], cnts[:], deps=[cc_], is_out=True)
    pmf = pm[:].rearrange("p g e -> p (g e)")
    a = P.stt(pmf, PO[:, 0:256], 1.0, maskf[:].rearrange("p g e -> p (g e)"), ALU.add, ALU.mult, deps=[lp, cc_])
    a = P.ts(pmf, pmf, -1.0, None, ALU.add, deps=[a])
    P.dma('sp', pm_o[:, :, :], pm[:], deps=[a], is_out=True)
    return P

def prep_C1(core, xT_core, attnT, hloc_all, pprod_all, grT_core, mod_l, norm2_g, attn_g, lru_g, w_out_l, router_w_l, router_b_l):
    summ = np.zeros((128, 8, 2, 12), np.float32)
    for c in range(8):
        summ[:, c, 0, :] = pprod_all[c][:, -1].reshape(12, 128).T
        summ[:, c, 1, :] = hloc_all[c][:, -1].reshape(12, 128).T
    sel = np.zeros((128, 8), np.float32); sel[:, core] = 1.0
    og = np.concatenate([attn_g.reshape(12, 128).T, lru_g.reshape(12, 128).T], axis=1)
    utri = (np.arange(128)[:, None] < np.arange(128)[None, :]).astype(np.float32)
    return dict(xT=xT_core, attnT=attnT, hloc=hloc_all[core], pprod=pprod_all[core], grT=grT_core, summ=summ, sel=sel,
                modv=np.ascontiguousarray(mod_l.reshape(6, 16, 128).transpose(2, 0, 1)), g2n=np.ascontiguousarray(norm2_g.reshape(16, 128).T),
                og=np.ascontiguousarray(og), wo=w_out_l, rw=np.ascontiguousarray(router_w_l.reshape(16, 128, 32).transpose(1, 0, 2)),
                rb=np.ascontiguousarray(router_b_l[None, :]), utri=utri.astype(ml_dtypes.bfloat16))


SCS = 1792
NBS = SCS // 128
NSC = 2
CAPT = SCS * NSC
NBT = CAPT // 128
BIG = 1000000.0
SUBS = [(0, 512), (512, 512), (1024, 512), (1536, 256)]

def _ind(P, kind, out, in_, idx_ap, deps, slot, **kw):
    P.dma_slots[slot] = P.dma_slots.get(slot, 0) + 1
    cnt = P.dma_slots[slot]
    if not hasattr(P, 'regcache'):
        P.regcache = {}
    if 'bounds_check' in kw:
        bval = kw.pop('bounds_check')
        okw = dict(kw)
        def fix(e, okw=okw, bval=bval):
            if bval not in P.regcache:
                P.regcache[bval] = e.to_reg(bval)
            d = dict(okw); d['bounds_check'] = P.regcache[bval]
            return d
    else:
        okw = dict(kw)
        def fix(e, okw=okw):
            return okw
    if kind == 'g':
        fn = lambda e: e.indirect_dma_start(out=out, out_offset=None, in_=in_, in_offset=bass.IndirectOffsetOnAxis(ap=idx_ap, axis=0), **fix(e))
    else:
        fn = lambda e: e.indirect_dma_start(out=out, out_offset=bass.IndirectOffsetOnAxis(ap=idx_ap, axis=0), in_=in_, in_offset=None, **fix(e))
    P.ops['pool'].append(dict(fn=fn, deps=[d for d in deps if d is not None], sig=False, dma=(slot, cnt)))
    return ('d', slot, cnt)

def _ind_old(P, kind, out, in_, idx_ap, deps, slot, **kw):
    cnt = 0
    if kind == 'g':
        fn = lambda e: e.indirect_dma_start(out=out, out_offset=None, in_=in_, in_offset=bass.IndirectOffsetOnAxis(ap=idx_ap, axis=0), **kw)
    else:
        fn = lambda e: e.indirect_dma_start(out=out, out_offset=bass.IndirectOffsetOnAxis(ap=idx_ap, axis=0), in_=in_, in_offset=None, **kw)
    P.ops['pool'].append(dict(fn=fn, deps=[d for d in deps if d is not None], sig=False, dma=(slot, cnt)))
    return ('d', slot, cnt)

def build_C3(NE=4, nsc=NSC):
    P = Prog(); nc = P.nc
    h_d = P.din('h2all', [8192, 2048], BF16); pm_d = P.din('pm', [128, 64, NE]); cnt_d = P.din('cnt', [128, 8, NE]); G_d = P.din('Grows', [8192, NE])
    tid_d = P.din('tid', [128, 64], I32); dum_d = P.din('dumrow', [128, 1]); id_d = P.din('ident', [128, 128], BF16)
    w1_d = P.din('w1', [NE, 2048, 4096]); w2_d = P.din('w2', [NE, 2048, 2048])
    b1g_d = P.din('b1g', [128, NE, 16]); b1l_d = P.din('b1l', [128, NE, 16]); b2_d = P.din('b2', [1, NE, 2048])
    y_o = P.dout('ypart', [8192 + 128, 2048])
    lists = [nc.dram_tensor("lists%d" % e, [CAPT, 1], I32, kind="Internal").ap() for e in range(NE)]

    pm = P.sb('pm', [128, 64, NE]); cnt = P.sb('cnt', [128, 8, NE]); start = P.sb('start', [128, 8, NE]); valid = P.sb('valid', [128, 64, NE])
    posg = P.sb('posg', [128, 64, NE]); inv = P.sb('inv', [128, 64, NE]); posi = P.sb('posi', [128, 64, NE], I32)
    tid = P.sb('tid', [128, 64], I32); dum = P.sb('dum', [128, 1]); ident = P.sb('ident', [128, 128], BF16)
    bigt = P.sb('bigt', [128, NBT], I32)
    idxt = P.sb('idxt', [128, NE, NBT], I32); idxf = P.sb('idxf', [128, NE, NBT]); v2 = P.sb('v2', [128, NE, NBT]); isc = P.sb('isc', [128, 4, NE, NBT], I32); iscf = P.sb('iscf', [128, NE, NBT])
    gsl = P.sb('gsl', [128, NBT, NE])
    b1g = P.sb('b1g', [128, NE, 16]); b1l = P.sb('b1l', [128, NE, 16]); b2 = P.sb('b2', [1, 2048]); ones1 = P.sb('ones1', [1, 128])
    XeT = P.sb('XeT', [128, 16, SCS], BF16); actT = P.sb('actT', [128, 16, SCS], BF16)
    Xs = [P.sb('Xs%d' % i, [128, 2048], BF16) for i in range(3)]
    w1b = [P.sb('w1b%d' % i, [128, 16, 256], BF16) for i in range(2)]
    w2b = [P.sb('w2b%d' % i, [128, 16, 512], BF16) for i in range(2)]
    Yst = [P.sb('Yst%d' % i, [128, 512]) for i in range(4)]
    gc = P.sb('gc', [128, 512]); sgm = P.sb('sgm', [128, 512]); lc = P.sb('lc', [128, 512])
    zer = Yst
    banks = [P.ps('b%d' % i) for i in range(6)]
    tbank = [P.ps('tb%d' % i, [128, 1024], BF16) for i in range(2)]
    bfree = [[] for _ in range(6)]; bptr = [0]
    def alloc():
        i = bptr[0] % 6; bptr[0] += 1
        return i, list(bfree[i])

    t_pm = P.dma('sp', pm[:], pm_d[:, :, :]); t_cnt = P.dma('sp', cnt[:], cnt_d[:, :, :]); t_tid = P.dma('sp', tid[:], tid_d[:, :])
    t_c = [P.dma('act', b1g[:], b1g_d[:, :, :]), P.dma('act', b1l[:], b1l_d[:, :, :]), P.dma('act', dum[:], dum_d[:, :]), P.dma('act', ident[:], id_d[:, :])]
    t_o1 = P.memset(ones1[:], 1.0)
    zt = [P.memset(Yst[i][:], 0.0, eng='pool') for i in range(4)]
    yv = y_o.rearrange("(b p) (c n) -> b p c n", p=128, n=512)
    t_zero = []
    for b in range(65):
        t_zero.append(P.dma('sp' if b % 2 == 0 else 'act', yv[b, :, :, :], Yst[0][:].rearrange("p (o n) -> p o n", o=1).broadcast(1, 4) if False else Yst[b % 4][:, None, :].to_broadcast([128, 4, 512]) if False else Yst[b % 4][:], deps=zt, slot='z%d' % (b % 4))) if False else None
    t_zero = []
    for b in range(65):
        for c4 in range(4):
            t_zero.append(P.dma('sp' if (b + c4) % 2 == 0 else 'act', y_o[b * 128:(b + 1) * 128, c4 * 512:(c4 + 1) * 512], Yst[c4][:], deps=zt, slot='z%d' % c4))
    t_zero_last = t_zero[-4:]
    w1v = w1_d.rearrange("e (kc p) n -> e p kc n", p=128); w2v = w2_d.rearrange("e (kc p) n -> e p kc n", p=128)
    w1_free = [None, None]; w2_free = [None, None]; w1_tok = {}; w2_tok = {}
    w1_seq = [(e, s, c) for e in range(NE) for s in range(nsc) for c in range(16)]
    w2_seq = [(e, s, c) for e in range(NE) for s in range(nsc) for c in range(4)]
    w1_i = [0]; w2_i = [0]
    def issue_w1():
        if w1_i[0] >= len(w1_seq): return
        k = w1_i[0]; e, s, c = w1_seq[k]
        w1_tok[(e, s, c)] = (P.dma('pool', w1b[k % 2][:], w1v[e, :, :, c * 256:(c + 1) * 256], deps=[w1_free[k % 2]], slot='w1_%d' % (k % 2)), k % 2)
        w1_i[0] += 1
    def issue_w2():
        if w2_i[0] >= len(w2_seq): return
        k = w2_i[0]; e, s, c = w2_seq[k]
        w2_tok[(e, s, c)] = (P.dma('pool', w2b[k % 2][:], w2v[e, :, :, c * 512:(c + 1) * 512], deps=[w2_free[k % 2]], slot='w2_%d' % (k % 2)), k % 2)
        w2_i[0] += 1
    a = P.memset(start[:, 0, :], 0.0, deps=[t_cnt])
    for s in range(1, 8):
        a = P.tt(start[:, s, :], start[:, s - 1, :], cnt[:, s - 1, :], ALU.add, deps=[a, t_cnt])
    t_start = a
    tv = P.ts(valid[:], pm[:], 0.0, None, ALU.is_ge, deps=[t_pm])
    last = tv
    for s in range(8):
        for e in range(NE):
            last = P.ts(posg[:, 8 * s:8 * s + 8, e], pm[:, 8 * s:8 * s + 8, e], start[:, s, e:e + 1], None, ALU.add, deps=[t_start, t_pm])
    a = P.tt(posg[:], posg[:], valid[:], ALU.mult, deps=[last, tv])
    b = P.ts(inv[:], valid[:], -1.0, -BIG, ALU.add, ALU.mult, deps=[tv])
    a = P.tt(posg[:], posg[:], inv[:], ALU.add, deps=[a, b])
    t_posi = P.cp(posi[:], posg[:], deps=[a])
    tb_ = P.memset(bigt[:], int(BIG))
    t_li = [P.dma('sp', lists[e].rearrange("(p b) o -> p (b o)", p=128), bigt[:], deps=[tb_]) for e in range(NE)]
    t_sc = []
    for e in range(NE):
        lastsc = None
        hist = []
        for G in range(64):
            lastsc = _ind(P, 's', lists[e], tid[:, G:G + 1], posi[:, G, e:e + 1], [t_posi, t_tid, t_li[e]] + ([hist[-8]] if len(hist) >= 8 else []), 'ls%d' % e, bounds_check=CAPT - 1, oob_is_err=False)
            hist.append(lastsc)
        t_sc.append(lastsc)
    t_idx = [P.dma('sp', idxt[:, e, :], lists[e].rearrange("(p b) o -> p (b o)", p=128), deps=[t_sc[e]]) for e in range(NE)]
    a = P.cp(idxf[:], idxt[:], deps=t_idx)
    a2 = P.ts(v2[:], idxf[:], 8192.0, None, ALU.is_lt, deps=[a])
    a3 = P.tt(idxf[:], idxf[:], v2[:], ALU.mult, deps=[a2])
    a4 = P.ts(v2[:], v2[:], -1.0, -1.0, ALU.add, ALU.mult, deps=[a3])
    a5 = P.ts(v2[:], v2[:], dum[:, 0:1], None, ALU.mult, deps=[a4, t_c[2]])
    a6 = P.tt(idxf[:], idxf[:], v2[:], ALU.add, deps=[a5])
    t_isc_l = []
    for c4 in range(4):
        a7 = P.ts(iscf[:], idxf[:], 4.0, float(c4), ALU.mult, ALU.add, deps=[a6] + t_isc_l)
        t_isc_l.append(P.cp(isc[:, c4, :, :], iscf[:], deps=[a7]))
    t_isc = t_isc_l[-1]
    y4 = y_o.rearrange("t (c n) -> (t c) n", n=512)
    issue_w1(); issue_w1(); issue_w2(); issue_w2()

    Xs_free = [[] for _ in range(3)]; Yst_free = [list(t_zero) if False else [] for _ in range(4)]
    XeT_free = []; actT_free = []; tmp_free = []; b2_free = []; gsl_free = []
    prev_scatter = list(t_zero_last) + t_zero
    tfree = [[], []]
    for e in range(NE):
        t_b2 = P.dma('sp', b2[:], b2_d[:, e, :], deps=b2_free, slot='b2')
        exp_scatter = []
        for sc in range(nsc):
            blk0 = sc * NBS
            t_g = None
            for bl in range(NBS):
                t_g = _ind(P, 'g', gsl[:, blk0 + bl, :], G_d[:, :], idxt[:, e, blk0 + bl:blk0 + bl + 1], [t_idx[e]] + gsl_free, 'gg', bounds_check=8191, oob_is_err=False)
            gsl_free = []
            xe_t = [[None] * NBS for _ in range(16)]
            lasttr = None
            for bl in range(NBS):
                r = bl % 3
                tg = _ind(P, 'g', Xs[r][:], h_d[:, :], idxt[:, e, blk0 + bl:blk0 + bl + 1], [t_idx[e]] + Xs_free[r], 'xg%d' % r, bounds_check=8191, oob_is_err=False)
                for q in range(4):
                    ti = (bl * 4 + q) % 2
                    for k4 in range(4):
                        kc = q * 4 + k4
                        lasttr = P.tr(tbank[ti][:, k4 * 128:(k4 + 1) * 128], Xs[r][:, kc * 128:(kc + 1) * 128], ident[:], deps=[tg, t_c[3]] + (tfree[ti] if k4 == 0 else []))
                    ev = P.cp(XeT[:, q * 4:(q + 1) * 4, bl * 128:(bl + 1) * 128], tbank[ti][:, 0:512].rearrange("p (k n) -> p k n", k=4), deps=[lasttr] + XeT_free,
                              eng=('act' if q % 2 == 0 else 'dve'))
                    tfree[ti] = [ev]
                    for k4 in range(4):
                        xe_t[q * 4 + k4][bl] = ev
                Xs_free[r] = [lasttr]
            XeT_free = []
            act_t = [[None] * len(SUBS) for _ in range(16)]
            lastw1 = None
            for ic in range(16):
                tw, wi = w1_tok[(e, sc, ic)]
                for si, (s0, sn) in enumerate(SUBS):
                    bG, dG = alloc(); bL, dL = alloc()
                    xdeps = [xe_t[kc_][bb] for kc_ in range(16) for bb in range(s0 // 128, (s0 + sn) // 128)]
                    for kc in range(16):
                        P.mm(banks[bG][:, 0:sn], w1b[wi][:, kc, 0:256:2], XeT[:, kc, s0:s0 + sn], start=(kc == 0), stop=(kc == 15), deps=(dG + [tw] + xdeps if kc == 0 else []))
                    mg = ('c', 'pe', len(P.ops['pe']) - 1)
                    for kc in range(16):
                        lastw1 = P.mm(banks[bL][:, 0:sn], w1b[wi][:, kc, 1:256:2], XeT[:, kc, s0:s0 + sn], start=(kc == 0), stop=(kc == 15), deps=(dL if kc == 0 else []))
                    e1 = P.ts(gc[:, 0:sn], banks[bG][:, 0:sn], b1g[:, e, ic:ic + 1], 7.0, ALU.add, ALU.min, deps=[mg] + tmp_free + t_c)
                    e2 = P.ts(lc[:, 0:sn], banks[bL][:, 0:sn], b1l[:, e, ic:ic + 1], 7.0, ALU.add, ALU.min, deps=[lastw1] + tmp_free)
                    bfree[bG] = [e1]; bfree[bL] = [e2]
                    e3 = P.act(sgm[:, 0:sn], gc[:, 0:sn], AF.Sigmoid, scale=1.702, deps=[e1] + tmp_free)
                    e4 = P.ts(lc[:, 0:sn], lc[:, 0:sn], -7.0, 1.0, ALU.max, ALU.add, deps=[e2])
                    e5 = P.tt(gc[:, 0:sn], gc[:, 0:sn], sgm[:, 0:sn], ALU.mult, deps=[e3, e1])
                    e6 = P.tt(actT[:, ic, s0:s0 + sn], gc[:, 0:sn], lc[:, 0:sn], ALU.mult, deps=[e5, e4] + actT_free)
                    tmp_free = [e6]
                    act_t[ic][si] = e6
                w1_free[wi] = lastw1
                issue_w1()
            actT_free = []
            XeT_free = [lastw1]
            lastw2 = None
            yk = 0
            for dmc in range(4):
                tw, wi = w2_tok[(e, sc, dmc)]
                for bl in range(NBS):
                    si = [i for i, (s0, sn) in enumerate(SUBS) if s0 <= bl * 128 < s0 + sn][0]
                    bk, dps = alloc()
                    for ic in range(16):
                        P.mm(banks[bk][:, 0:512], actT[:, ic, bl * 128:(bl + 1) * 128], w2b[wi][:, ic, :], start=(ic == 0), stop=False,
                             deps=(dps + [tw] if ic == 0 else []) + [act_t[ic][si]])
                    lastw2 = P.mm(banks[bk][:, 0:512], ones1[:, :], b2[:, dmc * 512:(dmc + 1) * 512], start=False, stop=True, deps=[t_o1, t_b2])
                    r = yk % 4; yk += 1
                    ev = P.act(Yst[r][:], banks[bk][:, 0:512], AF.Copy, scale=gsl[:, blk0 + bl, e:e + 1], deps=[lastw2, t_g] + Yst_free[r] + zt)
                    bfree[bk] = [ev]
                    sct = _ind(P, 's', y4, Yst[r][:], isc[:, dmc, e, blk0 + bl:blk0 + bl + 1], [ev, t_isc] + prev_scatter, 'ys%d' % r, compute_op=ALU.add)
                    Yst_free[r] = [sct]
                    exp_scatter.append(sct)
                w2_free[wi] = lastw2
                issue_w2()
            actT_free = [lastw2]
            gsl_free = [lastw2]
            b2_free = [lastw2]
            prev_scatter = prev_scatter if sc < nsc - 1 else []
        prev_scatter = exp_scatter[-4:] + [t for t in exp_scatter]
    for t in prev_scatter:
        P.out_toks.append(t)
    return P

def prep_C3(core, h2all, pm_all, cnt_all, G_all, w1_l, b1_l, w2_l, b2_l, NE=4):
    e0 = core * NE
    pm = np.concatenate([pm_all[s][:, :, e0:e0 + NE] for s in range(8)], axis=1)
    cnt = np.stack([np.tile(cnt_all[s][None, e0:e0 + NE], (128, 1)) for s in range(8)], axis=1)
    Grows = np.concatenate([G_all[s][:, :, e0:e0 + NE].transpose(1, 0, 2).reshape(1024, NE) for s in range(8)], axis=0)
    tid = (np.arange(64, dtype=np.int32)[None, :] * 128 + np.arange(128, dtype=np.int32)[:, None]).astype(np.int32)
    b1 = b1_l[e0:e0 + NE]
    b1g = b1[:, 0::2].reshape(NE, 16, 128).transpose(2, 0, 1); b1l = b1[:, 1::2].reshape(NE, 16, 128).transpose(2, 0, 1)
    return dict(h2all=h2all, pm=np.ascontiguousarray(pm), cnt=np.ascontiguousarray(cnt.astype(np.float32)), Grows=np.ascontiguousarray(Grows),
                tid=tid, dumrow=(8192 + np.arange(128, dtype=np.float32))[:, None], ident=np.eye(128, dtype=np.float32).astype(ml_dtypes.bfloat16),
                w1=np.ascontiguousarray(w1_l[e0:e0 + NE]), w2=np.ascontiguousarray(w2_l[e0:e0 + NE]),
                b1g=np.ascontiguousarray(b1g), b1l=np.ascontiguousarray(b1l), b2=np.ascontiguousarray(b2_l[None, e0:e0 + NE]))


def build_D(final=False):
    P = Prog()
    x_d = P.din('x1T', [2048, 1024]); y_d = P.din('yparts', [8, 2048, 1024]); modv_d = P.din('modv', [128, 6, 16]); fg_d = P.din('fg', [128, 16])
    o_d = P.dout('x2T', [2048, 1024])
    x = P.sb('x', [128, 16, 1024]); mods = P.sb('mods', [128, 6, 16]); fg = P.sb('fg', [128, 16])
    yb = [[P.sb('yb%d_%d' % (i, c), [128, 1024]) for c in range(8)] for i in range(2)]
    sq = [P.sb('sq%d' % i, [128, 1024], BF16) for i in range(2)]; ones = P.sb('ones', [128, 128], BF16); rstd = P.sb('rstd', [128, 1024])
    ps = [P.ps('b%d' % i) for i in range(2)]
    t_m = P.dma('act', mods[:], modv_d[:, :, :]); t_fg = P.dma('act', fg[:], fg_d[:, :])
    t_x = [P.dma('act', x[:, kc * 4:(kc + 1) * 4, :], x_d.rearrange("(kc p) t -> p kc t", p=128)[:, kc * 4:(kc + 1) * 4, :]) for kc in range(4)]
    t_ones = P.memset(ones[:], 1.0)
    yfree = [[None] * 8, [None] * 8]
    sq_free = [None, None]
    t_x2 = []
    lastN = [None, None]
    for kc in range(16):
        i = kc % 2
        lt = [P.dma('sp', yb[i][c][:], y_d[c, kc * 128:(kc + 1) * 128, :], deps=[yfree[i][c]], slot='y%d_%d' % (i, c)) for c in range(8)]
        a01 = P.tt(yb[i][0][:], yb[i][0][:], yb[i][1][:], ALU.add, deps=[lt[0], lt[1]])
        a23 = P.tt(yb[i][2][:], yb[i][2][:], yb[i][3][:], ALU.add, deps=[lt[2], lt[3]], eng='pool')
        a45 = P.tt(yb[i][4][:], yb[i][4][:], yb[i][5][:], ALU.add, deps=[lt[4], lt[5]])
        a67 = P.tt(yb[i][6][:], yb[i][6][:], yb[i][7][:], ALU.add, deps=[lt[6], lt[7]], eng='pool')
        b0 = P.tt(yb[i][0][:], yb[i][0][:], yb[i][2][:], ALU.add, deps=[a01, a23])
        b1 = P.tt(yb[i][4][:], yb[i][4][:], yb[i][6][:], ALU.add, deps=[a45, a67], eng='pool')
        c0 = P.tt(yb[i][0][:], yb[i][0][:], yb[i][4][:], ALU.add, deps=[b0, b1])
        xx = P.stt(x[:, kc, :], yb[i][0][:], mods[:, 5, kc:kc + 1], x[:, kc, :], ALU.mult, ALU.add, deps=[c0, t_m, t_x[kc // 4]])
        for c in range(8):
            yfree[i][c] = xx
        t_x2.append(xx)
        if final:
            s_ = P.act(sq[i][:], x[:, kc, :], AF.Square, deps=[xx, sq_free[i]])
            for hf in range(2):
                lastN[hf] = P.mm(ps[hf][:], ones[:], sq[i][:, hf * 512:(hf + 1) * 512], start=(kc == 0), stop=(kc == 15), deps=[s_, t_ones])
            sq_free[i] = lastN[1]
    if final:
        t_r = []
        for hf in range(2):
            cs = slice(hf * 512, (hf + 1) * 512)
            a = P.act(rstd[:, cs], ps[hf][:], AF.Sqrt, bias=1e-6, scale=1.0 / 2048, deps=[lastN[hf]])
            t_r.append(P.recip(rstd[:, cs], rstd[:, cs], deps=[a]))
        t_f = []
        for kc in range(16):
            t_f.append(P.stt(x[:, kc, :], x[:, kc, :], fg[:, kc:kc + 1], rstd[:], ALU.mult, ALU.mult, deps=t_r + [t_fg, t_x2[kc]]))
        t_x2 = t_f
    for kc in range(4):
        P.dma('sp', o_d.rearrange("(kc p) t -> p kc t", p=128)[:, kc * 4:(kc + 1) * 4, :], x[:, kc * 4:(kc + 1) * 4, :], deps=t_x2[kc * 4:(kc + 1) * 4], is_out=True)
    return P


def _np(a):
    return np.asarray(a)

def _to_bf16(a):
    a = np.asarray(a)
    if a.dtype == ml_dtypes.bfloat16:
        return a
    if a.dtype.itemsize == 2:
        return a.view(ml_dtypes.bfloat16)
    raise ValueError('unexpected dtype %s' % a.dtype)

def kernel(x, c, positions, ada_w, ada_b, norm1_g, norm2_g, w_in, conv_w, conv_b, lru_wa, lru_ba, lru_wx, lru_bx,
           lru_lambda, attn_out_g, lru_out_g, w_out, router_w, router_b, w1, b1, w2, b2, final_g):
    x = _np(x); c = _np(c); positions = _np(positions); ada_w = _np(ada_w); ada_b = _np(ada_b)
    norm1_g = _np(norm1_g); norm2_g = _np(norm2_g); w_in = _np(w_in); conv_w = _np(conv_w); conv_b = _np(conv_b)
    lru_wa = _np(lru_wa); lru_ba = _np(lru_ba); lru_wx = _np(lru_wx); lru_bx = _np(lru_bx); lru_lambda = _np(lru_lambda)
    attn_out_g = _np(attn_out_g); lru_out_g = _np(lru_out_g); w_out = _np(w_out); router_w = _np(router_w); router_b = _np(router_b)
    w1 = _np(w1); b1 = _np(b1); w2 = _np(w2); b2 = _np(b2); final_g = _np(final_g)
    NC = 8
    ims = []
    for core in range(NC):
        l = core // 4; cs = (core % 4) * 3072
        ims.append({'c': np.ascontiguousarray(c.reshape(16, 128).T), 'w': np.ascontiguousarray(ada_w[l][:, cs:cs + 3072]),
                    'b': np.ascontiguousarray(ada_b[l][None, cs:cs + 3072])})
    res = build_M().run(ims)
    mod = np.concatenate([np.asarray(r['mod'])[0] for r in res.results]).reshape(2, 12288)
    xT = [np.ascontiguousarray(x[0, k * 1024:(k + 1) * 1024, :].T) for k in range(NC)]
    fgl = np.ascontiguousarray(final_g.reshape(16, 128).T)
    for l in range(2):
        modv = np.ascontiguousarray(mod[l].reshape(6, 16, 128).transpose(2, 0, 1))
        w_in_l = np.ascontiguousarray(w_in[l])
        cA = consts_A()
        ims = []
        for k in range(NC):
            d = dict(xT=xT[k], modv=modv, gn=np.ascontiguousarray(norm1_g[l].reshape(16, 128).T), w=w_in_l,
                     pos=np.ascontiguousarray(positions[:, k * 1024:(k + 1) * 1024]))
            d.update(cA)
            ims.append(d)
        rA = build_A().run(ims).results
        qT = np.concatenate([_to_bf16(r['qT']) for r in rA], axis=1)
        kT = np.concatenate([_to_bf16(r['kT']) for r in rA], axis=1)
        vT = np.concatenate([_to_bf16(r['vT']) for r in rA], axis=1)
        xrT = np.concatenate([np.asarray(r['xrT']) for r in rA], axis=1)
        grT = [np.asarray(r['grT']) for r in rA]
        ims = []
        for k in range(NC):
            m = prep_B_attn(k, qT, kT, vT)
            m.update(prep_B_lru(k, xrT, conv_w[l], conv_b[l], lru_wa[l], lru_ba[l], lru_wx[l], lru_bx[l], lru_lambda[l], positions))
            ims.append(m)
        rB = build_B(True).run(ims).results
        hloc = [np.asarray(r['hloc']) for r in rB]; pprod = [np.asarray(r['pprod']) for r in rB]
        wo_l = np.ascontiguousarray(w_out[l])
        ims = [prep_C1(k, xT[k], np.asarray(rB[k]['attnT']), hloc, pprod, grT[k], mod[l], norm2_g[l], attn_out_g[l], lru_out_g[l],
                       wo_l, router_w[l], router_b[l]) for k in range(NC)]
        rC = build_C1().run(ims).results
        x1T = [np.asarray(r['x1T']) for r in rC]
        h2all = np.ascontiguousarray(np.concatenate([_to_bf16(r['h2T']).T for r in rC], axis=0))
        pm = [np.asarray(r['pm']) for r in rC]; G = [np.asarray(r['G']) for r in rC]
        cnt_all = [np.asarray(r['cnt'])[0] for r in rC]
        ims = [prep_C3(k, h2all, pm, cnt_all, G, w1[l], b1[l], w2[l], b2[l]) for k in range(NC)]
        rE = build_C3().run(ims).results
        yp = [np.asarray(r['ypart']) for r in rE]
        final = (l == 1)
        ims = [dict(x1T=x1T[k], yparts=np.ascontiguousarray(np.stack([yp[cc][k * 1024:(k + 1) * 1024].T for cc in range(NC)])), modv=modv, fg=fgl)
               for k in range(NC)]
        rD = build_D(final=final).run(ims).results
        xT = [np.asarray(r['x2T']) for r in rD]
    out = np.concatenate([t.T for t in xT], axis=0)[None].astype(np.float32)
    return out
```

```python
import contextlib
import numpy as np
import concourse.bass as bass
import concourse.mybir as mybir
from concourse.bass_utils import run_bass_kernel_spmd

F32 = mybir.dt.float32
BF16 = mybir.dt.bfloat16
I32 = mybir.dt.int32
AF = mybir.ActivationFunctionType
ALU = mybir.AluOpType


class Prog:
    ENG = ['pe', 'act', 'dve', 'pool', 'sp']

    def __init__(self):
        self.nc = bass.Bass("TRN2", target_bir_lowering=False)
        self.es = contextlib.ExitStack()
        self.ops = {e: [] for e in self.ENG}
        self.dma_slots = {}
        self.out_toks = []

    def din(self, name, shape, dt=F32):
        return self.nc.dram_tensor(name, list(shape), dt, kind="ExternalInput").ap()

    def dout(self, name, shape, dt=F32):
        return self.nc.dram_tensor(name, list(shape), dt, kind="ExternalOutput").ap()

    def sb(self, name, shape, dt=F32):
        return self.es.enter_context(self.nc.sbuf_tensor('sb_' + name, list(shape), dt))

    def ps(self, name, shape=(128, 512), dt=F32):
        return self.es.enter_context(self.nc.psum_tensor('ps_' + name, list(shape), dt))

    def op(self, eng, fn, deps=()):
        o = dict(fn=fn, deps=[d for d in deps if d is not None], sig=False, dma=None)
        self.ops[eng].append(o)
        return ('c', eng, len(self.ops[eng]) - 1)

    def dma(self, eng, out, in_, deps=(), slot=None, is_out=False, **kw):
        if slot is None:
            slot = '_a%d' % len(self.dma_slots)
        cnt = self.dma_slots.get(slot, 0) + 1
        self.dma_slots[slot] = cnt
        o = dict(fn=lambda e: e.dma_start(out=out, in_=in_, **kw),
                 deps=[d for d in deps if d is not None], sig=False, dma=(slot, cnt))
        self.ops[eng].append(o)
        tok = ('d', slot, cnt)
        if is_out:
            self.out_toks.append(tok)
        return tok

    def mm(self, out, lhsT, rhs, start=True, stop=True, deps=()):
        return self.op('pe', lambda e: e.matmul(out, lhsT, rhs, start=start, stop=stop), deps)

    def tr(self, out, in_, ident, deps=()):
        return self.op('pe', lambda e: e.transpose(out, in_, ident), deps)

    def act(self, out, in_, func, bias=None, scale=None, deps=(), accum_out=None):
        kw = {}
        if bias is not None:
            kw['bias'] = bias
        if scale is not None:
            kw['scale'] = scale
        if accum_out is not None:
            kw['accum_out'] = accum_out
        return self.op('act', lambda e: e.activation(out, in_, func, **kw), deps)

    def tt(self, out, in0, in1, op, deps=(), eng='dve'):
        return self.op(eng, lambda e: e.tensor_tensor(out, in0, in1, op), deps)

    def ts(self, out, in0, s1, s2, op0, op1=None, deps=(), eng='dve'):
        if op1 is None:
            return self.op(eng, lambda e: e.tensor_scalar(out, in0, s1, None, op0), deps)
        return self.op(eng, lambda e: e.tensor_scalar(out, in0, s1, s2, op0, op1), deps)

    def stt(self, out, in0, scalar, in1, op0, op1, deps=()):
        return self.op('dve', lambda e: e.scalar_tensor_tensor(out, in0, scalar, in1, op0, op1), deps)

    def cp(self, out, in_, deps=(), eng='dve'):
        if eng == 'act':
            return self.op('act', lambda e: e.copy(out, in_), deps)
        return self.op(eng, lambda e: e.tensor_copy(out, in_), deps)

    def recip(self, out, in_, deps=()):
        return self.op('dve', lambda e: e.reciprocal(out, in_), deps)

    def memset(self, ap, val, deps=(), eng='dve'):
        return self.op(eng, lambda e: e.memset(ap, val), deps)

    def build(self):
        nc = self.nc
        for eng in self.ENG:
            for o in self.ops[eng]:
                for d in o['deps']:
                    if d[0] == 'c':
                        self.ops[d[1]][d[2]]['sig'] = True
        for eng in self.ENG:
            c = 0
            for o in self.ops[eng]:
                if o['sig'] and o['dma'] is None:
                    c += 1
                    o['cnt'] = c
        if self.out_toks:
            self.ops['sp'].append(dict(fn=None, deps=list(self.out_toks), sig=False, dma=None))
        self.check_deadlock()
        self.sem = {e: self.es.enter_context(nc.semaphore('s_' + e)) for e in self.ENG}
        self.dsem = {s: self.es.enter_context(nc.semaphore('d_' + s)) for s in self.dma_slots}
        block = self.es.enter_context(nc.Block())

        def replay(name, e):
            waited = {}
            for o in self.ops[name]:
                for d in o['deps']:
                    if d[0] == 'c':
                        key = ('c', d[1]); sem = self.sem[d[1]]; val = self.ops[d[1]][d[2]]['cnt']
                    else:
                        key = ('d', d[1]); sem = self.dsem[d[1]]; val = 16 * d[2]
                    if waited.get(key, 0) < val:
                        e.wait_ge(sem, val)
                        waited[key] = val
                if o['fn'] is None:
                    continue
                try:
                    ins = o['fn'](e)
                except Exception:
                    print('EMIT FAIL engine', name, 'op index', self.ops[name].index(o), 'dma', o['dma'])
                    raise
                if o['dma'] is not None:
                    ins.then_inc(self.dsem[o['dma'][0]], 16)
                elif o['sig']:
                    ins.then_inc(self.sem[name], 1)

        @block.tensor
        def _(e):
            replay('pe', e)

        @block.scalar
        def _(e):
            replay('act', e)

        @block.vector
        def _(e):
            replay('dve', e)

        @block.gpsimd
        def _(e):
            replay('pool', e)

        @block.sync
        def _(e):
            replay('sp', e)

        self.es.close()
        return nc

    def check_deadlock(self):
        ptr = {e: 0 for e in self.ENG}
        done_c = {e: -1 for e in self.ENG}
        dcnt = {s: 0 for s in self.dma_slots}
        total = sum(len(v) for v in self.ops.values())
        ndone = 0
        while ndone < total:
            prog = False
            for e in self.ENG:
                while ptr[e] < len(self.ops[e]):
                    o = self.ops[e][ptr[e]]
                    ok = True
                    for d in o['deps']:
                        if d[0] == 'c':
                            if done_c[d[1]] < d[2]:
                                ok = False; break
                        else:
                            if dcnt[d[1]] < d[2]:
                                ok = False; break
                    if not ok:
                        break
                    if o['dma'] is not None:
                        dcnt[o['dma'][0]] += 1
                    done_c[e] = ptr[e]
                    ptr[e] += 1; ndone += 1; prog = True
            if not prog:
                msg = []
                for e in self.ENG:
                    if ptr[e] < len(self.ops[e]):
                        o = self.ops[e][ptr[e]]
                        msg.append('%s blocked at op %d deps=%s' % (e, ptr[e], o['deps']))
                raise RuntimeError('DEADLOCK: ' + ' | '.join(msg))

    def run(self, in_maps, trace=False):
        nc = self.build()
        n = len(in_maps)
        res = run_bass_kernel_spmd(nc, in_maps, core_ids=list(range(n)), trace=trace)
        return res

import math
import ml_dtypes


def build_M():
    P = Prog()
    c_in = P.din('c', [128, 16])
    w_in = P.din('w', [2048, 3072])
    b_in = P.din('b', [1, 3072])
    o = P.dout('mod', [1, 3072])
    cs = P.sb('cs', [128, 16]); cond = P.sb('cond', [128, 16])
    wb = [P.sb('wb%d' % i, [128, 16, 512]) for i in range(2)]
    bb = P.sb('bb', [1, 3072]); ob = P.sb('ob', [1, 3072])
    ps = [P.ps('ps%d' % i) for i in range(2)]
    t_c = P.dma('sp', cs[:], c_in[:, :])
    t_b = P.dma('sp', bb[:], b_in[:, :])
    t_cond = P.act(cond[:], cs[:], AF.Silu, deps=[t_c])
    wv = w_in.rearrange("(kc p) n -> p kc n", p=128)
    ev = [None, None]
    outs = []
    for j in range(6):
        t_w = P.dma('sp' if j % 2 == 0 else 'act', wb[j % 2][:], wv[:, :, j * 512:(j + 1) * 512], deps=[ev[j % 2]], slot='w%d' % (j % 2))
        last = None
        for kc in range(16):
            last = P.mm(ps[j % 2][0:1, :], cond[:, kc:kc + 1], wb[j % 2][:, kc, :], start=(kc == 0), stop=(kc == 15),
                        deps=[t_w, t_cond, ev[j % 2]] if kc == 0 else [])
        ev[j % 2] = P.tt(ob[:, j * 512:(j + 1) * 512], ps[j % 2][0:1, :], bb[:, j * 512:(j + 1) * 512], ALU.add, deps=[last, t_b])
        outs.append(ev[j % 2])
    P.dma('sp', o[:, :], ob[:], deps=outs, is_out=True)
    return P


TWO_PI = 2 * math.pi
MAGIC = 12582912.0

def consts_A():
    invf = (10000.0 ** (-np.arange(0, 128, 2, dtype=np.float32) / 128)).astype(np.float32)
    invf = np.concatenate([invf, invf])[None, :].astype(np.float32)
    sign = np.concatenate([-np.ones(64), np.ones(64)]).astype(np.float32)[:, None]
    swap = np.zeros((128, 128), np.float32)
    for e in range(128):
        swap[(e + 64) % 128, e] = 1.0
    return dict(invf=invf, sign=sign, swap=swap)

def build_A(cc_list=None):
    P = Prog()
    xT = P.din('xT', [2048, 1024]); modv = P.din('modv', [128, 6, 16]); g1n = P.din('gn', [128, 16])
    w = P.din('w', [2048, 7680]); pos = P.din('pos', [1, 1024], I32)
    invf_d = P.din('invf', [1, 128]); sign_d = P.din('sign', [128, 1]); swap_d = P.din('swap', [128, 128])
    qT = P.dout('qT', [1536, 1024], BF16); kT = P.dout('kT', [1536, 1024], BF16); vT = P.dout('vT', [1536, 1024], BF16)
    xrT = P.dout('xrT', [1536, 1024]); grT = P.dout('grT', [1536, 1024])

    x = P.sb('x', [128, 16, 1024]); h = P.sb('h', [128, 16, 1024], BF16); sq = h
    mods = P.sb('mods', [128, 6, 16]); gn = P.sb('gns', [128, 16]); G = P.sb('G', [128, 16])
    ones = P.sb('ones', [128, 128], BF16); swp = P.sb('swp', [128, 128], BF16)
    invf = P.sb('invfs', [1, 128]); sign = P.sb('signs', [128, 1]); posi = P.sb('posi', [1, 1024], I32); posf = P.sb('posf', [1, 1024])
    rstd = P.sb('rstd', [128, 1024]); tmp = [P.sb('tmp%d' % i, [128, 1024]) for i in range(2)]
    cosT = P.sb('cosT', [128, 1024]); sinT = P.sb('sinT', [128, 1024])
    r1 = tmp[0]; r2 = tmp[1]
    wb = [P.sb('wb%d' % i, [128, 16, 512], BF16) for i in range(2)]
    tb = [P.sb('tb%d' % i, [128, 512], BF16) for i in range(2)]
    ta = [P.sb('ta%d' % i, [128, 512]) for i in range(2)]
    tb2 = [P.sb('tbb%d' % i, [128, 512]) for i in range(2)]
    ost = [P.sb('ost%d' % i, [128, 1024], BF16) for i in range(2)]
    ost32 = [P.sb('ost32_%d' % i, [128, 1024]) for i in range(2)]
    pm = [P.ps('pm%d' % i) for i in range(4)]
    pr = [P.ps('pr%d' % i) for i in range(2)]
    pz = [P.ps('pz%d' % i) for i in range(2)]

    t_x = [P.dma('sp', x[:, kc * 4:(kc + 1) * 4, :], xT.rearrange("(kc p) t -> p kc t", p=128)[:, kc * 4:(kc + 1) * 4, :]) for kc in range(4)]
    t_m = P.dma('act', mods[:], modv[:, :, :]); t_g = P.dma('act', gn[:], g1n[:, :])
    t_if = P.dma('act', invf[:], invf_d[:, :]); t_sg = P.dma('act', sign[:], sign_d[:, :])
    t_pos = P.dma('act', posi[:], pos[:, :])
    t_sw = P.dma('pool', swp[:], swap_d[:, :])
    t_ones = P.memset(ones[:], 1.0)
    wv = w.rearrange("(kc p) n -> p kc n", p=128)
    NWB = 15
    t_w = [None] * NWB
    wfree = [None, None]
    def load_w(j, deps):
        t_w[j] = P.dma('pool', wb[j % 2][:], wv[:, :, j * 512:(j + 1) * 512], deps=deps, slot='w%d' % (j % 2))
    if cc_list is None:
        load_w(0, []); load_w(1, [])
    else:
        for jj in sorted(set(c // 4 for c in cc_list)):
            load_w(jj, [wfree[jj % 2]])
    t_G = P.stt(G[:], mods[:, 1, :], 1.0, gn[:], ALU.add, ALU.mult, deps=[t_m, t_g])
    t_pf = P.cp(posf[:], posi[:], deps=[t_pos])
    t_ang = []
    for hf in range(2):
        t_ang.append(P.mm(pz[hf][:], invf[:], posf[:, hf * 512:(hf + 1) * 512], deps=[t_if, t_pf]))
    def reduce_sin(dst, off, deps_extra):
        toks = []
        for hf in range(2):
            sl = slice(hf * 512, (hf + 1) * 512)
            a0 = P.ts(r1[:, sl], pz[hf][:], 1.0 / TWO_PI, off / TWO_PI, ALU.mult, ALU.add, deps=[t_ang[hf]] + deps_extra)
            a = P.ts(r1[:, sl], r1[:, sl], MAGIC, None, ALU.add, deps=[a0])
            b = P.ts(r1[:, sl], r1[:, sl], MAGIC, None, ALU.subtract, deps=[a])
            c = P.stt(r2[:, sl], r1[:, sl], -TWO_PI, pz[hf][:], ALU.mult, ALU.add, deps=[b])
            d = P.ts(r2[:, sl], r2[:, sl], off + math.pi, TWO_PI - 1e-5, ALU.add, ALU.min, deps=[c])
            e = P.ts(r2[:, sl], r2[:, sl], 1e-5, -math.pi, ALU.max, ALU.add, deps=[d])
            toks.append(e)
        return toks
    tk = reduce_sin(sinT, 0.0, [])
    t_sin = P.act(sinT[:], r2[:], AF.Sin, scale=sign[:], deps=tk + [t_sg])
    tk = reduce_sin(cosT, math.pi / 2, [t_sin])
    t_cos = P.act(cosT[:], r2[:], AF.Sin, deps=tk)
    t_sq = [P.act(sq[:, kc, :], x[:, kc, :], AF.Square, deps=[t_x[kc // 4]]) for kc in range(16)]
    t_ss = []
    for hf in range(2):
        last = None
        for kc in range(16):
            last = P.mm(pm[hf][:], ones[:], sq[:, kc, hf * 512:(hf + 1) * 512], start=(kc == 0), stop=(kc == 15), deps=[t_sq[kc], t_ones])
        t_ss.append(last)
    t_rs = []
    for hf in range(2):
        sl = slice(hf * 512, (hf + 1) * 512)
        a = P.act(rstd[:, sl], pm[hf][:], AF.Sqrt, bias=1e-6, scale=1.0 / 2048, deps=[t_ss[hf]])
        t_rs.append(P.recip(rstd[:, sl], rstd[:, sl], deps=[a]))
    t_h = []
    tfree = [t_cos, t_cos]
    for kc in range(16):
        a = P.tt(tmp[kc % 2][:], x[:, kc, :], rstd[:], ALU.mult, deps=t_rs + [t_x[kc // 4], tfree[kc % 2]])
        b = P.act(h[:, kc, :], tmp[kc % 2][:], AF.Identity, bias=mods[:, 0, kc:kc + 1], scale=G[:, kc:kc + 1], deps=[a, t_G])
        tfree[kc % 2] = b
        t_h.append(b)
    bank_free = [[t_rs[0]], [t_rs[1]], [], []]
    tb_free = [None, None]; pr_free = [None, None]; ta_free = [None, None]
    ost_free = [None, None]; ost32_free = [None, None]
    pending = None
    u = 0
    outs = [qT, kT, vT, xrT, grT]
    for cc in (cc_list if cc_list is not None else range(60)):
        j = cc // 4
        sec = cc // 12; row0 = (cc % 12) * 128
        half_toks = []
        for hf in range(2):
            sl = slice(hf * 512, (hf + 1) * 512)
            mb = u % 4
            last = None
            for kc in range(16):
                deps = [t_h[kc]]
                if kc == 0:
                    deps += [t_w[j]] + bank_free[mb]
                last = P.mm(pm[mb][:], wb[j % 2][:, kc, (cc % 4) * 128:(cc % 4 + 1) * 128], h[:, kc, sl], start=(kc == 0), stop=(kc == 15), deps=deps)
            if cc % 4 == 3 and hf == 1:
                if j + 2 < NWB and cc_list is None:
                    load_w(j + 2, [last])
            if sec < 2:
                i2 = u % 2
                e1 = P.cp(tb[i2][:], pm[mb][:], deps=[last, tb_free[i2]], eng='act')
                e3 = P.tt(ta[i2][:], pm[mb][:], cosT[:, sl], ALU.mult, deps=[last, t_cos, ta_free[i2], e1])
                bank_free[mb] = [e1, e3]
                if pending is not None:
                    pending()
                def mk(i2=i2, e1=e1, e3=e3, sl=sl, slot=cc % 2, hf=hf):
                    def f():
                        e2 = P.mm(pr[i2][:], swp[:], tb[i2][:], deps=[e1, t_sw, pr_free[i2]])
                        tb_free[i2] = e2
                        e4 = P.tt(tb2[i2][:], pr[i2][:], sinT[:, sl], ALU.mult, deps=[e2, t_sin])
                        pr_free[i2] = e4
                        e5 = P.tt(ost[slot][:, sl], ta[i2][:], tb2[i2][:], ALU.add, deps=[e3, e4, ost_free[slot]])
                        ta_free[i2] = e5
                        return e5
                    return f
                g = mk()
                res = {}
                def pend(g=g, res=res):
                    res['t'] = g()
                pending = pend
                half_toks.append(res)
            else:
                if pending is not None:
                    pending(); pending = None
                if sec == 2:
                    slot = cc % 2
                    e = P.cp(ost[slot][:, sl], pm[mb][:], deps=[last, ost_free[slot]], eng='act')
                else:
                    slot = cc % 2
                    e = P.cp(ost32[slot][:, sl], pm[mb][:], deps=[last, ost32_free[slot]], eng=('act' if hf == 0 else 'dve'))
                bank_free[mb] = [e]
                half_toks.append({'t': e})
            u += 1
        def mkout(cc=cc, sec=sec, row0=row0, half_toks=half_toks):
            def f():
                slot = cc % 2
                deps = [r['t'] for r in half_toks]
                if sec < 3:
                    t = P.dma('sp', outs[sec][row0:row0 + 128, :], ost[slot][:], deps=deps, slot='o%d' % slot, is_out=True)
                    ost_free[slot] = t
                else:
                    t = P.dma('sp', outs[sec][row0:row0 + 128, :], ost32[slot][:], deps=deps, slot='o32_%d' % slot, is_out=True)
                    ost32_free[slot] = t
            return f
        if sec < 2:
            prev_pending = pending
            outf = mkout()
            def pend2(prev_pending=prev_pending, outf=outf):
                prev_pending(); outf()
            pending = pend2
        else:
            mkout()()
    if pending is not None:
        pending()
    return P

def prep_A(core, x, mod_l, norm_g, w_in_l, positions):
    T0 = core * 1024
    d = dict(xT=np.ascontiguousarray(x[0, T0:T0 + 1024, :].T),
             modv=np.ascontiguousarray(mod_l.reshape(6, 16, 128).transpose(2, 0, 1)),
             gn=np.ascontiguousarray(norm_g.reshape(16, 128).T),
             w=w_in_l, pos=np.ascontiguousarray(positions[:, T0:T0 + 1024]))
    d.update(consts_A())
    return d


NEG = -1e30
NT = 53

def attn_tiles():
    groups = []
    for gi in range(4):
        g = []
        for i in (2 * gi, 2 * gi + 1):
            bank = i // 4; c0 = (i % 4) * 128
            g.append(dict(ks=2048 + 128 * (i - 1), kst=1, kp=128, qs=128 * i, qst=1, nq=128, m=i, vt=i, outs=[(bank, c0, 1, 128, 0)]))
            g.append(dict(ks=2048 + 128 * i, kst=1, kp=128, qs=128 * i, qst=1, nq=128, m=11, vt=i + 1, outs=[(bank, c0, 1, 128, 0)]))
        groups.append(g)
    for r in range(4):
        g = []
        for i in range(2):
            g.append(dict(ks=2048 + 512 * (i - 1) + r, kst=4, kp=128, qs=512 * i + r, qst=4, nq=128, m=8 + i, vt=9 + r * 3 + i, outs=[(i, r, 4, 128, 0)]))
            g.append(dict(ks=2048 + 512 * i + r, kst=4, kp=128, qs=512 * i + r, qst=4, nq=128, m=11, vt=9 + r * 3 + i + 1, outs=[(i, r, 4, 128, 0)]))
        groups.append(g)
    for r4 in range(4):
        g = []
        for r in range(4 * r4, 4 * r4 + 4):
            outs = [(0, r, 16, 32, 0), (1, r, 16, 32, 32)]
            g.append(dict(ks=r, kst=16, kp=128, qs=r, qst=16, nq=64, m=10, vt=21 + 2 * r, outs=outs))
            g.append(dict(ks=2048 + r, kst=16, kp=64, qs=r, qst=16, nq=64, m=11, vt=21 + 2 * r + 1, outs=outs))
        groups.append(g)
    return groups

def sl(start, step, n):
    return slice(start, start + step * (n - 1) + 1, step)

def emit_attn(P, q_d, k_d, v_d, mask_d, ident_d, out_d, psS, psN, psD, after_head=None):
    qb = [P.sb('qb%d' % i, [128, 1024], BF16) for i in range(2)]
    kb = [P.sb('kb%d' % i, [128, 3072], BF16) for i in range(2)]
    vb = [P.sb('vb%d' % i, [128, NT, 128], BF16) for i in range(2)]
    masks = P.sb('masks', [128, 12, 128], BF16); ident = P.sb('ident', [128, 128], BF16); ones = P.sb('onesb', [128, 128], BF16)
    pb = [P.sb('pb%d' % i, [128, 512], BF16) for i in range(2)]
    rden = P.sb('rden', [128, 1024]); ao = [P.sb('ao%d' % i, [128, 1024]) for i in range(2)]
    t_mask = P.dma('act', masks[:], mask_d[:, :, :]); t_id = P.dma('act', ident[:], ident_d[:, :])
    t_ones = P.memset(ones[:], 1.0)
    groups = attn_tiles()
    scale = 1.0 / math.sqrt(128.0)
    load_tok = [None, None]; buf_free = [[], []]
    def load(h):
        b = h % 2
        t1 = P.dma('sp', qb[b][:], q_d[:, h, :], deps=buf_free[b], slot='q%d' % b)
        t2 = P.dma('sp', kb[b][:], k_d[:, h, :], deps=buf_free[b], slot='k%d' % b)
        t3 = P.dma('sp', vb[b][:], v_d[:, h, :, :], deps=buf_free[b], slot='v%d' % b)
        load_tok[b] = [t1, t2, t3]
    load(0); load(1)
    S_free = [None, None]; pb_free = [None, None]
    nd_free = []
    ao_free = [None, None]
    gcount = 0
    for h in range(12):
        b = h % 2
        first_in_bank = {('N', 0): True, ('N', 1): True, ('D', 0): True, ('D', 1): True}
        pend = None
        last_pv = None
        def do_pv(g, sb_i, t_exp):
            nonlocal last_pv
            c0 = 0
            for t in g:
                for (bank, st, step, n, pc0) in t['outs']:
                    for kind, ps, lhs in (('N', psN, vb[b][0:t['kp'], t['vt'], :]), ('D', psD, ones[0:t['kp'], :])):
                        fst = first_in_bank[(kind, bank)]
                        first_in_bank[(kind, bank)] = False
                        last_pv = P.mm(ps[bank][:, sl(st, step, n)], lhs, pb[sb_i][0:t['kp'], c0 + pc0:c0 + pc0 + n],
                                       start=fst, stop=False, deps=[t_exp, t_ones] + (nd_free if fst else []))
                c0 += t['nq']
            return last_pv
        for g in groups:
            si = gcount % 2
            c0 = 0
            last = None
            for ti, t in enumerate(g):
                deps = load_tok[b] + [S_free[si], t_mask, t_id] if ti == 0 else []
                P.mm(psS[si][0:t['kp'], c0:c0 + t['nq']], kb[b][:, sl(t['ks'], t['kst'], t['kp'])], qb[b][:, sl(t['qs'], t['qst'], t['nq'])],
                     start=True, stop=False, deps=deps)
                mslice = masks[0:t['kp'], t['m'], 0:t['nq']]
                last = P.mm(psS[si][0:t['kp'], c0:c0 + t['nq']], ident[0:t['kp'], 0:t['kp']], mslice, start=False, stop=True)
                c0 += t['nq']
            t_exp = P.act(pb[si][:, 0:c0], psS[si][:, 0:c0], AF.Exp, scale=scale, deps=[last, pb_free[si]])
            S_free[si] = t_exp
            if pend is not None:
                pg, psi, ptexp = pend
                pb_free[psi] = do_pv(pg, psi, ptexp)
            pend = (g, si, t_exp)
            gcount += 1
        pg, psi, ptexp = pend
        pb_free[psi] = do_pv(pg, psi, ptexp)
        buf_free[b] = [last_pv]
        if h + 2 < 12:
            load(h + 2)
        oi = h % 2
        evs = []
        for bank in range(2):
            cs = slice(bank * 512, (bank + 1) * 512)
            a = P.recip(rden[:, cs], psD[bank][:], deps=[last_pv])
            e = P.tt(ao[oi][:, cs], psN[bank][:], rden[:, cs], ALU.mult, deps=[a, ao_free[oi]])
            evs.append(e)
        nd_free = evs
        ao_free[oi] = P.dma('act', out_d[h * 128:(h + 1) * 128, :], ao[oi][:], deps=evs, slot='ao%d' % oi, is_out=True)
        if after_head is not None:
            after_head(h)

def build_B(do_lru=True):
    P = Prog()
    q_d = P.din('q', [128, 12, 1024], BF16); k_d = P.din('k', [128, 12, 3072], BF16); v_d = P.din('v', [128, 12, NT, 128], BF16)
    mask_d = P.din('masks', [128, 12, 128], BF16); ident_d = P.din('ident', [128, 128], BF16)
    out_d = P.dout('attnT', [1536, 1024])
    psS = [P.ps('psS%d' % i) for i in range(2)]; psN = [P.ps('psN%d' % i) for i in range(2)]; psD = [P.ps('psD%d' % i) for i in range(2)]
    blk = None
    if do_lru:
        psG = [P.ps('psG%d' % i) for i in range(2)]
        blk = emit_lru(P, psG)
    emit_attn(P, q_d, k_d, v_d, mask_d, ident_d, out_d, psS, psN, psD, after_head=blk)
    return P

def emit_lru(P, psG):
    xr_d = P.din('xr', [128, 12, 1027]); cw_d = P.din('cw', [128, 12, 4]); vec_d = P.din('vecs', [128, 4, 12])
    wa_d = P.din('wa', [128, 12, 128]); wx_d = P.din('wx', [128, 12, 128]); pos_d = P.din('pos', [1, 1024], I32)
    hl_d = P.dout('hloc', [1536, 1024]); pp_d = P.dout('pprod', [1536, 1024])
    xr = P.sb('xr', [128, 12, 1027]); cw = P.sb('cw', [128, 12, 4]); vecs = P.sb('vecs', [128, 4, 12])
    wa = P.sb('wa', [128, 12, 128], BF16); wx = P.sb('wx', [128, 12, 128], BF16)
    posi = P.sb('lposi', [1, 1024], I32); nzr = P.sb('nzr', [1, 1024]); nz = P.sb('nz', [128, 1024]); ones1 = P.sb('ones1', [1, 128])
    sca = P.sb('sca', [128, 12]); sca2 = P.sb('sca2', [128, 12]); zeros = P.sb('zeros', [128, 1024])
    xc = P.sb('xc', [128, 1024]); xcb = P.sb('xcb', [128, 1024], BF16)
    rb = P.sb('rb', [128, 1024]); ib = P.sb('ib', [128, 1024]); ab = P.sb('ab', [128, 1024]); mb_ = P.sb('mb', [128, 1024])
    ho = [P.sb('ho%d' % i, [128, 1024]) for i in range(2)]; po = [P.sb('po%d' % i, [128, 1024]) for i in range(2)]
    t_xr = P.dma('sp', xr[:], xr_d[:, :, :]); t_cw = P.dma('sp', cw[:], cw_d[:, :, :]); t_v = P.dma('sp', vecs[:], vec_d[:, :, :])
    t_wa = P.dma('pool', wa[:], wa_d[:, :, :]); t_wx = P.dma('pool', wx[:], wx_d[:, :, :]); t_pos = P.dma('sp', posi[:], pos_d[:, :])
    t_z = P.memset(zeros[:], 0.0, eng='pool'); t_o1 = P.memset(ones1[:], 1.0, eng='pool')
    a = P.cp(nzr[:], posi[:], deps=[t_pos])
    a = P.ts(nzr[:], nzr[:], 0.0, None, ALU.not_equal, deps=[a])
    t_nz = []
    for hf in range(2):
        cs = slice(hf * 512, (hf + 1) * 512)
        m = P.mm(psG[hf][:], ones1[:], nzr[:, cs], deps=[a, t_o1])
        t_nz.append(P.cp(nz[:, cs], psG[hf][:], deps=[m]))
    s1 = P.act(sca[:], vecs[:, 3, :], AF.Exp, scale=-1.0, deps=[t_v])
    s2 = P.act(sca[:], sca[:], AF.Ln, bias=1.0, deps=[s1])
    s3 = P.ts(sca2[:], sca[:], -16.0, None, ALU.mult, deps=[s2])
    s4 = P.ts(sca[:], sca[:], -8.0, None, ALU.mult, deps=[s3])
    g_free = t_nz
    prev = []
    ho_free = [None, None]; po_free = [None, None]
    state = dict(g_free=g_free, prev=prev)
    def do_block(b):
        g_free = state['g_free']; prev = state['prev']
        c = P.act(xc[:], xr[:, b, 3:1027], AF.Identity, bias=vecs[:, 0, b:b + 1], scale=cw[:, b, 3:4], deps=[t_xr, t_cw, t_v] + prev)
        for k in range(3):
            c = P.stt(xc[:], xr[:, b, k:k + 1024], cw[:, b, k:k + 1], xc[:], ALU.mult, ALU.add, deps=[c])
        cb = P.cp(xcb[:], xc[:], deps=[c] + prev, eng='pool')
        gr_ = []
        for gi, (wt, bias_i, dst, tw) in enumerate(((wa, 1, rb, t_wa), (wx, 2, ib, t_wx))):
            evs = []
            for hf in range(2):
                cs = slice(hf * 512, (hf + 1) * 512)
                m = P.mm(psG[hf][:], wt[:, b, :], xcb[:, cs], deps=[cb, tw] + (g_free if isinstance(g_free, list) else [g_free]))
                evs.append(P.act(dst[:, cs], psG[hf][:], AF.Sigmoid, bias=vecs[:, bias_i, b:b + 1], deps=[m] + prev))
            g_free = evs
            gr_.append(evs)
        ta_ = P.act(ab[:], rb[:], AF.Exp, scale=sca[:, b:b + 1], deps=gr_[0] + [s4] + prev)
        tm = P.act(mb_[:], rb[:], AF.Exp, scale=sca2[:, b:b + 1], deps=gr_[0] + [s4] + prev)
        tm = P.act(mb_[:], mb_[:], AF.Sqrt, scale=-1.0, bias=1.0, deps=[tm])
        ta2 = P.tt(ab[:], ab[:], nz[:], ALU.mult, deps=[ta_] + t_nz)
        tm = P.stt(mb_[:], mb_[:], -1.0, nz[:], ALU.add, ALU.mult, deps=[tm] + t_nz)
        tm = P.ts(mb_[:], mb_[:], 1.0, None, ALU.add, deps=[tm])
        tb_ = P.tt(ib[:], ib[:], xc[:], ALU.mult, deps=gr_[1] + [c])
        tb_ = P.tt(ib[:], ib[:], mb_[:], ALU.mult, deps=[tb_, tm])
        oi = b % 2
        sc1 = P.op('dve', lambda e, oi=oi: e.tensor_tensor_scan(ho[oi][:], ab[:], ib[:], 0.0, ALU.mult, ALU.add), deps=[ta2, tb_, ho_free[oi]])
        sc2 = P.op('dve', lambda e, oi=oi: e.tensor_tensor_scan(po[oi][:], ab[:], zeros[:], 1.0, ALU.mult, ALU.add), deps=[ta2, t_z, po_free[oi]])
        ho_free[oi] = P.dma('sp', hl_d[b * 128:(b + 1) * 128, :], ho[oi][:], deps=[sc1], slot='ho%d' % oi, is_out=True)
        po_free[oi] = P.dma('sp', pp_d[b * 128:(b + 1) * 128, :], po[oi][:], deps=[sc2], slot='po%d' % oi, is_out=True)
        state['g_free'] = g_free; state['prev'] = [sc1, sc2, cb]
    return do_block

def bf(a):
    return np.ascontiguousarray(a).astype(ml_dtypes.bfloat16)

def consts_B(core):
    kk = np.arange(128)[:, None]; qi = np.arange(128)[None, :]
    A = np.where(kk >= qi, 0.0, NEG).astype(np.float32)
    Bm = np.where(kk <= qi, 0.0, NEG).astype(np.float32)
    allneg = np.full((128, 128), NEG, np.float32)
    m = np.zeros((128, 12, 128), np.float32)
    for i in range(8):
        m[:, i, :] = allneg if (core == 0 and i == 0) else A
    for i in range(2):
        m[:, 8 + i, :] = allneg if (core == 0 and i == 0) else A
    if core == 0:
        m[:, 10, :] = allneg
    elif core == 1:
        mm_ = A.copy(); mm_[:64, :] = NEG
        m[:, 10, :] = mm_
    else:
        m[:, 10, :] = A
    m[:, 11, :] = Bm
    return dict(masks=bf(m), ident=bf(np.eye(128, dtype=np.float32)))

def prep_B_attn(core, qT_all, kT_all, vT_all):
    T0 = core * 1024
    q = qT_all[:, T0:T0 + 1024].reshape(12, 128, 1024).transpose(1, 0, 2)
    kpad = np.zeros((1536, 2048 + 8192), dtype=kT_all.dtype); kpad[:, 2048:] = kT_all
    k = kpad[:, T0:T0 + 3072].reshape(12, 128, 3072).transpose(1, 0, 2)
    vtok = np.zeros((2048 + 8192, 1536), dtype=vT_all.dtype); vtok[2048:] = vT_all.T
    base = T0 + 2048
    idx = np.zeros((NT, 128), np.int64); valid = np.ones((NT, 128), bool)
    for j in range(9):
        idx[j] = base + 128 * (j - 1) + np.arange(128)
    for r in range(4):
        for j in range(3):
            idx[9 + r * 3 + j] = base + 512 * (j - 1) + 4 * np.arange(128) + r
    for r in range(16):
        idx[21 + 2 * r] = base - 2048 + 16 * np.arange(128) + r
        ii = base + 16 * np.arange(128) + r
        valid[21 + 2 * r + 1, 64:] = False
        ii[64:] = 0
        idx[21 + 2 * r + 1] = ii
    vt = vtok[idx]
    vt[~valid] = 0
    v = vt.reshape(NT, 128, 12, 128).transpose(1, 2, 0, 3)
    d = dict(q=np.ascontiguousarray(q), k=np.ascontiguousarray(k), v=np.ascontiguousarray(v))
    d.update(consts_B(core))
    return d

def prep_B_lru(core, xrT_all, conv_w, conv_b, wa, ba, wx, bx, lam, positions):
    T0 = core * 1024
    xpad = np.zeros((1536, 3 + 8192), np.float32); xpad[:, 3:] = xrT_all
    xr = xpad[:, T0:T0 + 1027].reshape(12, 128, 1027).transpose(1, 0, 2)
    cw = conv_w.T.reshape(12, 128, 4).transpose(1, 0, 2)
    f = lambda v: v.reshape(12, 128).T
    vecs = np.stack([f(conv_b), f(ba), f(bx), f(lam)], axis=1)
    return dict(xr=np.ascontiguousarray(xr), cw=np.ascontiguousarray(cw), vecs=np.ascontiguousarray(vecs),
                wa=np.ascontiguousarray(wa.transpose(1, 0, 2)), wx=np.ascontiguousarray(wx.transpose(1, 0, 2)),
                pos=np.ascontiguousarray(positions[:, T0:T0 + 1024]))


def build_C1():
    P = Prog()
    xT_d = P.din('xT', [2048, 1024]); at_d = P.din('attnT', [1536, 1024]); hl_d = P.din('hloc', [1536, 1024])
    pp_d = P.din('pprod', [1536, 1024]); gr_d = P.din('grT', [1536, 1024])
    summ_d = P.din('summ', [128, 8, 2, 12]); sel_d = P.din('sel', [128, 8])
    modv_d = P.din('modv', [128, 6, 16]); g2n_d = P.din('g2n', [128, 16]); og_d = P.din('og', [128, 24])
    wo_d = P.din('wo', [3072, 2048]); rw_d = P.din('rw', [128, 16, 32]); rb_d = P.din('rb', [1, 32])
    utri_d = P.din('utri', [128, 128], BF16)
    x1_o = P.dout('x1T', [2048, 1024]); h2_o = P.dout('h2T', [2048, 1024], BF16)
    G_o = P.dout('G', [128, 8, 32]); pm_o = P.dout('pm', [128, 8, 32]); cnt_o = P.dout('cnt', [128, 32])

    x = P.sb('x', [128, 16, 1024])
    y = P.sb('y', [128, 24, 1024], BF16)
    st = [P.sb('st%d' % i, [128, 1024]) for i in range(2)]
    st2 = [P.sb('st2_%d' % i, [128, 1024]) for i in range(2)]
    st3 = [P.sb('st3_%d' % i, [128, 1024]) for i in range(2)]
    sq = [P.sb('sq%d' % i, [128, 1024], BF16) for i in range(2)]
    summ = P.sb('summ', [128, 8, 2, 12]); sel = P.sb('sel', [128, 8]); Hc = P.sb('Hc', [128, 12]); Hs = P.sb('Hs', [128, 12])
    mods = P.sb('mods', [128, 6, 16]); g2n = P.sb('g2n', [128, 16]); og = P.sb('og', [128, 24]); G2 = P.sb('G2', [128, 16])
    ones = P.sb('ones', [128, 128], BF16); utri = P.sb('utri', [128, 128], BF16); ones1 = P.sb('ones1', [1, 128])
    rw = P.sb('rw', [128, 16, 32]); rb = P.sb('rb', [1, 32])
    rstdA = P.sb('rstdA', [128, 1024]); rstdL = P.sb('rstdL', [128, 1024])
    wb = [P.sb('wb%d' % i, [128, 24, 256], BF16) for i in range(2)]
    t1 = [P.sb('t1_%d' % i, [128, 512]) for i in range(2)]; t2 = [P.sb('t2_%d' % i, [128, 512]) for i in range(2)]
    hb = [P.sb('hb%d' % i, [128, 1024], BF16) for i in range(2)]
    lg = P.sb('lg', [128, 8, 32]); top8 = P.sb('top8', [128, 8, 8]); nmax = P.sb('nmax', [128, 8]); maskf = P.sb('maskf', [128, 8, 32])
    maskb = P.sb('maskb', [128, 8, 32], BF16); ex = P.sb('ex', [128, 8, 32]); den = P.sb('den', [128, 8]); Gs = P.sb('Gs', [128, 8, 32]); pm = P.sb('pm', [128, 8, 32])
    ps = [P.ps('b%d' % i) for i in range(8)]

    t_x = [P.dma('sp', x[:, kc * 4:(kc + 1) * 4, :], xT_d.rearrange("(kc p) t -> p kc t", p=128)[:, kc * 4:(kc + 1) * 4, :]) for kc in range(4)]
    t_su = P.dma('act', summ[:], summ_d[:, :, :, :]); t_sel = P.dma('act', sel[:], sel_d[:, :])
    t_m = P.dma('act', mods[:], modv_d[:, :, :]); t_g2 = P.dma('act', g2n[:], g2n_d[:, :]); t_og = P.dma('act', og[:], og_d[:, :])
    t_rw = P.dma('act', rw[:], rw_d[:, :, :]); t_rb = P.dma('act', rb[:], rb_d[:, :]); t_ut = P.dma('act', utri[:], utri_d[:, :])
    t_ones = P.memset(ones[:], 1.0); t_o1 = P.memset(ones1[:], 1.0)
    wv = wo_d.rearrange("(kc p) n -> p kc n", p=128)
    t_w = [None] * 8
    def load_w(j, deps):
        t_w[j] = P.dma('pool', wb[j % 2][:], wv[:, :, j * 256:(j + 1) * 256], deps=deps, slot='w%d' % (j % 2))
    load_w(0, []); load_w(1, [])
    a = P.memset(Hc[:], 0.0, deps=[]); b = P.memset(Hs[:], 0.0)
    tH = [a, b]
    for c in range(8):
        s = P.stt(Hs[:], Hc[:], sel[:, c:c + 1], Hs[:], ALU.mult, ALU.add, deps=tH + [t_sel, t_su])
        u = P.tt(Hc[:], Hc[:], summ[:, c, 0, :], ALU.mult, deps=[s])
        u = P.tt(Hc[:], Hc[:], summ[:, c, 1, :], ALU.add, deps=[u])
        tH = [u]
    t_Hs = tH
    G2t = P.stt(G2[:], mods[:, 4, :], 1.0, g2n[:], ALU.add, ALU.mult, deps=[t_m, t_g2])
    st_free = [None, None]; st2_free = [None, None]; st3_free = [None, None]; sq_free = [None, None]
    lastA = [None, None]; lastL = [None, None]
    t_y = []
    for ci in range(24):
        i = ci % 2
        if ci < 12:
            b_ = ci
            ld = P.dma('sp', st[i][:], at_d[b_ * 128:(b_ + 1) * 128, :], deps=[st_free[i]], slot='st%d' % i)
            src_ready = [ld]
        else:
            b_ = ci - 12
            l1 = P.dma('sp', st[i][:], hl_d[b_ * 128:(b_ + 1) * 128, :], deps=[st_free[i]], slot='st%d' % i)
            l2 = P.dma('sp', st2[i][:], pp_d[b_ * 128:(b_ + 1) * 128, :], deps=[st2_free[i]], slot='st2_%d' % i)
            l3 = P.dma('sp', st3[i][:], gr_d[b_ * 128:(b_ + 1) * 128, :], deps=[st3_free[i]], slot='st3_%d' % i)
            hf_ = P.stt(st[i][:], st2[i][:], Hs[:, b_:b_ + 1], st[i][:], ALU.mult, ALU.add, deps=[l1, l2] + t_Hs)
            ge = P.act(st2[i][:], st3[i][:], AF.Gelu_apprx_tanh, deps=[l3, hf_])
            st3_free[i] = ge
            lr = P.tt(st[i][:], st[i][:], st2[i][:], ALU.mult, deps=[hf_, ge])
            st2_free[i] = lr
            src_ready = [lr]
        s_ = P.act(sq[i][:], st[i][:], AF.Square, deps=src_ready + [sq_free[i]])
        mmt = None
        for hf in range(2):
            bank = (0 if ci < 12 else 2) + hf
            mmt = P.mm(ps[bank][:], ones[:], sq[i][:, hf * 512:(hf + 1) * 512], start=(ci % 12 == 0), stop=(ci % 12 == 11), deps=[s_, t_ones])
            if ci < 12: lastA[hf] = mmt
            else: lastL[hf] = mmt
        sq_free[i] = mmt
        yy = P.act(y[:, ci, :], st[i][:], AF.Copy, scale=og[:, ci:ci + 1], deps=src_ready + [t_og])
        st_free[i] = [yy, s_]
        st_free[i] = yy
        st_free[i] = P.op('pool', lambda e: e.memset(ones1[0:1, 0:1], 1.0), deps=[yy, s_])
        t_y.append(yy)
    t_rA = []; t_rL = []
    for hf in range(2):
        cs = slice(hf * 512, (hf + 1) * 512)
        a = P.act(rstdA[:, cs], ps[hf][:], AF.Sqrt, bias=1e-6, scale=1.0 / 1536, deps=[lastA[hf]])
        t_rA.append(P.recip(rstdA[:, cs], rstdA[:, cs], deps=[a]))
        a = P.act(rstdL[:, cs], ps[2 + hf][:], AF.Sqrt, bias=1e-6, scale=1.0 / 1536, deps=[lastL[hf]])
        t_rL.append(P.recip(rstdL[:, cs], rstdL[:, cs], deps=[a]))
    bank_free = {4: None, 5: None, 6: None, 7: None}
    tfree = [None, None]
    t_x1 = []
    u = 0
    for dmc in range(16):
        j = dmc // 2
        for hf in range(2):
            cs = slice(hf * 512, (hf + 1) * 512)
            bA = 4 + (u % 2) * 2; bL = bA + 1
            la = None; ll = None
            for ci in range(12):
                la = P.mm(ps[bA][:], wb[j % 2][:, ci, (dmc % 2) * 128:(dmc % 2 + 1) * 128], y[:, ci, cs], start=(ci == 0), stop=(ci == 11),
                          deps=[t_y[ci]] + ([t_w[j], bank_free[bA]] if ci == 0 else []))
            for ci in range(12, 24):
                ll = P.mm(ps[bL][:], wb[j % 2][:, ci, (dmc % 2) * 128:(dmc % 2 + 1) * 128], y[:, ci, cs], start=(ci == 12), stop=(ci == 23),
                          deps=[t_y[ci]] + ([bank_free[bL]] if ci == 12 else []))
            if dmc % 2 == 1 and hf == 1 and j + 2 < 8:
                load_w(j + 2, [ll])
            i2 = u % 2
            e1 = P.tt(t1[i2][:], ps[bA][:], rstdA[:, cs], ALU.mult, deps=[la, t_rA[hf], tfree[i2]])
            e2 = P.tt(t2[i2][:], ps[bL][:], rstdL[:, cs], ALU.mult, deps=[ll, t_rL[hf], tfree[i2]])
            bank_free[bA] = e1; bank_free[bL] = e2
            e3 = P.tt(t1[i2][:], t1[i2][:], t2[i2][:], ALU.add, deps=[e1, e2], eng='pool')
            e4 = P.stt(x[:, dmc, cs], t1[i2][:], mods[:, 2, dmc:dmc + 1], x[:, dmc, cs], ALU.mult, ALU.add, deps=[e3, t_m, t_x[dmc // 4]])
            tfree[i2] = e4
            t_x1.append(e4)
            u += 1
    for kc in range(4):
        P.dma('sp', x1_o.rearrange("(kc p) t -> p kc t", p=128)[:, kc * 4:(kc + 1) * 4, :], x[:, kc * 4:(kc + 1) * 4, :], deps=t_x1[kc * 8:(kc + 1) * 8], is_out=True)
    sq_free = [t_y[-1], t_y[-1]]
    lastN = [None, None]
    for kc in range(16):
        i = kc % 2
        s_ = P.act(sq[i][:], x[:, kc, :], AF.Square, deps=t_x1[2 * kc:2 * kc + 2] + [sq_free[i]])
        for hf in range(2):
            lastN[hf] = P.mm(ps[hf][:], ones[:], sq[i][:, hf * 512:(hf + 1) * 512], start=(kc == 0), stop=(kc == 15), deps=[s_] + (t_rA + t_rL if kc == 0 else []))
        sq_free[i] = lastN[1]
    rstd2 = rstdA
    t_r2 = []
    for hf in range(2):
        cs = slice(hf * 512, (hf + 1) * 512)
        a = P.act(rstd2[:, cs], ps[hf][:], AF.Sqrt, bias=1e-6, scale=1.0 / 2048, deps=[lastN[hf]] + t_x1)
        t_r2.append(P.recip(rstd2[:, cs], rstd2[:, cs], deps=[a]))
    LG = ps[2]
    st_free = [None, None]; st2_free = [None, None]; hb_free = [None, None]
    last_r = None
    for kc in range(16):
        i = kc % 2
        a = P.tt(st[i][:], x[:, kc, :], rstd2[:], ALU.mult, deps=t_r2 + [st_free[i]])
        hh = P.act(st2[i][:], st[i][:], AF.Identity, bias=mods[:, 3, kc:kc + 1], scale=G2[:, kc:kc + 1], deps=[a, G2t, st2_free[i]])
        st_free[i] = hh
        for g in range(8):
            last_r = P.mm(LG[:, g * 32:(g + 1) * 32], st2[i][:, g * 128:(g + 1) * 128], rw[:, kc, :], start=(kc == 0 and g == 0), stop=False,
                          deps=[hh, t_rw] + t_rL)
        cb = P.cp(hb[i][:], st2[i][:], deps=[hh, hb_free[i]], eng='pool')
        st2_free[i] = P.op('pool', lambda e: e.memset(ones1[0:1, 0:1], 1.0), deps=[cb, last_r])
        hb_free[i] = P.dma('sp', h2_o[kc * 128:(kc + 1) * 128, :], hb[i][:], deps=[cb], slot='hb%d' % i, is_out=True)
    for g in range(8):
        last_r = P.mm(LG[:, g * 32:(g + 1) * 32], ones1[:, :], rb[:, :], start=False, stop=(g == 7), deps=[t_rb, t_o1])
    c0 = P.cp(lg[:].rearrange("p g e -> p (g e)"), LG[:, 0:256], deps=[last_r])
    tk = []
    for g in range(8):
        m = P.op('dve', lambda e, g=g: e.max(top8[:, g, :], lg[:, g, :]), deps=[c0])
        tk.append(m)
    n1 = P.ts(nmax[:], top8[:, :, 0], -1.0, None, ALU.mult, deps=tk)
    last = n1
    for g in range(8):
        mk = P.ts(maskf[:, g, :], lg[:, g, :], top8[:, g, 3:4], None, ALU.is_ge, deps=[last])
        e_ = P.act(ex[:, g, :], lg[:, g, :], AF.Exp, bias=nmax[:, g:g + 1], deps=[n1])
        em = P.tt(ex[:, g, :], ex[:, g, :], maskf[:, g, :], ALU.mult, deps=[mk, e_])
        dn = P.op('dve', lambda e, g=g: e.tensor_reduce(den[:, g:g + 1], ex[:, g, :], mybir.AxisListType.X, ALU.add), deps=[em])
        last = dn
    rd = P.recip(den[:], den[:], deps=[last])
    for g in range(8):
        last = P.ts(Gs[:, g, :], ex[:, g, :], den[:, g:g + 1], None, ALU.mult, deps=[rd])
    P.dma('sp', G_o[:, :, :], Gs[:], deps=[last], is_out=True)
    mb = P.cp(maskb[:], maskf[:], deps=[last])
    PO = ps[3]
    lp = None
    for g in range(8):
        lp = P.mm(PO[:, g * 32:(g + 1) * 32], utri[:], maskb[:, g, :], start=(g == 0), stop=False, deps=[mb, t_ut] + t_r2)
        for g2 in range(g):
            lp = P.mm(PO[:, g * 32:(g + 1) * 32], ones[:], maskb[:, g2, :], start=False, stop=False)
    lc_ = None
    for g in range(8):
        lc_ = P.mm(PO[:, 256:288], ones[:], maskb[:, g, :], start=False, stop=False, deps=[mb])
    cnts = P.sb('cnts', [128, 32])
    cc_ = P.cp(cnts[:], PO[:, 256:288], deps=[lc_])
    P.dma('sp', cnt_o[:, :], cnts[:], deps=[cc_], is_out=True)
    pmf = pm[:].rearrange("p g e -> p (g e)")
    a = P.stt(pmf, PO[:, 0:256], 1.0, maskf[:].rearrange("p g e -> p (g e)"), ALU.add, ALU.mult, deps=[lp, cc_])
    a = P.ts(pmf, pmf, -1.0, None, ALU.add, deps=[a])
    P.dma('sp', pm_o[:, :, :], pm[:], deps=[a], is_out=True)
    return P

def prep_C1(core, xT_core, attnT, hloc_all, pprod_all, grT_core, mod_l, norm2_g, attn_g, lru_g, w_out_l, router_w_l, router_b_l):
    summ = np.zeros((128, 8, 2, 12), np.float32)
    for c in range(8):
        summ[:, c, 0, :] = pprod_all[c][:, -1].reshape(12, 128).T
        summ[:, c, 1, :] = hloc_all[c][:, -1].reshape(12, 128).T
    sel = np.zeros((128, 8), np.float32); sel[:, core] = 1.0
    og = np.concatenate([attn_g.reshape(12, 128).T, lru_g.reshape(12, 128).T], axis=1)
    utri = (np.arange(128)[:, None] < np.arange(128)[None, :]).astype(np.float32)
    return dict(xT=xT_core, attnT=attnT, hloc=hloc_all[core], pprod=pprod_all[core], grT=grT_core, summ=summ, sel=sel,
                modv=np.ascontiguousarray(mod_l.reshape(6, 16, 128).transpose(2, 0, 1)), g2n=np.ascontiguousarray(norm2_g.reshape(16, 128).T),
                og=np.ascontiguousarray(og), wo=w_out_l, rw=np.ascontiguousarray(router_w_l.reshape(16, 128, 32).transpose(1, 0, 2)),
                rb=np.ascontiguousarray(router_b_l[None, :]), utri=utri.astype(ml_dtypes.bfloat16))


SCS = 1792
NBS = SCS // 128
NSC = 2
CAPT = SCS * NSC
NBT = CAPT // 128
BIG = 1000000.0
SUBS = [(0, 512), (512, 512), (1024, 512), (1536, 256)]

def _ind(P, kind, out, in_, idx_ap, deps, slot, **kw):
    P.dma_slots[slot] = P.dma_slots.get(slot, 0) + 1
    cnt = P.dma_slots[slot]
    if not hasattr(P, 'regcache'):
        P.regcache = {}
    if 'bounds_check' in kw:
        bval = kw.pop('bounds_check')
        okw = dict(kw)
        def fix(e, okw=okw, bval=bval):
            if bval not in P.regcache:
                P.regcache[bval] = e.to_reg(bval)
            d = dict(okw); d['bounds_check'] = P.regcache[bval]
            return d
    else:
        okw = dict(kw)
        def fix(e, okw=okw):
            return okw
    if kind == 'g':
        fn = lambda e: e.indirect_dma_start(out=out, out_offset=None, in_=in_, in_offset=bass.IndirectOffsetOnAxis(ap=idx_ap, axis=0), **fix(e))
    else:
        fn = lambda e: e.indirect_dma_start(out=out, out_offset=bass.IndirectOffsetOnAxis(ap=idx_ap, axis=0), in_=in_, in_offset=None, **fix(e))
    P.ops['pool'].append(dict(fn=fn, deps=[d for d in deps if d is not None], sig=False, dma=(slot, cnt)))
    return ('d', slot, cnt)

def _ind_old(P, kind, out, in_, idx_ap, deps, slot, **kw):
    cnt = 0
    if kind == 'g':
        fn = lambda e: e.indirect_dma_start(out=out, out_offset=None, in_=in_, in_offset=bass.IndirectOffsetOnAxis(ap=idx_ap, axis=0), **kw)
    else:
        fn = lambda e: e.indirect_dma_start(out=out, out_offset=bass.IndirectOffsetOnAxis(ap=idx_ap, axis=0), in_=in_, in_offset=None, **kw)
    P.ops['pool'].append(dict(fn=fn, deps=[d for d in deps if d is not None], sig=False, dma=(slot, cnt)))
    return ('d', slot, cnt)

def build_C3(NE=4, nsc=NSC):
    P = Prog(); nc = P.nc
    h_d = P.din('h2all', [8192, 2048], BF16); pm_d = P.din('pm', [128, 64, NE]); cnt_d = P.din('cnt', [128, 8, NE]); G_d = P.din('Grows', [8192, NE])
    tid_d = P.din('tid', [128, 64], I32); dum_d = P.din('dumrow', [128, 1]); id_d = P.din('ident', [128, 128], BF16)
    w1_d = P.din('w1', [NE, 2048, 4096]); w2_d = P.din('w2', [NE, 2048, 2048])
    b1g_d = P.din('b1g', [128, NE, 16]); b1l_d = P.din('b1l', [128, NE, 16]); b2_d = P.din('b2', [1, NE, 2048])
    y_o = P.dout('ypart', [8192 + 128, 2048])
    lists = [nc.dram_tensor("lists%d" % e, [CAPT, 1], I32, kind="Internal").ap() for e in range(NE)]

    pm = P.sb('pm', [128, 64, NE]); cnt = P.sb('cnt', [128, 8, NE]); start = P.sb('start', [128, 8, NE]); valid = P.sb('valid', [128, 64, NE])
    posg = P.sb('posg', [128, 64, NE]); inv = P.sb('inv', [128, 64, NE]); posi = P.sb('posi', [128, 64, NE], I32)
    tid = P.sb('tid', [128, 64], I32); dum = P.sb('dum', [128, 1]); ident = P.sb('ident', [128, 128], BF16)
    bigt = P.sb('bigt', [128, NBT], I32)
    idxt = P.sb('idxt', [128, NE, NBT], I32); idxf = P.sb('idxf', [128, NE, NBT]); v2 = P.sb('v2', [128, NE, NBT]); isc = P.sb('isc', [128, 4, NE, NBT], I32); iscf = P.sb('iscf', [128, NE, NBT])
    gsl = P.sb('gsl', [128, NBT, NE])
    b1g = P.sb('b1g', [128, NE, 16]); b1l = P.sb('b1l', [128, NE, 16]); b2 = P.sb('b2', [1, 2048]); ones1 = P.sb('ones1', [1, 128])
    XeT = P.sb('XeT', [128, 16, SCS], BF16); actT = P.sb('actT', [128, 16, SCS], BF16)
    Xs = [P.sb('Xs%d' % i, [128, 2048], BF16) for i in range(3)]
    w1b = [P.sb('w1b%d' % i, [128, 16, 256], BF16) for i in range(2)]
    w2b = [P.sb('w2b%d' % i, [128, 16, 512], BF16) for i in range(2)]
    Yst = [P.sb('Yst%d' % i, [128, 512]) for i in range(4)]
    gc = P.sb('gc', [128, 512]); sgm = P.sb('sgm', [128, 512]); lc = P.sb('lc', [128, 512])
    zer = Yst
    banks = [P.ps('b%d' % i) for i in range(6)]
    tbank = [P.ps('tb%d' % i, [128, 1024], BF16) for i in range(2)]
    bfree = [[] for _ in range(6)]; bptr = [0]
    def alloc():
        i = bptr[0] % 6; bptr[0] += 1
        return i, list(bfree[i])

    t_pm = P.dma('sp', pm[:], pm_d[:, :, :]); t_cnt = P.dma('sp', cnt[:], cnt_d[:, :, :]); t_tid = P.dma('sp', tid[:], tid_d[:, :])
    t_c = [P.dma('act', b1g[:], b1g_d[:, :, :]), P.dma('act', b1l[:], b1l_d[:, :, :]), P.dma('act', dum[:], dum_d[:, :]), P.dma('act', ident[:], id_d[:, :])]
    t_o1 = P.memset(ones1[:], 1.0)
    zt = [P.memset(Yst[i][:], 0.0, eng='pool') for i in range(4)]
    yv = y_o.rearrange("(b p) (c n) -> b p c n", p=128, n=512)
    t_zero = []
    for b in range(65):
        t_zero.append(P.dma('sp' if b % 2 == 0 else 'act', yv[b, :, :, :], Yst[0][:].rearrange("p (o n) -> p o n", o=1).broadcast(1, 4) if False else Yst[b % 4][:, None, :].to_broadcast([128, 4, 512]) if False else Yst[b % 4][:], deps=zt, slot='z%d' % (b % 4))) if False else None
    t_zero = []
    for b in range(65):
        for c4 in range(4):
            t_zero.append(P.dma('sp' if (b + c4) % 2 == 0 else 'act', y_o[b * 128:(b + 1) * 128, c4 * 512:(c4 + 1) * 512], Yst[c4][:], deps=zt, slot='z%d' % c4))
    t_zero_last = t_zero[-4:]
    w1v = w1_d.rearrange("e (kc p) n -> e p kc n", p=128); w2v = w2_d.rearrange("e (kc p) n -> e p kc n", p=128)
    w1_free = [None, None]; w2_free = [None, None]; w1_tok = {}; w2_tok = {}
    w1_seq = [(e, s, c) for e in range(NE) for s in range(nsc) for c in range(16)]
    w2_seq = [(e, s, c) for e in range(NE) for s in range(nsc) for c in range(4)]
    w1_i = [0]; w2_i = [0]
    def issue_w1():
        if w1_i[0] >= len(w1_seq): return
        k = w1_i[0]; e, s, c = w1_seq[k]
        w1_tok[(e, s, c)] = (P.dma('pool', w1b[k % 2][:], w1v[e, :, :, c * 256:(c + 1) * 256], deps=[w1_free[k % 2]], slot='w1_%d' % (k % 2)), k % 2)
        w1_i[0] += 1
    def issue_w2():
        if w2_i[0] >= len(w2_seq): return
        k = w2_i[0]; e, s, c = w2_seq[k]
        w2_tok[(e, s, c)] = (P.dma('pool', w2b[k % 2][:], w2v[e, :, :, c * 512:(c + 1) * 512], deps=[w2_free[k % 2]], slot='w2_%d' % (k % 2)), k % 2)
        w2_i[0] += 1
    a = P.memset(start[:, 0, :], 0.0, deps=[t_cnt])
    for s in range(1, 8):
        a = P.tt(start[:, s, :], start[:, s - 1, :], cnt[:, s - 1, :], ALU.add, deps=[a, t_cnt])
    t_start = a
    tv = P.ts(valid[:], pm[:], 0.0, None, ALU.is_ge, deps=[t_pm])
    last = tv
    for s in range(8):
        for e in range(NE):
            last = P.ts(posg[:, 8 * s:8 * s + 8, e], pm[:, 8 * s:8 * s + 8, e], start[:, s, e:e + 1], None, ALU.add, deps=[t_start, t_pm])
    a = P.tt(posg[:], posg[:], valid[:], ALU.mult, deps=[last, tv])
    b = P.ts(inv[:], valid[:], -1.0, -BIG, ALU.add, ALU.mult, deps=[tv])
    a = P.tt(posg[:], posg[:], inv[:], ALU.add, deps=[a, b])
    t_posi = P.cp(posi[:], posg[:], deps=[a])
    tb_ = P.memset(bigt[:], int(BIG))
    t_li = [P.dma('sp', lists[e].rearrange("(p b) o -> p (b o)", p=128), bigt[:], deps=[tb_]) for e in range(NE)]
    t_sc = []
    for e in range(NE):
        lastsc = None
        hist = []
        for G in range(64):
            lastsc = _ind(P, 's', lists[e], tid[:, G:G + 1], posi[:, G, e:e + 1], [t_posi, t_tid, t_li[e]] + ([hist[-8]] if len(hist) >= 8 else []), 'ls%d' % e, bounds_check=CAPT - 1, oob_is_err=False)
            hist.append(lastsc)
        t_sc.append(lastsc)
    t_idx = [P.dma('sp', idxt[:, e, :], lists[e].rearrange("(p b) o -> p (b o)", p=128), deps=[t_sc[e]]) for e in range(NE)]
    a = P.cp(idxf[:], idxt[:], deps=t_idx)
    a2 = P.ts(v2[:], idxf[:], 8192.0, None, ALU.is_lt, deps=[a])
    a3 = P.tt(idxf[:], idxf[:], v2[:], ALU.mult, deps=[a2])
    a4 = P.ts(v2[:], v2[:], -1.0, -1.0, ALU.add, ALU.mult, deps=[a3])
    a5 = P.ts(v2[:], v2[:], dum[:, 0:1], None, ALU.mult, deps=[a4, t_c[2]])
    a6 = P.tt(idxf[:], idxf[:], v2[:], ALU.add, deps=[a5])
    t_isc_l = []
    for c4 in range(4):
        a7 = P.ts(iscf[:], idxf[:], 4.0, float(c4), ALU.mult, ALU.add, deps=[a6] + t_isc_l)
        t_isc_l.append(P.cp(isc[:, c4, :, :], iscf[:], deps=[a7]))
    t_isc = t_isc_l[-1]
    y4 = y_o.rearrange("t (c n) -> (t c) n", n=512)
    issue_w1(); issue_w1(); issue_w2(); issue_w2()

    Xs_free = [[] for _ in range(3)]; Yst_free = [list(t_zero) if False else [] for _ in range(4)]
    XeT_free = []; actT_free = []; tmp_free = []; b2_free = []; gsl_free = []
    prev_scatter = list(t_zero_last) + t_zero
    tfree = [[], []]
    for e in range(NE):
        t_b2 = P.dma('sp', b2[:], b2_d[:, e, :], deps=b2_free, slot='b2')
        exp_scatter = []
        for sc in range(nsc):
            blk0 = sc * NBS
            t_g = None
            for bl in range(NBS):
                t_g = _ind(P, 'g', gsl[:, blk0 + bl, :], G_d[:, :], idxt[:, e, blk0 + bl:blk0 + bl + 1], [t_idx[e]] + gsl_free, 'gg', bounds_check=8191, oob_is_err=False)
            gsl_free = []
            xe_t = [[None] * NBS for _ in range(16)]
            lasttr = None
            for bl in range(NBS):
                r = bl % 3
                tg = _ind(P, 'g', Xs[r][:], h_d[:, :], idxt[:, e, blk0 + bl:blk0 + bl + 1], [t_idx[e]] + Xs_free[r], 'xg%d' % r, bounds_check=8191, oob_is_err=False)
                for q in range(4):
                    ti = (bl * 4 + q) % 2
                    for k4 in range(4):
                        kc = q * 4 + k4
                        lasttr = P.tr(tbank[ti][:, k4 * 128:(k4 + 1) * 128], Xs[r][:, kc * 128:(kc + 1) * 128], ident[:], deps=[tg, t_c[3]] + (tfree[ti] if k4 == 0 else []))
                    ev = P.cp(XeT[:, q * 4:(q + 1) * 4, bl * 128:(bl + 1) * 128], tbank[ti][:, 0:512].rearrange("p (k n) -> p k n", k=4), deps=[lasttr] + XeT_free,
                              eng=('act' if q % 2 == 0 else 'dve'))
                    tfree[ti] = [ev]
                    for k4 in range(4):
                        xe_t[q * 4 + k4][bl] = ev
                Xs_free[r] = [lasttr]
            XeT_free = []
            act_t = [[None] * len(SUBS) for _ in range(16)]
            lastw1 = None
            for ic in range(16):
                tw, wi = w1_tok[(e, sc, ic)]
                for si, (s0, sn) in enumerate(SUBS):
                    bG, dG = alloc(); bL, dL = alloc()
                    xdeps = [xe_t[kc_][bb] for kc_ in range(16) for bb in range(s0 // 128, (s0 + sn) // 128)]
                    for kc in range(16):
                        P.mm(banks[bG][:, 0:sn], w1b[wi][:, kc, 0:256:2], XeT[:, kc, s0:s0 + sn], start=(kc == 0), stop=(kc == 15), deps=(dG + [tw] + xdeps if kc == 0 else []))
                    mg = ('c', 'pe', len(P.ops['pe']) - 1)
                    for kc in range(16):
                        lastw1 = P.mm(banks[bL][:, 0:sn], w1b[wi][:, kc, 1:256:2], XeT[:, kc, s0:s0 + sn], start=(kc == 0), stop=(kc == 15), deps=(dL if kc == 0 else []))
                    e1 = P.ts(gc[:, 0:sn], banks[bG][:, 0:sn], b1g[:, e, ic:ic + 1], 7.0, ALU.add, ALU.min, deps=[mg] + tmp_free + t_c)
                    e2 = P.ts(lc[:, 0:sn], banks[bL][:, 0:sn], b1l[:, e, ic:ic + 1], 7.0, ALU.add, ALU.min, deps=[lastw1] + tmp_free)
                    bfree[bG] = [e1]; bfree[bL] = [e2]
                    e3 = P.act(sgm[:, 0:sn], gc[:, 0:sn], AF.Sigmoid, scale=1.702, deps=[e1] + tmp_free)
                    e4 = P.ts(lc[:, 0:sn], lc[:, 0:sn], -7.0, 1.0, ALU.max, ALU.add, deps=[e2])
                    e5 = P.tt(gc[:, 0:sn], gc[:, 0:sn], sgm[:, 0:sn], ALU.mult, deps=[e3, e1])
                    e6 = P.tt(actT[:, ic, s0:s0 + sn], gc[:, 0:sn], lc[:, 0:sn], ALU.mult, deps=[e5, e4] + actT_free)
                    tmp_free = [e6]
                    act_t[ic][si] = e6
                w1_free[wi] = lastw1
                issue_w1()
            actT_free = []
            XeT_free = [lastw1]
            lastw2 = None
            yk = 0
            for dmc in range(4):
                tw, wi = w2_tok[(e, sc, dmc)]
                for bl in range(NBS):
                    si = [i for i, (s0, sn) in enumerate(SUBS) if s0 <= bl * 128 < s0 + sn][0]
                    bk, dps = alloc()
                    for ic in range(16):
                        P.mm(banks[bk][:, 0:512], actT[:, ic, bl * 128:(bl + 1) * 128], w2b[wi][:, ic, :], start=(ic == 0), stop=False,
                             deps=(dps + [tw] if ic == 0 else []) + [act_t[ic][si]])
                    lastw2 = P.mm(banks[bk][:, 0:512], ones1[:, :], b2[:, dmc * 512:(dmc + 1) * 512], start=False, stop=True, deps=[t_o1, t_b2])
                    r = yk % 4; yk += 1
                    ev = P.act(Yst[r][:], banks[bk][:, 0:512], AF.Copy, scale=gsl[:, blk0 + bl, e:e + 1], deps=[lastw2, t_g] + Yst_free[r] + zt)
                    bfree[bk] = [ev]
                    sct = _ind(P, 's', y4, Yst[r][:], isc[:, dmc, e, blk0 + bl:blk0 + bl + 1], [ev, t_isc] + prev_scatter, 'ys%d' % r, compute_op=ALU.add)
                    Yst_free[r] = [sct]
                    exp_scatter.append(sct)
                w2_free[wi] = lastw2
                issue_w2()
            actT_free = [lastw2]
            gsl_free = [lastw2]
            b2_free = [lastw2]
            prev_scatter = prev_scatter if sc < nsc - 1 else []
        prev_scatter = exp_scatter[-4:] + [t for t in exp_scatter]
    for t in prev_scatter:
        P.out_toks.append(t)
    return P

def prep_C3(core, h2all, pm_all, cnt_all, G_all, w1_l, b1_l, w2_l, b2_l, NE=4):
    e0 = core * NE
    pm = np.concatenate([pm_all[s][:, :, e0:e0 + NE] for s in range(8)], axis=1)
    cnt = np.stack([np.tile(cnt_all[s][None, e0:e0 + NE], (128, 1)) for s in range(8)], axis=1)
    Grows = np.concatenate([G_all[s][:, :, e0:e0 + NE].transpose(1, 0, 2).reshape(1024, NE) for s in range(8)], axis=0)
    tid = (np.arange(64, dtype=np.int32)[None, :] * 128 + np.arange(128, dtype=np.int32)[:, None]).astype(np.int32)
    b1 = b1_l[e0:e0 + NE]
    b1g = b1[:, 0::2].reshape(NE, 16, 128).transpose(2, 0, 1); b1l = b1[:, 1::2].reshape(NE, 16, 128).transpose(2, 0, 1)
    return dict(h2all=h2all, pm=np.ascontiguousarray(pm), cnt=np.ascontiguousarray(cnt.astype(np.float32)), Grows=np.ascontiguousarray(Grows),
                tid=tid, dumrow=(8192 + np.arange(128, dtype=np.float32))[:, None], ident=np.eye(128, dtype=np.float32).astype(ml_dtypes.bfloat16),
                w1=np.ascontiguousarray(w1_l[e0:e0 + NE]), w2=np.ascontiguousarray(w2_l[e0:e0 + NE]),
                b1g=np.ascontiguousarray(b1g), b1l=np.ascontiguousarray(b1l), b2=np.ascontiguousarray(b2_l[None, e0:e0 + NE]))


def build_D(final=False):
    P = Prog()
    x_d = P.din('x1T', [2048, 1024]); y_d = P.din('yparts', [8, 2048, 1024]); modv_d = P.din('modv', [128, 6, 16]); fg_d = P.din('fg', [128, 16])
    o_d = P.dout('x2T', [2048, 1024])
    x = P.sb('x', [128, 16, 1024]); mods = P.sb('mods', [128, 6, 16]); fg = P.sb('fg', [128, 16])
    yb = [[P.sb('yb%d_%d' % (i, c), [128, 1024]) for c in range(8)] for i in range(2)]
    sq = [P.sb('sq%d' % i, [128, 1024], BF16) for i in range(2)]; ones = P.sb('ones', [128, 128], BF16); rstd = P.sb('rstd', [128, 1024])
    ps = [P.ps('b%d' % i) for i in range(2)]
    t_m = P.dma('act', mods[:], modv_d[:, :, :]); t_fg = P.dma('act', fg[:], fg_d[:, :])
    t_x = [P.dma('act', x[:, kc * 4:(kc + 1) * 4, :], x_d.rearrange("(kc p) t -> p kc t", p=128)[:, kc * 4:(kc + 1) * 4, :]) for kc in range(4)]
    t_ones = P.memset(ones[:], 1.0)
    yfree = [[None] * 8, [None] * 8]
    sq_free = [None, None]
    t_x2 = []
    lastN = [None, None]
    for kc in range(16):
        i = kc % 2
        lt = [P.dma('sp', yb[i][c][:], y_d[c, kc * 128:(kc + 1) * 128, :], deps=[yfree[i][c]], slot='y%d_%d' % (i, c)) for c in range(8)]
        a01 = P.tt(yb[i][0][:], yb[i][0][:], yb[i][1][:], ALU.add, deps=[lt[0], lt[1]])
        a23 = P.tt(yb[i][2][:], yb[i][2][:], yb[i][3][:], ALU.add, deps=[lt[2], lt[3]], eng='pool')
        a45 = P.tt(yb[i][4][:], yb[i][4][:], yb[i][5][:], ALU.add, deps=[lt[4], lt[5]])
        a67 = P.tt(yb[i][6][:], yb[i][6][:], yb[i][7][:], ALU.add, deps=[lt[6], lt[7]], eng='pool')
        b0 = P.tt(yb[i][0][:], yb[i][0][:], yb[i][2][:], ALU.add, deps=[a01, a23])
        b1 = P.tt(yb[i][4][:], yb[i][4][:], yb[i][6][:], ALU.add, deps=[a45, a67], eng='pool')
        c0 = P.tt(yb[i][0][:], yb[i][0][:], yb[i][4][:], ALU.add, deps=[b0, b1])
        xx = P.stt(x[:, kc, :], yb[i][0][:], mods[:, 5, kc:kc + 1], x[:, kc, :], ALU.mult, ALU.add, deps=[c0, t_m, t_x[kc // 4]])
        for c in range(8):
            yfree[i][c] = xx
        t_x2.append(xx)
        if final:
            s_ = P.act(sq[i][:], x[:, kc, :], AF.Square, deps=[xx, sq_free[i]])
            for hf in range(2):
                lastN[hf] = P.mm(ps[hf][:], ones[:], sq[i][:, hf * 512:(hf + 1) * 512], start=(kc == 0), stop=(kc == 15), deps=[s_, t_ones])
            sq_free[i] = lastN[1]
    if final:
        t_r = []
        for hf in range(2):
            cs = slice(hf * 512, (hf + 1) * 512)
            a = P.act(rstd[:, cs], ps[hf][:], AF.Sqrt, bias=1e-6, scale=1.0 / 2048, deps=[lastN[hf]])
            t_r.append(P.recip(rstd[:, cs], rstd[:, cs], deps=[a]))
        t_f = []
        for kc in range(16):
            t_f.append(P.stt(x[:, kc, :], x[:, kc, :], fg[:, kc:kc + 1], rstd[:], ALU.mult, ALU.mult, deps=t_r + [t_fg, t_x2[kc]]))
        t_x2 = t_f
    for kc in range(4):
        P.dma('sp', o_d.rearrange("(kc p) t -> p kc t", p=128)[:, kc * 4:(kc + 1) * 4, :], x[:, kc * 4:(kc + 1) * 4, :], deps=t_x2[kc * 4:(kc + 1) * 4], is_out=True)
    return P


def _np(a):
    return np.asarray(a)

def _to_bf16(a):
    a = np.asarray(a)
    if a.dtype == ml_dtypes.bfloat16:
        return a
    if a.dtype.itemsize == 2:
        return a.view(ml_dtypes.bfloat16)
    raise ValueError('unexpected dtype %s' % a.dtype)

def kernel(x, c, positions, ada_w, ada_b, norm1_g, norm2_g, w_in, conv_w, conv_b, lru_wa, lru_ba, lru_wx, lru_bx,
           lru_lambda, attn_out_g, lru_out_g, w_out, router_w, router_b, w1, b1, w2, b2, final_g):
    x = _np(x); c = _np(c); positions = _np(positions); ada_w = _np(ada_w); ada_b = _np(ada_b)
    norm1_g = _np(norm1_g); norm2_g = _np(norm2_g); w_in = _np(w_in); conv_w = _np(conv_w); conv_b = _np(conv_b)
    lru_wa = _np(lru_wa); lru_ba = _np(lru_ba); lru_wx = _np(lru_wx); lru_bx = _np(lru_bx); lru_lambda = _np(lru_lambda)
    attn_out_g = _np(attn_out_g); lru_out_g = _np(lru_out_g); w_out = _np(w_out); router_w = _np(router_w); router_b = _np(router_b)
    w1 = _np(w1); b1 = _np(b1); w2 = _np(w2); b2 = _np(b2); final_g = _np(final_g)
    NC = 8
    ims = []
    for core in range(NC):
        l = core // 4; cs = (core % 4) * 3072
        ims.append({'c': np.ascontiguousarray(c.reshape(16, 128).T), 'w': np.ascontiguousarray(ada_w[l][:, cs:cs + 3072]),
                    'b': np.ascontiguousarray(ada_b[l][None, cs:cs + 3072])})
    res = build_M().run(ims)
    mod = np.concatenate([np.asarray(r['mod'])[0] for r in res.results]).reshape(2, 12288)
    xT = [np.ascontiguousarray(x[0, k * 1024:(k + 1) * 1024, :].T) for k in range(NC)]
    fgl = np.ascontiguousarray(final_g.reshape(16, 128).T)
    for l in range(2):
        modv = np.ascontiguousarray(mod[l].reshape(6, 16, 128).transpose(2, 0, 1))
        w_in_l = np.ascontiguousarray(w_in[l])
        cA = consts_A()
        ims = []
        for k in range(NC):
            d = dict(xT=xT[k], modv=modv, gn=np.ascontiguousarray(norm1_g[l].reshape(16, 128).T), w=w_in_l,
                     pos=np.ascontiguousarray(positions[:, k * 1024:(k + 1) * 1024]))
            d.update(cA)
            ims.append(d)
        rA = build_A().run(ims).results
        qT = np.concatenate([_to_bf16(r['qT']) for r in rA], axis=1)
        kT = np.concatenate([_to_bf16(r['kT']) for r in rA], axis=1)
        vT = np.concatenate([_to_bf16(r['vT']) for r in rA], axis=1)
        xrT = np.concatenate([np.asarray(r['xrT']) for r in rA], axis=1)
        grT = [np.asarray(r['grT']) for r in rA]
        ims = []
        for k in range(NC):
            m = prep_B_attn(k, qT, kT, vT)
            m.update(prep_B_lru(k, xrT, conv_w[l], conv_b[l], lru_wa[l], lru_ba[l], lru_wx[l], lru_bx[l], lru_lambda[l], positions))
            ims.append(m)
        rB = build_B(True).run(ims).results
        hloc = [np.asarray(r['hloc']) for r in rB]; pprod = [np.asarray(r['pprod']) for r in rB]
        wo_l = np.ascontiguousarray(w_out[l])
        ims = [prep_C1(k, xT[k], np.asarray(rB[k]['attnT']), hloc, pprod, grT[k], mod[l], norm2_g[l], attn_out_g[l], lru_out_g[l],
                       wo_l, router_w[l], router_b[l]) for k in range(NC)]
        rC = build_C1().run(ims).results
        x1T = [np.asarray(r['x1T']) for r in rC]
        h2all = np.ascontiguousarray(np.concatenate([_to_bf16(r['h2T']).T for r in rC], axis=0))
        pm = [np.asarray(r['pm']) for r in rC]; G = [np.asarray(r['G']) for r in rC]
        cnt_all = [np.asarray(r['cnt'])[0] for r in rC]
        ims = [prep_C3(k, h2all, pm, cnt_all, G, w1[l], b1[l], w2[l], b2[l]) for k in range(NC)]
        rE = build_C3().run(ims).results
        yp = [np.asarray(r['ypart']) for r in rE]
        final = (l == 1)
        ims = [dict(x1T=x1T[k], yparts=np.ascontiguousarray(np.stack([yp[cc][k * 1024:(k + 1) * 1024].T for cc in range(NC)])), modv=modv, fg=fgl)
               for k in range(NC)]
        rD = build_D(final=final).run(ims).results
        xT = [np.asarray(r['x2T']) for r in rD]
    out = np.concatenate([t.T for t in xT], axis=0)[None].astype(np.float32)
    return out
```

```python
import contextlib
import numpy as np
import concourse.bass as bass
import concourse.mybir as mybir
from concourse.bass_utils import run_bass_kernel_spmd

F32 = mybir.dt.float32
BF16 = mybir.dt.bfloat16
I32 = mybir.dt.int32
AF = mybir.ActivationFunctionType
ALU = mybir.AluOpType


class Prog:
    ENG = ['pe', 'act', 'dve', 'pool', 'sp']

    def __init__(self):
        self.nc = bass.Bass("TRN2", target_bir_lowering=False)
        self.es = contextlib.ExitStack()
        self.ops = {e: [] for e in self.ENG}
        self.dma_slots = {}
        self.out_toks = []

    def din(self, name, shape, dt=F32):
        return self.nc.dram_tensor(name, list(shape), dt, kind="ExternalInput").ap()

    def dout(self, name, shape, dt=F32):
        return self.nc.dram_tensor(name, list(shape), dt, kind="ExternalOutput").ap()

    def sb(self, name, shape, dt=F32):
        return self.es.enter_context(self.nc.sbuf_tensor('sb_' + name, list(shape), dt))

    def ps(self, name, shape=(128, 512), dt=F32):
        return self.es.enter_context(self.nc.psum_tensor('ps_' + name, list(shape), dt))

    def op(self, eng, fn, deps=()):
        o = dict(fn=fn, deps=[d for d in deps if d is not None], sig=False, dma=None)
        self.ops[eng].append(o)
        return ('c', eng, len(self.ops[eng]) - 1)

    def dma(self, eng, out, in_, deps=(), slot=None, is_out=False, **kw):
        if slot is None:
            slot = '_a%d' % len(self.dma_slots)
        cnt = self.dma_slots.get(slot, 0) + 1
        self.dma_slots[slot] = cnt
        o = dict(fn=lambda e: e.dma_start(out=out, in_=in_, **kw),
                 deps=[d for d in deps if d is not None], sig=False, dma=(slot, cnt))
        self.ops[eng].append(o)
        tok = ('d', slot, cnt)
        if is_out:
            self.out_toks.append(tok)
        return tok

    def mm(self, out, lhsT, rhs, start=True, stop=True, deps=()):
        return self.op('pe', lambda e: e.matmul(out, lhsT, rhs, start=start, stop=stop), deps)

    def tr(self, out, in_, ident, deps=()):
        return self.op('pe', lambda e: e.transpose(out, in_, ident), deps)

    def act(self, out, in_, func, bias=None, scale=None, deps=(), accum_out=None):
        kw = {}
        if bias is not None:
            kw['bias'] = bias
        if scale is not None:
            kw['scale'] = scale
        if accum_out is not None:
            kw['accum_out'] = accum_out
        return self.op('act', lambda e: e.activation(out, in_, func, **kw), deps)

    def tt(self, out, in0, in1, op, deps=(), eng='dve'):
        return self.op(eng, lambda e: e.tensor_tensor(out, in0, in1, op), deps)

    def ts(self, out, in0, s1, s2, op0, op1=None, deps=(), eng='dve'):
        if op1 is None:
            return self.op(eng, lambda e: e.tensor_scalar(out, in0, s1, None, op0), deps)
        return self.op(eng, lambda e: e.tensor_scalar(out, in0, s1, s2, op0, op1), deps)

    def stt(self, out, in0, scalar, in1, op0, op1, deps=()):
        return self.op('dve', lambda e: e.scalar_tensor_tensor(out, in0, scalar, in1, op0, op1), deps)

    def cp(self, out, in_, deps=(), eng='dve'):
        if eng == 'act':
            return self.op('act', lambda e: e.copy(out, in_), deps)
        return self.op(eng, lambda e: e.tensor_copy(out, in_), deps)

    def recip(self, out, in_, deps=()):
        return self.op('dve', lambda e: e.reciprocal(out, in_), deps)

    def memset(self, ap, val, deps=(), eng='dve'):
        return self.op(eng, lambda e: e.memset(ap, val), deps)

    def build(self):
        nc = self.nc
        for eng in self.ENG:
            for o in self.ops[eng]:
                for d in o['deps']:
                    if d[0] == 'c':
                        self.ops[d[1]][d[2]]['sig'] = True
        for eng in self.ENG:
            c = 0
            for o in self.ops[eng]:
                if o['sig'] and o['dma'] is None:
                    c += 1
                    o['cnt'] = c
        if self.out_toks:
            self.ops['sp'].append(dict(fn=None, deps=list(self.out_toks), sig=False, dma=None))
        self.check_deadlock()
        self.sem = {e: self.es.enter_context(nc.semaphore('s_' + e)) for e in self.ENG}
        self.dsem = {s: self.es.enter_context(nc.semaphore('d_' + s)) for s in self.dma_slots}
        block = self.es.enter_context(nc.Block())

        def replay(name, e):
            waited = {}
            for o in self.ops[name]:
                for d in o['deps']:
                    if d[0] == 'c':
                        key = ('c', d[1]); sem = self.sem[d[1]]; val = self.ops[d[1]][d[2]]['cnt']
                    else:
                        key = ('d', d[1]); sem = self.dsem[d[1]]; val = 16 * d[2]
                    if waited.get(key, 0) < val:
                        e.wait_ge(sem, val)
                        waited[key] = val
                if o['fn'] is None:
                    continue
                try:
                    ins = o['fn'](e)
                except Exception:
                    print('EMIT FAIL engine', name, 'op index', self.ops[name].index(o), 'dma', o['dma'])
                    raise
                if o['dma'] is not None:
                    ins.then_inc(self.dsem[o['dma'][0]], 16)
                elif o['sig']:
                    ins.then_inc(self.sem[name], 1)

        @block.tensor
        def _(e):
            replay('pe', e)

        @block.scalar
        def _(e):
            replay('act', e)

        @block.vector
        def _(e):
            replay('dve', e)

        @block.gpsimd
        def _(e):
            replay('pool', e)

        @block.sync
        def _(e):
            replay('sp', e)

        self.es.close()
        return nc

    def check_deadlock(self):
        ptr = {e: 0 for e in self.ENG}
        done_c = {e: -1 for e in self.ENG}
        dcnt = {s: 0 for s in self.dma_slots}
        total = sum(len(v) for v in self.ops.values())
        ndone = 0
        while ndone < total:
            prog = False
            for e in self.ENG:
                while ptr[e] < len(self.ops[e]):
                    o = self.ops[e][ptr[e]]
                    ok = True
                    for d in o['deps']:
                        if d[0] == 'c':
                            if done_c[d[1]] < d[2]:
                                ok = False; break
                        else:
                            if dcnt[d[1]] < d[2]:
                                ok = False; break
                    if not ok:
                        break
                    if o['dma'] is not None:
                        dcnt[o['dma'][0]] += 1
                    done_c[e] = ptr[e]
                    ptr[e] += 1; ndone += 1; prog = True
            if not prog:
                msg = []
                for e in self.ENG:
                    if ptr[e] < len(self.ops[e]):
                        o = self.ops[e][ptr[e]]
                        msg.append('%s blocked at op %d deps=%s' % (e, ptr[e], o['deps']))
                raise RuntimeError('DEADLOCK: ' + ' | '.join(msg))

    def run(self, in_maps, trace=False):
        nc = self.build()
        n = len(in_maps)
        res = run_bass_kernel_spmd(nc, in_maps, core_ids=list(range(n)), trace=trace)
        return res

import math
import ml_dtypes


def build_M():
    P = Prog()
    c_in = P.din('c', [128, 16])
    w_in = P.din('w', [2048, 3072])
    b_in = P.din('b', [1, 3072])
    o = P.dout('mod', [1, 3072])
    cs = P.sb('cs', [128, 16]); cond = P.sb('cond', [128, 16])
    wb = [P.sb('wb%d' % i, [128, 16, 512]) for i in range(2)]
    bb = P.sb('bb', [1, 3072]); ob = P.sb('ob', [1, 3072])
    ps = [P.ps('ps%d' % i) for i in range(2)]
    t_c = P.dma('sp', cs[:], c_in[:, :])
    t_b = P.dma('sp', bb[:], b_in[:, :])
    t_cond = P.act(cond[:], cs[:], AF.Silu, deps=[t_c])
    wv = w_in.rearrange("(kc p) n -> p kc n", p=128)
    ev = [None, None]
    outs = []
    for j in range(6):
        t_w = P.dma('sp' if j % 2 == 0 else 'act', wb[j % 2][:], wv[:, :, j * 512:(j + 1) * 512], deps=[ev[j % 2]], slot='w%d' % (j % 2))
        last = None
        for kc in range(16):
            last = P.mm(ps[j % 2][0:1, :], cond[:, kc:kc + 1], wb[j % 2][:, kc, :], start=(kc == 0), stop=(kc == 15),
                        deps=[t_w, t_cond, ev[j % 2]] if kc == 0 else [])
        ev[j % 2] = P.tt(ob[:, j * 512:(j + 1) * 512], ps[j % 2][0:1, :], bb[:, j * 512:(j + 1) * 512], ALU.add, deps=[last, t_b])
        outs.append(ev[j % 2])
    P.dma('sp', o[:, :], ob[:], deps=outs, is_out=True)
    return P


TWO_PI = 2 * math.pi
MAGIC = 12582912.0

def consts_A():
    invf = (10000.0 ** (-np.arange(0, 128, 2, dtype=np.float32) / 128)).astype(np.float32)
    invf = np.concatenate([invf, invf])[None, :].astype(np.float32)
    sign = np.concatenate([-np.ones(64), np.ones(64)]).astype(np.float32)[:, None]
    swap = np.zeros((128, 128), np.float32)
    for e in range(128):
        swap[(e + 64) % 128, e] = 1.0
    return dict(invf=invf, sign=sign, swap=swap)

def build_A(cc_list=None):
    P = Prog()
    xT = P.din('xT', [2048, 1024]); modv = P.din('modv', [128, 6, 16]); g1n = P.din('gn', [128, 16])
    w = P.din('w', [2048, 7680]); pos = P.din('pos', [1, 1024], I32)
    invf_d = P.din('invf', [1, 128]); sign_d = P.din('sign', [128, 1]); swap_d = P.din('swap', [128, 128])
    qT = P.dout('qT', [1536, 1024], BF16); kT = P.dout('kT', [1536, 1024], BF16); vT = P.dout('vT', [1536, 1024], BF16)
    xrT = P.dout('xrT', [1536, 1024]); grT = P.dout('grT', [1536, 1024])

    x = P.sb('x', [128, 16, 1024]); h = P.sb('h', [128, 16, 1024], BF16); sq = h
    mods = P.sb('mods', [128, 6, 16]); gn = P.sb('gns', [128, 16]); G = P.sb('G', [128, 16])
    ones = P.sb('ones', [128, 128], BF16); swp = P.sb('swp', [128, 128], BF16)
    invf = P.sb('invfs', [1, 128]); sign = P.sb('signs', [128, 1]); posi = P.sb('posi', [1, 1024], I32); posf = P.sb('posf', [1, 1024])
    rstd = P.sb('rstd', [128, 1024]); tmp = [P.sb('tmp%d' % i, [128, 1024]) for i in range(2)]
    cosT = P.sb('cosT', [128, 1024]); sinT = P.sb('sinT', [128, 1024])
    r1 = tmp[0]; r2 = tmp[1]
    wb = [P.sb('wb%d' % i, [128, 16, 512], BF16) for i in range(2)]
    tb = [P.sb('tb%d' % i, [128, 512], BF16) for i in range(2)]
    ta = [P.sb('ta%d' % i, [128, 512]) for i in range(2)]
    tb2 = [P.sb('tbb%d' % i, [128, 512]) for i in range(2)]
    ost = [P.sb('ost%d' % i, [128, 1024], BF16) for i in range(2)]
    ost32 = [P.sb('ost32_%d' % i, [128, 1024]) for i in range(2)]
    pm = [P.ps('pm%d' % i) for i in range(4)]
    pr = [P.ps('pr%d' % i) for i in range(2)]
    pz = [P.ps('pz%d' % i) for i in range(2)]

    t_x = [P.dma('sp', x[:, kc * 4:(kc + 1) * 4, :], xT.rearrange("(kc p) t -> p kc t", p=128)[:, kc * 4:(kc + 1) * 4, :]) for kc in range(4)]
    t_m = P.dma('act', mods[:], modv[:, :, :]); t_g = P.dma('act', gn[:], g1n[:, :])
    t_if = P.dma('act', invf[:], invf_d[:, :]); t_sg = P.dma('act', sign[:], sign_d[:, :])
    t_pos = P.dma('act', posi[:], pos[:, :])
    t_sw = P.dma('pool', swp[:], swap_d[:, :])
    t_ones = P.memset(ones[:], 1.0)
    wv = w.rearrange("(kc p) n -> p kc n", p=128)
    NWB = 15
    t_w = [None] * NWB
    wfree = [None, None]
    def load_w(j, deps):
        t_w[j] = P.dma('pool', wb[j % 2][:], wv[:, :, j * 512:(j + 1) * 512], deps=deps, slot='w%d' % (j % 2))
    if cc_list is None:
        load_w(0, []); load_w(1, [])
    else:
        for jj in sorted(set(c // 4 for c in cc_list)):
            load_w(jj, [wfree[jj % 2]])
    t_G = P.stt(G[:], mods[:, 1, :], 1.0, gn[:], ALU.add, ALU.mult, deps=[t_m, t_g])
    t_pf = P.cp(posf[:], posi[:], deps=[t_pos])
    t_ang = []
    for hf in range(2):
        t_ang.append(P.mm(pz[hf][:], invf[:], posf[:, hf * 512:(hf + 1) * 512], deps=[t_if, t_pf]))
    def reduce_sin(dst, off, deps_extra):
        toks = []
        for hf in range(2):
            sl = slice(hf * 512, (hf + 1) * 512)
            a0 = P.ts(r1[:, sl], pz[hf][:], 1.0 / TWO_PI, off / TWO_PI, ALU.mult, ALU.add, deps=[t_ang[hf]] + deps_extra)
            a = P.ts(r1[:, sl], r1[:, sl], MAGIC, None, ALU.add, deps=[a0])
            b = P.ts(r1[:, sl], r1[:, sl], MAGIC, None, ALU.subtract, deps=[a])
            c = P.stt(r2[:, sl], r1[:, sl], -TWO_PI, pz[hf][:], ALU.mult, ALU.add, deps=[b])
            d = P.ts(r2[:, sl], r2[:, sl], off + math.pi, TWO_PI - 1e-5, ALU.add, ALU.min, deps=[c])
            e = P.ts(r2[:, sl], r2[:, sl], 1e-5, -math.pi, ALU.max, ALU.add, deps=[d])
            toks.append(e)
        return toks
    tk = reduce_sin(sinT, 0.0, [])
    t_sin = P.act(sinT[:], r2[:], AF.Sin, scale=sign[:], deps=tk + [t_sg])
    tk = reduce_sin(cosT, math.pi / 2, [t_sin])
    t_cos = P.act(cosT[:], r2[:], AF.Sin, deps=tk)
    t_sq = [P.act(sq[:, kc, :], x[:, kc, :], AF.Square, deps=[t_x[kc // 4]]) for kc in range(16)]
    t_ss = []
    for hf in range(2):
        last = None
        for kc in range(16):
            last = P.mm(pm[hf][:], ones[:], sq[:, kc, hf * 512:(hf + 1) * 512], start=(kc == 0), stop=(kc == 15), deps=[t_sq[kc], t_ones])
        t_ss.append(last)
    t_rs = []
    for hf in range(2):
        sl = slice(hf * 512, (hf + 1) * 512)
        a = P.act(rstd[:, sl], pm[hf][:], AF.Sqrt, bias=1e-6, scale=1.0 / 2048, deps=[t_ss[hf]])
        t_rs.append(P.recip(rstd[:, sl], rstd[:, sl], deps=[a]))
    t_h = []
    tfree = [t_cos, t_cos]
    for kc in range(16):
        a = P.tt(tmp[kc % 2][:], x[:, kc, :], rstd[:], ALU.mult, deps=t_rs + [t_x[kc // 4], tfree[kc % 2]])
        b = P.act(h[:, kc, :], tmp[kc % 2][:], AF.Identity, bias=mods[:, 0, kc:kc + 1], scale=G[:, kc:kc + 1], deps=[a, t_G])
        tfree[kc % 2] = b
        t_h.append(b)
    bank_free = [[t_rs[0]], [t_rs[1]], [], []]
    tb_free = [None, None]; pr_free = [None, None]; ta_free = [None, None]
    ost_free = [None, None]; ost32_free = [None, None]
    pending = None
    u = 0
    outs = [qT, kT, vT, xrT, grT]
    for cc in (cc_list if cc_list is not None else range(60)):
        j = cc // 4
        sec = cc // 12; row0 = (cc % 12) * 128
        half_toks = []
        for hf in range(2):
            sl = slice(hf * 512, (hf + 1) * 512)
            mb = u % 4
            last = None
            for kc in range(16):
                deps = [t_h[kc]]
                if kc == 0:
                    deps += [t_w[j]] + bank_free[mb]
                last = P.mm(pm[mb][:], wb[j % 2][:, kc, (cc % 4) * 128:(cc % 4 + 1) * 128], h[:, kc, sl], start=(kc == 0), stop=(kc == 15), deps=deps)
            if cc % 4 == 3 and hf == 1:
                if j + 2 < NWB and cc_list is None:
                    load_w(j + 2, [last])
            if sec < 2:
                i2 = u % 2
                e1 = P.cp(tb[i2][:], pm[mb][:], deps=[last, tb_free[i2]], eng='act')
                e3 = P.tt(ta[i2][:], pm[mb][:], cosT[:, sl], ALU.mult, deps=[last, t_cos, ta_free[i2], e1])
                bank_free[mb] = [e1, e3]
                if pending is not None:
                    pending()
                def mk(i2=i2, e1=e1, e3=e3, sl=sl, slot=cc % 2, hf=hf):
                    def f():
                        e2 = P.mm(pr[i2][:], swp[:], tb[i2][:], deps=[e1, t_sw, pr_free[i2]])
                        tb_free[i2] = e2
                        e4 = P.tt(tb2[i2][:], pr[i2][:], sinT[:, sl], ALU.mult, deps=[e2, t_sin])
                        pr_free[i2] = e4
                        e5 = P.tt(ost[slot][:, sl], ta[i2][:], tb2[i2][:], ALU.add, deps=[e3, e4, ost_free[slot]])
                        ta_free[i2] = e5
                        return e5
                    return f
                g = mk()
                res = {}
                def pend(g=g, res=res):
                    res['t'] = g()
                pending = pend
                half_toks.append(res)
            else:
                if pending is not None:
                    pending(); pending = None
                if sec == 2:
                    slot = cc % 2
                    e = P.cp(ost[slot][:, sl], pm[mb][:], deps=[last, ost_free[slot]], eng='act')
                else:
                    slot = cc % 2
                    e = P.cp(ost32[slot][:, sl], pm[mb][:], deps=[last, ost32_free[slot]], eng=('act' if hf == 0 else 'dve'))
                bank_free[mb] = [e]
                half_toks.append({'t': e})
            u += 1
        def mkout(cc=cc, sec=sec, row0=row0, half_toks=half_toks):
            def f():
                slot = cc % 2
                deps = [r['t'] for r in half_toks]
                if sec < 3:
                    t = P.dma('sp', outs[sec][row0:row0 + 128, :], ost[slot][:], deps=deps, slot='o%d' % slot, is_out=True)
                    ost_free[slot] = t
                else:
                    t = P.dma('sp', outs[sec][row0:row0 + 128, :], ost32[slot][:], deps=deps, slot='o32_%d' % slot, is_out=True)
                    ost32_free[slot] = t
            return f
        if sec < 2:
            prev_pending = pending
            outf = mkout()
            def pend2(prev_pending=prev_pending, outf=outf):
                prev_pending(); outf()
            pending = pend2
        else:
            mkout()()
    if pending is not None:
        pending()
    return P

def prep_A(core, x, mod_l, norm_g, w_in_l, positions):
    T0 = core * 1024
    d = dict(xT=np.ascontiguousarray(x[0, T0:T0 + 1024, :].T),
             modv=np.ascontiguousarray(mod_l.reshape(6, 16, 128).transpose(2, 0, 1)),
             gn=np.ascontiguousarray(norm_g.reshape(16, 128).T),
             w=w_in_l, pos=np.ascontiguousarray(positions[:, T0:T0 + 1024]))
    d.update(consts_A())
    return d


NEG = -1e30
NT = 53

def attn_tiles():
    groups = []
    for gi in range(4):
        g = []
        for i in (2 * gi, 2 * gi + 1):
            bank = i // 4; c0 = (i % 4) * 128
            g.append(dict(ks=2048 + 128 * (i - 1), kst=1, kp=128, qs=128 * i, qst=1, nq=128, m=i, vt=i, outs=[(bank, c0, 1, 128, 0)]))
            g.append(dict(ks=2048 + 128 * i, kst=1, kp=128, qs=128 * i, qst=1, nq=128, m=11, vt=i + 1, outs=[(bank, c0, 1, 128, 0)]))
        groups.append(g)
    for r in range(4):
        g = []
        for i in range(2):
            g.append(dict(ks=2048 + 512 * (i - 1) + r, kst=4, kp=128, qs=512 * i + r, qst=4, nq=128, m=8 + i, vt=9 + r * 3 + i, outs=[(i, r, 4, 128, 0)]))
            g.append(dict(ks=2048 + 512 * i + r, kst=4, kp=128, qs=512 * i + r, qst=4, nq=128, m=11, vt=9 + r * 3 + i + 1, outs=[(i, r, 4, 128, 0)]))
        groups.append(g)
    for r4 in range(4):
        g = []
        for r in range(4 * r4, 4 * r4 + 4):
            outs = [(0, r, 16, 32, 0), (1, r, 16, 32, 32)]
            g.append(dict(ks=r, kst=16, kp=128, qs=r, qst=16, nq=64, m=10, vt=21 + 2 * r, outs=outs))
            g.append(dict(ks=2048 + r, kst=16, kp=64, qs=r, qst=16, nq=64, m=11, vt=21 + 2 * r + 1, outs=outs))
        groups.append(g)
    return groups

def sl(start, step, n):
    return slice(start, start + step * (n - 1) + 1, step)

def emit_attn(P, q_d, k_d, v_d, mask_d, ident_d, out_d, psS, psN, psD, after_head=None):
    qb = [P.sb('qb%d' % i, [128, 1024], BF16) for i in range(2)]
    kb = [P.sb('kb%d' % i, [128, 3072], BF16) for i in range(2)]
    vb = [P.sb('vb%d' % i, [128, NT, 128], BF16) for i in range(2)]
    masks = P.sb('masks', [128, 12, 128], BF16); ident = P.sb('ident', [128, 128], BF16); ones = P.sb('onesb', [128, 128], BF16)
    pb = [P.sb('pb%d' % i, [128, 512], BF16) for i in range(2)]
    rden = P.sb('rden', [128, 1024]); ao = [P.sb('ao%d' % i, [128, 1024]) for i in range(2)]
    t_mask = P.dma('act', masks[:], mask_d[:, :, :]); t_id = P.dma('act', ident[:], ident_d[:, :])
    t_ones = P.memset(ones[:], 1.0)
    groups = attn_tiles()
    scale = 1.0 / math.sqrt(128.0)
    load_tok = [None, None]; buf_free = [[], []]
    def load(h):
        b = h % 2
        t1 = P.dma('sp', qb[b][:], q_d[:, h, :], deps=buf_free[b], slot='q%d' % b)
        t2 = P.dma('sp', kb[b][:], k_d[:, h, :], deps=buf_free[b], slot='k%d' % b)
        t3 = P.dma('sp', vb[b][:], v_d[:, h, :, :], deps=buf_free[b], slot='v%d' % b)
        load_tok[b] = [t1, t2, t3]
    load(0); load(1)
    S_free = [None, None]; pb_free = [None, None]
    nd_free = []
    ao_free = [None, None]
    gcount = 0
    for h in range(12):
        b = h % 2
        first_in_bank = {('N', 0): True, ('N', 1): True, ('D', 0): True, ('D', 1): True}
        pend = None
        last_pv = None
        def do_pv(g, sb_i, t_exp):
            nonlocal last_pv
            c0 = 0
            for t in g:
                for (bank, st, step, n, pc0) in t['outs']:
                    for kind, ps, lhs in (('N', psN, vb[b][0:t['kp'], t['vt'], :]), ('D', psD, ones[0:t['kp'], :])):
                        fst = first_in_bank[(kind, bank)]
                        first_in_bank[(kind, bank)] = False
                        last_pv = P.mm(ps[bank][:, sl(st, step, n)], lhs, pb[sb_i][0:t['kp'], c0 + pc0:c0 + pc0 + n],
                                       start=fst, stop=False, deps=[t_exp, t_ones] + (nd_free if fst else []))
                c0 += t['nq']
            return last_pv
        for g in groups:
            si = gcount % 2
            c0 = 0
            last = None
            for ti, t in enumerate(g):
                deps = load_tok[b] + [S_free[si], t_mask, t_id] if ti == 0 else []
                P.mm(psS[si][0:t['kp'], c0:c0 + t['nq']], kb[b][:, sl(t['ks'], t['kst'], t['kp'])], qb[b][:, sl(t['qs'], t['qst'], t['nq'])],
                     start=True, stop=False, deps=deps)
                mslice = masks[0:t['kp'], t['m'], 0:t['nq']]
                last = P.mm(psS[si][0:t['kp'], c0:c0 + t['nq']], ident[0:t['kp'], 0:t['kp']], mslice, start=False, stop=True)
                c0 += t['nq']
            t_exp = P.act(pb[si][:, 0:c0], psS[si][:, 0:c0], AF.Exp, scale=scale, deps=[last, pb_free[si]])
            S_free[si] = t_exp
            if pend is not None:
                pg, psi, ptexp = pend
                pb_free[psi] = do_pv(pg, psi, ptexp)
            pend = (g, si, t_exp)
            gcount += 1
        pg, psi, ptexp = pend
        pb_free[psi] = do_pv(pg, psi, ptexp)
        buf_free[b] = [last_pv]
        if h + 2 < 12:
            load(h + 2)
        oi = h % 2
        evs = []
        for bank in range(2):
            cs = slice(bank * 512, (bank + 1) * 512)
            a = P.recip(rden[:, cs], psD[bank][:], deps=[last_pv])
            e = P.tt(ao[oi][:, cs], psN[bank][:], rden[:, cs], ALU.mult, deps=[a, ao_free[oi]])
            evs.append(e)
        nd_free = evs
        ao_free[oi] = P.dma('act', out_d[h * 128:(h + 1) * 128, :], ao[oi][:], deps=evs, slot='ao%d' % oi, is_out=True)
        if after_head is not None:
            after_head(h)

def build_B(do_lru=True):
    P = Prog()
    q_d = P.din('q', [128, 12, 1024], BF16); k_d = P.din('k', [128, 12, 3072], BF16); v_d = P.din('v', [128, 12, NT, 128], BF16)
    mask_d = P.din('masks', [128, 12, 128], BF16); ident_d = P.din('ident', [128, 128], BF16)
    out_d = P.dout('attnT', [1536, 1024])
    psS = [P.ps('psS%d' % i) for i in range(2)]; psN = [P.ps('psN%d' % i) for i in range(2)]; psD = [P.ps('psD%d' % i) for i in range(2)]
    blk = None
    if do_lru:
        psG = [P.ps('psG%d' % i) for i in range(2)]
        blk = emit_lru(P, psG)
    emit_attn(P, q_d, k_d, v_d, mask_d, ident_d, out_d, psS, psN, psD, after_head=blk)
    return P

def emit_lru(P, psG):
    xr_d = P.din('xr', [128, 12, 1027]); cw_d = P.din('cw', [128, 12, 4]); vec_d = P.din('vecs', [128, 4, 12])
    wa_d = P.din('wa', [128, 12, 128]); wx_d = P.din('wx', [128, 12, 128]); pos_d = P.din('pos', [1, 1024], I32)
    hl_d = P.dout('hloc', [1536, 1024]); pp_d = P.dout('pprod', [1536, 1024])
    xr = P.sb('xr', [128, 12, 1027]); cw = P.sb('cw', [128, 12, 4]); vecs = P.sb('vecs', [128, 4, 12])
    wa = P.sb('wa', [128, 12, 128], BF16); wx = P.sb('wx', [128, 12, 128], BF16)
    posi = P.sb('lposi', [1, 1024], I32); nzr = P.sb('nzr', [1, 1024]); nz = P.sb('nz', [128, 1024]); ones1 = P.sb('ones1', [1, 128])
    sca = P.sb('sca', [128, 12]); sca2 = P.sb('sca2', [128, 12]); zeros = P.sb('zeros', [128, 1024])
    xc = P.sb('xc', [128, 1024]); xcb = P.sb('xcb', [128, 1024], BF16)
    rb = P.sb('rb', [128, 1024]); ib = P.sb('ib', [128, 1024]); ab = P.sb('ab', [128, 1024]); mb_ = P.sb('mb', [128, 1024])
    ho = [P.sb('ho%d' % i, [128, 1024]) for i in range(2)]; po = [P.sb('po%d' % i, [128, 1024]) for i in range(2)]
    t_xr = P.dma('sp', xr[:], xr_d[:, :, :]); t_cw = P.dma('sp', cw[:], cw_d[:, :, :]); t_v = P.dma('sp', vecs[:], vec_d[:, :, :])
    t_wa = P.dma('pool', wa[:], wa_d[:, :, :]); t_wx = P.dma('pool', wx[:], wx_d[:, :, :]); t_pos = P.dma('sp', posi[:], pos_d[:, :])
    t_z = P.memset(zeros[:], 0.0, eng='pool'); t_o1 = P.memset(ones1[:], 1.0, eng='pool')
    a = P.cp(nzr[:], posi[:], deps=[t_pos])
    a = P.ts(nzr[:], nzr[:], 0.0, None, ALU.not_equal, deps=[a])
    t_nz = []
    for hf in range(2):
        cs = slice(hf * 512, (hf + 1) * 512)
        m = P.mm(psG[hf][:], ones1[:], nzr[:, cs], deps=[a, t_o1])
        t_nz.append(P.cp(nz[:, cs], psG[hf][:], deps=[m]))
    s1 = P.act(sca[:], vecs[:, 3, :], AF.Exp, scale=-1.0, deps=[t_v])
    s2 = P.act(sca[:], sca[:], AF.Ln, bias=1.0, deps=[s1])
    s3 = P.ts(sca2[:], sca[:], -16.0, None, ALU.mult, deps=[s2])
    s4 = P.ts(sca[:], sca[:], -8.0, None, ALU.mult, deps=[s3])
    g_free = t_nz
    prev = []
    ho_free = [None, None]; po_free = [None, None]
    state = dict(g_free=g_free, prev=prev)
    def do_block(b):
        g_free = state['g_free']; prev = state['prev']
        c = P.act(xc[:], xr[:, b, 3:1027], AF.Identity, bias=vecs[:, 0, b:b + 1], scale=cw[:, b, 3:4], deps=[t_xr, t_cw, t_v] + prev)
        for k in range(3):
            c = P.stt(xc[:], xr[:, b, k:k + 1024], cw[:, b, k:k + 1], xc[:], ALU.mult, ALU.add, deps=[c])
        cb = P.cp(xcb[:], xc[:], deps=[c] + prev, eng='pool')
        gr_ = []
        for gi, (wt, bias_i, dst, tw) in enumerate(((wa, 1, rb, t_wa), (wx, 2, ib, t_wx))):
            evs = []
            for hf in range(2):
                cs = slice(hf * 512, (hf + 1) * 512)
                m = P.mm(psG[hf][:], wt[:, b, :], xcb[:, cs], deps=[cb, tw] + (g_free if isinstance(g_free, list) else [g_free]))
                evs.append(P.act(dst[:, cs], psG[hf][:], AF.Sigmoid, bias=vecs[:, bias_i, b:b + 1], deps=[m] + prev))
            g_free = evs
            gr_.append(evs)
        ta_ = P.act(ab[:], rb[:], AF.Exp, scale=sca[:, b:b + 1], deps=gr_[0] + [s4] + prev)
        tm = P.act(mb_[:], rb[:], AF.Exp, scale=sca2[:, b:b + 1], deps=gr_[0] + [s4] + prev)
        tm = P.act(mb_[:], mb_[:], AF.Sqrt, scale=-1.0, bias=1.0, deps=[tm])
        ta2 = P.tt(ab[:], ab[:], nz[:], ALU.mult, deps=[ta_] + t_nz)
        tm = P.stt(mb_[:], mb_[:], -1.0, nz[:], ALU.add, ALU.mult, deps=[tm] + t_nz)
        tm = P.ts(mb_[:], mb_[:], 1.0, None, ALU.add, deps=[tm])
        tb_ = P.tt(ib[:], ib[:], xc[:], ALU.mult, deps=gr_[1] + [c])
        tb_ = P.tt(ib[:], ib[:], mb_[:], ALU.mult, deps=[tb_, tm])
        oi = b % 2
        sc1 = P.op('dve', lambda e, oi=oi: e.tensor_tensor_scan(ho[oi][:], ab[:], ib[:], 0.0, ALU.mult, ALU.add), deps=[ta2, tb_, ho_free[oi]])
        sc2 = P.op('dve', lambda e, oi=oi: e.tensor_tensor_scan(po[oi][:], ab[:], zeros[:], 1.0, ALU.mult, ALU.add), deps=[ta2, t_z, po_free[oi]])
        ho_free[oi] = P.dma('sp', hl_d[b * 128:(b + 1) * 128, :], ho[oi][:], deps=[sc1], slot='ho%d' % oi, is_out=True)
        po_free[oi] = P.dma('sp', pp_d[b * 128:(b + 1) * 128, :], po[oi][:], deps=[sc2], slot='po%d' % oi, is_out=True)
        state['g_free'] = g_free; state['prev'] = [sc1, sc2, cb]
    return do_block

def bf(a):
    return np.ascontiguousarray(a).astype(ml_dtypes.bfloat16)

def consts_B(core):
    kk = np.arange(128)[:, None]; qi = np.arange(128)[None, :]
    A = np.where(kk >= qi, 0.0, NEG).astype(np.float32)
    Bm = np.where(kk <= qi, 0.0, NEG).astype(np.float32)
    allneg = np.full((128, 128), NEG, np.float32)
    m = np.zeros((128, 12, 128), np.float32)
    for i in range(8):
        m[:, i, :] = allneg if (core == 0 and i == 0) else A
    for i in range(2):
        m[:, 8 + i, :] = allneg if (core == 0 and i == 0) else A
    if core == 0:
        m[:, 10, :] = allneg
    elif core == 1:
        mm_ = A.copy(); mm_[:64, :] = NEG
        m[:, 10, :] = mm_
    else:
        m[:, 10, :] = A
    m[:, 11, :] = Bm
    return dict(masks=bf(m), ident=bf(np.eye(128, dtype=np.float32)))

def prep_B_attn(core, qT_all, kT_all, vT_all):
    T0 = core * 1024
    q = qT_all[:, T0:T0 + 1024].reshape(12, 128, 1024).transpose(1, 0, 2)
    kpad = np.zeros((1536, 2048 + 8192), dtype=kT_all.dtype); kpad[:, 2048:] = kT_all
    k = kpad[:, T0:T0 + 3072].reshape(12, 128, 3072).transpose(1, 0, 2)
    vtok = np.zeros((2048 + 8192, 1536), dtype=vT_all.dtype); vtok[2048:] = vT_all.T
    base = T0 + 2048
    idx = np.zeros((NT, 128), np.int64); valid = np.ones((NT, 128), bool)
    for j in range(9):
        idx[j] = base + 128 * (j - 1) + np.arange(128)
    for r in range(4):
        for j in range(3):
            idx[9 + r * 3 + j] = base + 512 * (j - 1) + 4 * np.arange(128) + r
    for r in range(16):
        idx[21 + 2 * r] = base - 2048 + 16 * np.arange(128) + r
        ii = base + 16 * np.arange(128) + r
        valid[21 + 2 * r + 1, 64:] = False
        ii[64:] = 0
        idx[21 + 2 * r + 1] = ii
    vt = vtok[idx]
    vt[~valid] = 0
    v = vt.reshape(NT, 128, 12, 128).transpose(1, 2, 0, 3)
    d = dict(q=np.ascontiguousarray(q), k=np.ascontiguousarray(k), v=np.ascontiguousarray(v))
    d.update(consts_B(core))
    return d

def prep_B_lru(core, xrT_all, conv_w, conv_b, wa, ba, wx, bx, lam, positions):
    T0 = core * 1024
    xpad = np.zeros((1536, 3 + 8192), np.float32); xpad[:, 3:] = xrT_all
    xr = xpad[:, T0:T0 + 1027].reshape(12, 128, 1027).transpose(1, 0, 2)
    cw = conv_w.T.reshape(12, 128, 4).transpose(1, 0, 2)
    f = lambda v: v.reshape(12, 128).T
    vecs = np.stack([f(conv_b), f(ba), f(bx), f(lam)], axis=1)
    return dict(xr=np.ascontiguousarray(xr), cw=np.ascontiguousarray(cw), vecs=np.ascontiguousarray(vecs),
                wa=np.ascontiguousarray(wa.transpose(1, 0, 2)), wx=np.ascontiguousarray(wx.transpose(1, 0, 2)),
                pos=np.ascontiguousarray(positions[:, T0:T0 + 1024]))


def build_C1():
    P = Prog()
    xT_d = P.din('xT', [2048, 1024]); at_d = P.din('attnT', [1536, 1024]); hl_d = P.din('hloc', [1536, 1024])
    pp_d = P.din('pprod', [1536, 1024]); gr_d = P.din('grT', [1536, 1024])
    summ_d = P.din('summ', [128, 8, 2, 12]); sel_d = P.din('sel', [128, 8])
    modv_d = P.din('modv', [128, 6, 16]); g2n_d = P.din('g2n', [128, 16]); og_d = P.din('og', [128, 24])
    wo_d = P.din('wo', [3072, 2048]); rw_d = P.din('rw', [128, 16, 32]); rb_d = P.din('rb', [1, 32])
    utri_d = P.din('utri', [128, 128], BF16)
    x1_o = P.dout('x1T', [2048, 1024]); h2_o = P.dout('h2T', [2048, 1024], BF16)
    G_o = P.dout('G', [128, 8, 32]); pm_o = P.dout('pm', [128, 8, 32]); cnt_o = P.dout('cnt', [128, 32])

    x = P.sb('x', [128, 16, 1024])
    y = P.sb('y', [128, 24, 1024], BF16)
    st = [P.sb('st%d' % i, [128, 1024]) for i in range(2)]
    st2 = [P.sb('st2_%d' % i, [128, 1024]) for i in range(2)]
    st3 = [P.sb('st3_%d' % i, [128, 1024]) for i in range(2)]
    sq = [P.sb('sq%d' % i, [128, 1024], BF16) for i in range(2)]
    summ = P.sb('summ', [128, 8, 2, 12]); sel = P.sb('sel', [128, 8]); Hc = P.sb('Hc', [128, 12]); Hs = P.sb('Hs', [128, 12])
    mods = P.sb('mods', [128, 6, 16]); g2n = P.sb('g2n', [128, 16]); og = P.sb('og', [128, 24]); G2 = P.sb('G2', [128, 16])
    ones = P.sb('ones', [128, 128], BF16); utri = P.sb('utri', [128, 128], BF16); ones1 = P.sb('ones1', [1, 128])
    rw = P.sb('rw', [128, 16, 32]); rb = P.sb('rb', [1, 32])
    rstdA = P.sb('rstdA', [128, 1024]); rstdL = P.sb('rstdL', [128, 1024])
    wb = [P.sb('wb%d' % i, [128, 24, 256], BF16) for i in range(2)]
    t1 = [P.sb('t1_%d' % i, [128, 512]) for i in range(2)]; t2 = [P.sb('t2_%d' % i, [128, 512]) for i in range(2)]
    hb = [P.sb('hb%d' % i, [128, 1024], BF16) for i in range(2)]
    lg = P.sb('lg', [128, 8, 32]); top8 = P.sb('top8', [128, 8, 8]); nmax = P.sb('nmax', [128, 8]); maskf = P.sb('maskf', [128, 8, 32])
    maskb = P.sb('maskb', [128, 8, 32], BF16); ex = P.sb('ex', [128, 8, 32]); den = P.sb('den', [128, 8]); Gs = P.sb('Gs', [128, 8, 32]); pm = P.sb('pm', [128, 8, 32])
    ps = [P.ps('b%d' % i) for i in range(8)]

    t_x = [P.dma('sp', x[:, kc * 4:(kc + 1) * 4, :], xT_d.rearrange("(kc p) t -> p kc t", p=128)[:, kc * 4:(kc + 1) * 4, :]) for kc in range(4)]
    t_su = P.dma('act', summ[:], summ_d[:, :, :, :]); t_sel = P.dma('act', sel[:], sel_d[:, :])
    t_m = P.dma('act', mods[:], modv_d[:, :, :]); t_g2 = P.dma('act', g2n[:], g2n_d[:, :]); t_og = P.dma('act', og[:], og_d[:, :])
    t_rw = P.dma('act', rw[:], rw_d[:, :, :]); t_rb = P.dma('act', rb[:], rb_d[:, :]); t_ut = P.dma('act', utri[:], utri_d[:, :])
    t_ones = P.memset(ones[:], 1.0); t_o1 = P.memset(ones1[:], 1.0)
    wv = wo_d.rearrange("(kc p) n -> p kc n", p=128)
    t_w = [None] * 8
    def load_w(j, deps):
        t_w[j] = P.dma('pool', wb[j % 2][:], wv[:, :, j * 256:(j + 1) * 256], deps=deps, slot='w%d' % (j % 2))
    load_w(0, []); load_w(1, [])
    a = P.memset(Hc[:], 0.0, deps=[]); b = P.memset(Hs[:], 0.0)
    tH = [a, b]
    for c in range(8):
        s = P.stt(Hs[:], Hc[:], sel[:, c:c + 1], Hs[:], ALU.mult, ALU.add, deps=tH + [t_sel, t_su])
        u = P.tt(Hc[:], Hc[:], summ[:, c, 0, :], ALU.mult, deps=[s])
        u = P.tt(Hc[:], Hc[:], summ[:, c, 1, :], ALU.add, deps=[u])
        tH = [u]
    t_Hs = tH
    G2t = P.stt(G2[:], mods[:, 4, :], 1.0, g2n[:], ALU.add, ALU.mult, deps=[t_m, t_g2])
    st_free = [None, None]; st2_free = [None, None]; st3_free = [None, None]; sq_free = [None, None]
    lastA = [None, None]; lastL = [None, None]
    t_y = []
    for ci in range(24):
        i = ci % 2
        if ci < 12:
            b_ = ci
            ld = P.dma('sp', st[i][:], at_d[b_ * 128:(b_ + 1) * 128, :], deps=[st_free[i]], slot='st%d' % i)
            src_ready = [ld]
        else:
            b_ = ci - 12
            l1 = P.dma('sp', st[i][:], hl_d[b_ * 128:(b_ + 1) * 128, :], deps=[st_free[i]], slot='st%d' % i)
            l2 = P.dma('sp', st2[i][:], pp_d[b_ * 128:(b_ + 1) * 128, :], deps=[st2_free[i]], slot='st2_%d' % i)
            l3 = P.dma('sp', st3[i][:], gr_d[b_ * 128:(b_ + 1) * 128, :], deps=[st3_free[i]], slot='st3_%d' % i)
            hf_ = P.stt(st[i][:], st2[i][:], Hs[:, b_:b_ + 1], st[i][:], ALU.mult, ALU.add, deps=[l1, l2] + t_Hs)
            ge = P.act(st2[i][:], st3[i][:], AF.Gelu_apprx_tanh, deps=[l3, hf_])
            st3_free[i] = ge
            lr = P.tt(st[i][:], st[i][:], st2[i][:], ALU.mult, deps=[hf_, ge])
            st2_free[i] = lr
            src_ready = [lr]
        s_ = P.act(sq[i][:], st[i][:], AF.Square, deps=src_ready + [sq_free[i]])
        mmt = None
        for hf in range(2):
            bank = (0 if ci < 12 else 2) + hf
            mmt = P.mm(ps[bank][:], ones[:], sq[i][:, hf * 512:(hf + 1) * 512], start=(ci % 12 == 0), stop=(ci % 12 == 11), deps=[s_, t_ones])
            if ci < 12: lastA[hf] = mmt
            else: lastL[hf] = mmt
        sq_free[i] = mmt
        yy = P.act(y[:, ci, :], st[i][:], AF.Copy, scale=og[:, ci:ci + 1], deps=src_ready + [t_og])
        st_free[i] = [yy, s_]
        st_free[i] = yy
        st_free[i] = P.op('pool', lambda e: e.memset(ones1[0:1, 0:1], 1.0), deps=[yy, s_])
        t_y.append(yy)
    t_rA = []; t_rL = []
    for hf in range(2):
        cs = slice(hf * 512, (hf + 1) * 512)
        a = P.act(rstdA[:, cs], ps[hf][:], AF.Sqrt, bias=1e-6, scale=1.0 / 1536, deps=[lastA[hf]])
        t_rA.append(P.recip(rstdA[:, cs], rstdA[:, cs], deps=[a]))
        a = P.act(rstdL[:, cs], ps[2 + hf][:], AF.Sqrt, bias=1e-6, scale=1.0 / 1536, deps=[lastL[hf]])
        t_rL.append(P.recip(rstdL[:, cs], rstdL[:, cs], deps=[a]))
    bank_free = {4: None, 5: None, 6: None, 7: None}
    tfree = [None, None]
    t_x1 = []
    u = 0
    for dmc in range(16):
        j = dmc // 2
        for hf in range(2):
            cs = slice(hf * 512, (hf + 1) * 512)
            bA = 4 + (u % 2) * 2; bL = bA + 1
            la = None; ll = None
            for ci in range(12):
                la = P.mm(ps[bA][:], wb[j % 2][:, ci, (dmc % 2) * 128:(dmc % 2 + 1) * 128], y[:, ci, cs], start=(ci == 0), stop=(ci == 11),
                          deps=[t_y[ci]] + ([t_w[j], bank_free[bA]] if ci == 0 else []))
            for ci in range(12, 24):
                ll = P.mm(ps[bL][:], wb[j % 2][:, ci, (dmc % 2) * 128:(dmc % 2 + 1) * 128], y[:, ci, cs], start=(ci == 12), stop=(ci == 23),
                          deps=[t_y[ci]] + ([bank_free[bL]] if ci == 12 else []))
            if dmc % 2 == 1 and hf == 1 and j + 2 < 8:
                load_w(j + 2, [ll])
            i2 = u % 2
            e1 = P.tt(t1[i2][:], ps[bA][:], rstdA[:, cs], ALU.mult, deps=[la, t_rA[hf], tfree[i2]])
            e2 = P.tt(t2[i2][:], ps[bL][:], rstdL[:, cs], ALU.mult, deps=[ll, t_rL[hf], tfree[i2]])
            bank_free[bA] = e1; bank_free[bL] = e2
            e3 = P.tt(t1[i2][:], t1[i2][:], t2[i2][:], ALU.add, deps=[e1, e2], eng='pool')
            e4 = P.stt(x[:, dmc, cs], t1[i2][:], mods[:, 2, dmc:dmc + 1], x[:, dmc, cs], ALU.mult, ALU.add, deps=[e3, t_m, t_x[dmc // 4]])
            tfree[i2] = e4
            t_x1.append(e4)
            u += 1
    for kc in range(4):
        P.dma('sp', x1_o.rearrange("(kc p) t -> p kc t", p=128)[:, kc * 4:(kc + 1) * 4, :], x[:, kc * 4:(kc + 1) * 4, :], deps=t_x1[kc * 8:(kc + 1) * 8], is_out=True)
    sq_free = [t_y[-1], t_y[-1]]
    lastN = [None, None]
    for kc in range(16):
        i = kc % 2
        s_ = P.act(sq[i][:], x[:, kc, :], AF.Square, deps=t_x1[2 * kc:2 * kc + 2] + [sq_free[i]])
        for hf in range(2):
            lastN[hf] = P.mm(ps[hf][:], ones[:], sq[i][:, hf * 512:(hf + 1) * 512], start=(kc == 0), stop=(kc == 15), deps=[s_] + (t_rA + t_rL if kc == 0 else []))
        sq_free[i] = lastN[1]
    rstd2 = rstdA
    t_r2 = []
    for hf in range(2):
        cs = slice(hf * 512, (hf + 1) * 512)
        a = P.act(rstd2[:, cs], ps[hf][:], AF.Sqrt, bias=1e-6, scale=1.0 / 2048, deps=[lastN[hf]] + t_x1)
        t_r2.append(P.recip(rstd2[:, cs], rstd2[:, cs], deps=[a]))
    LG = ps[2]
    st_free = [None, None]; st2_free = [None, None]; hb_free = [None, None]
    last_r = None
    for kc in range(16):
        i = kc % 2
        a = P.tt(st[i][:], x[:, kc, :], rstd2[:], ALU.mult, deps=t_r2 + [st_free[i]])
        hh = P.act(st2[i][:], st[i][:], AF.Identity, bias=mods[:, 3, kc:kc + 1], scale=G2[:, kc:kc + 1], deps=[a, G2t, st2_free[i]])
        st_free[i] = hh
        for g in range(8):
            last_r = P.mm(LG[:, g * 32:(g + 1) * 32], st2[i][:, g * 128:(g + 1) * 128], rw[:, kc, :], start=(kc == 0 and g == 0), stop=False,
                          deps=[hh, t_rw] + t_rL)
        cb = P.cp(hb[i][:], st2[i][:], deps=[hh, hb_free[i]], eng='pool')
        st2_free[i] = P.op('pool', lambda e: e.memset(ones1[0:1, 0:1], 1.0), deps=[cb, last_r])
        hb_free[i] = P.dma('sp', h2_o[kc * 128:(kc + 1) * 128, :], hb[i][:], deps=[cb], slot='hb%d' % i, is_out=True)
    for g in range(8):
        last_r = P.mm(LG[:, g * 32:(g + 1) * 32], ones1[:, :], rb[:, :], start=False, stop=(g == 7), deps=[t_rb, t_o1])
    c0 = P.cp(lg[:].rearrange("p g e -> p (g e)"), LG[:, 0:256], deps=[last_r])
    tk = []
    for g in range(8):
        m = P.op('dve', lambda e, g=g: e.max(top8[:, g, :], lg[:, g, :]), deps=[c0])
        tk.append(m)
    n1 = P.ts(nmax[:], top8[:, :, 0], -1.0, None, ALU.mult, deps=tk)
    last = n1
    for g in range(8):
        mk = P.ts(maskf[:, g, :], lg[:, g, :], top8[:, g, 3:4], None, ALU.is_ge, deps=[last])
        e_ = P.act(ex[:, g, :], lg[:, g, :], AF.Exp, bias=nmax[:, g:g + 1], deps=[n1])
        em = P.tt(ex[:, g, :], ex[:, g, :], maskf[:, g, :], ALU.mult, deps=[mk, e_])
        dn = P.op('dve', lambda e, g=g: e.tensor_reduce(den[:, g:g + 1], ex[:, g, :], mybir.AxisListType.X, ALU.add), deps=[em])
        last = dn
    rd = P.recip(den[:], den[:], deps=[last])
    for g in range(8):
        last = P.ts(Gs[:, g, :], ex[:, g, :], den[:, g:g + 1], None, ALU.mult, deps=[rd])
    P.dma('sp', G_o[:, :, :], Gs[:], deps=[last], is_out=True)
    mb = P.cp(maskb[:], maskf[:], deps=[last])
    PO = ps[3]
    lp = None
    for g in range(8):
        lp = P.mm(PO[:, g * 32:(g + 1) * 32], utri[:], maskb[:, g, :], start=(g == 0), stop=False, deps=[mb, t_ut] + t_r2)
        for g2 in range(g):
            lp = P.mm(PO[:, g * 32:(g + 1) * 32], ones[:], maskb[:, g2, :], start=False, stop=False)
    lc_ = None
    for g in range(8):
        lc_ = P.mm(PO[:, 256:288], ones[:], maskb[:, g, :], start=False, stop=False, deps=[mb])
    cnts = P.sb('cnts', [128, 32])
    cc_ = P.cp(cnts[:], PO[:, 256:288], deps=[lc_])
    P.dma('sp', cnt_o[:, :], cnts[:], deps=[cc_], is_out=True)
    pmf = pm[:].rearrange("p g e -> p (g e)")
    a = P.stt(pmf, PO[:, 0:256], 1.0, maskf[:].rearrange("p g e -> p (g e)"), ALU.add, ALU.mult, deps=[lp, cc_])
    a = P.ts(pmf, pmf, -1.0, None, ALU.add, deps=[a])
    P.dma('sp', pm_o[:, :, :], pm[:], deps=[a], is_out=True)
    return P

def prep_C1(core, xT_core, attnT, hloc_all, pprod_all, grT_core, mod_l, norm2_g, attn_g, lru_g, w_out_l, router_w_l, router_b_l):
    summ = np.zeros((128, 8, 2, 12), np.float32)
    for c in range(8):
        summ[:, c, 0, :] = pprod_all[c][:, -1].reshape(12, 128).T
        summ[:, c, 1, :] = hloc_all[c][:, -1].reshape(12, 128).T
    sel = np.zeros((128, 8), np.float32); sel[:, core] = 1.0
    og = np.concatenate([attn_g.reshape(12, 128).T, lru_g.reshape(12, 128).T], axis=1)
    utri = (np.arange(128)[:, None] < np.arange(128)[None, :]).astype(np.float32)
    return dict(xT=xT_core, attnT=attnT, hloc=hloc_all[core], pprod=pprod_all[core], grT=grT_core, summ=summ, sel=sel,
                modv=np.ascontiguousarray(mod_l.reshape(6, 16, 128).transpose(2, 0, 1)), g2n=np.ascontiguousarray(norm2_g.reshape(16, 128).T),
                og=np.ascontiguousarray(og), wo=w_out_l, rw=np.ascontiguousarray(router_w_l.reshape(16, 128, 32).transpose(1, 0, 2)),
                rb=np.ascontiguousarray(router_b_l[None, :]), utri=utri.astype(ml_dtypes.bfloat16))


SCS = 1792
NBS = SCS // 128
NSC = 2
CAPT = SCS * NSC
NBT = CAPT // 128
BIG = 1000000.0
SUBS = [(0, 512), (512, 512), (1024, 512), (1536, 256)]

def _ind(P, kind, out, in_, idx_ap, deps, slot, **kw):
    P.dma_slots[slot] = P.dma_slots.get(slot, 0) + 1
    cnt = P.dma_slots[slot]
    if not hasattr(P, 'regcache'):
        P.regcache = {}
    if 'bounds_check' in kw:
        bval = kw.pop('bounds_check')
        okw = dict(kw)
        def fix(e, okw=okw, bval=bval):
            if bval not in P.regcache:
                P.regcache[bval] = e.to_reg(bval)
            d = dict(okw); d['bounds_check'] = P.regcache[bval]
            return d
    else:
        okw = dict(kw)
        def fix(e, okw=okw):
            return okw
    if kind == 'g':
        fn = lambda e: e.indirect_dma_start(out=out, out_offset=None, in_=in_, in_offset=bass.IndirectOffsetOnAxis(ap=idx_ap, axis=0), **fix(e))
    else:
        fn = lambda e: e.indirect_dma_start(out=out, out_offset=bass.IndirectOffsetOnAxis(ap=idx_ap, axis=0), in_=in_, in_offset=None, **fix(e))
    P.ops['pool'].append(dict(fn=fn, deps=[d for d in deps if d is not None], sig=False, dma=(slot, cnt)))
    return ('d', slot, cnt)

def _ind_old(P, kind, out, in_, idx_ap, deps, slot, **kw):
    cnt = 0
    if kind == 'g':
        fn = lambda e: e.indirect_dma_start(out=out, out_offset=None, in_=in_, in_offset=bass.IndirectOffsetOnAxis(ap=idx_ap, axis=0), **kw)
    else:
        fn = lambda e: e.indirect_dma_start(out=out, out_offset=bass.IndirectOffsetOnAxis(ap=idx_ap, axis=0), in_=in_, in_offset=None, **kw)
    P.ops['pool'].append(dict(fn=fn, deps=[d for d in deps if d is not None], sig=False, dma=(slot, cnt)))
    return ('d', slot, cnt)

def build_C3(NE=4, nsc=NSC):
    P = Prog(); nc = P.nc
    h_d = P.din('h2all', [8192, 2048], BF16); pm_d = P.din('pm', [128, 64, NE]); cnt_d = P.din('cnt', [128, 8, NE]); G_d = P.din('Grows', [8192, NE])
    tid_d = P.din('tid', [128, 64], I32); dum_d = P.din('dumrow', [128, 1]); id_d = P.din('ident', [128, 128], BF16)
    w1_d = P.din('w1', [NE, 2048, 4096]); w2_d = P.din('w2', [NE, 2048, 2048])
    b1g_d = P.din('b1g', [128, NE, 16]); b1l_d = P.din('b1l', [128, NE, 16]); b2_d = P.din('b2', [1, NE, 2048])
    y_o = P.dout('ypart', [8192 + 128, 2048])
    lists = [nc.dram_tensor("lists%d" % e, [CAPT, 1], I32, kind="Internal").ap() for e in range(NE)]

    pm = P.sb('pm', [128, 64, NE]); cnt = P.sb('cnt', [128, 8, NE]); start = P.sb('start', [128, 8, NE]); valid = P.sb('valid', [128, 64, NE])
    posg = P.sb('posg', [128, 64, NE]); inv = P.sb('inv', [128, 64, NE]); posi = P.sb('posi', [128, 64, NE], I32)
    tid = P.sb('tid', [128, 64], I32); dum = P.sb('dum', [128, 1]); ident = P.sb('ident', [128, 128], BF16)
    bigt = P.sb('bigt', [128, NBT], I32)
    idxt = P.sb('idxt', [128, NE, NBT], I32); idxf = P.sb('idxf', [128, NE, NBT]); v2 = P.sb('v2', [128, NE, NBT]); isc = P.sb('isc', [128, 4, NE, NBT], I32); iscf = P.sb('iscf', [128, NE, NBT])
    gsl = P.sb('gsl', [128, NBT, NE])
    b1g = P.sb('b1g', [128, NE, 16]); b1l = P.sb('b1l', [128, NE, 16]); b2 = P.sb('b2', [128, 2048]); ones1 = P.sb('ones1', [1, 128])
    XeT = P.sb('XeT', [128, 16, SCS], BF16); actT = P.sb('actT', [128, 16, SCS], BF16)
    Xs = [P.sb('Xs%d' % i, [128, 2048], BF16) for i in range(3)]
    w1b = [P.sb('w1b%d' % i, [128, 16, 256], BF16) for i in range(2)]
    w2b = [P.sb('w2b%d' % i, [128, 16, 512], BF16) for i in range(2)]
    Yst = [P.sb('Yst%d' % i, [128, 512]) for i in range(4)]
    gc = P.sb('gc', [128, 512]); sgm = P.sb('sgm', [128, 512]); lc = P.sb('lc', [128, 512])
    zer = Yst
    banks = [P.ps('b%d' % i) for i in range(6)]
    tbank = [P.ps('tb%d' % i, [128, 1024], BF16) for i in range(2)]
    bfree = [[] for _ in range(6)]; bptr = [0]
    def alloc():
        i = bptr[0] % 6; bptr[0] += 1
        return i, list(bfree[i])

    t_pm = P.dma('sp', pm[:], pm_d[:, :, :]); t_cnt = P.dma('sp', cnt[:], cnt_d[:, :, :]); t_tid = P.dma('sp', tid[:], tid_d[:, :])
    t_c = [P.dma('act', b1g[:], b1g_d[:, :, :]), P.dma('act', b1l[:], b1l_d[:, :, :]), P.dma('act', dum[:], dum_d[:, :]), P.dma('act', ident[:], id_d[:, :])]
    t_o1 = P.memset(ones1[:], 1.0)
    zt = [P.memset(Yst[i][:], 0.0, eng='pool') for i in range(4)]
    yv = y_o.rearrange("(b p) (c n) -> b p c n", p=128, n=512)
    t_zero = []
    for b in range(65):
        t_zero.append(P.dma('sp' if b % 2 == 0 else 'act', yv[b, :, :, :], Yst[0][:].rearrange("p (o n) -> p o n", o=1).broadcast(1, 4) if False else Yst[b % 4][:, None, :].to_broadcast([128, 4, 512]) if False else Yst[b % 4][:], deps=zt, slot='z%d' % (b % 4))) if False else None
    t_zero = []
    for b in range(65):
        for c4 in range(4):
            t_zero.append(P.dma('sp' if (b + c4) % 2 == 0 else 'act', y_o[b * 128:(b + 1) * 128, c4 * 512:(c4 + 1) * 512], Yst[c4][:], deps=zt, slot='z%d' % c4))
    t_zero_last = t_zero[-4:]
    w1v = w1_d.rearrange("e (kc p) n -> e p kc n", p=128); w2v = w2_d.rearrange("e (kc p) n -> e p kc n", p=128)
    w1_free = [None, None]; w2_free = [None, None]; w1_tok = {}; w2_tok = {}
    w1_seq = [(e, s, c) for e in range(NE) for s in range(nsc) for c in range(16)]
    w2_seq = [(e, s, c) for e in range(NE) for s in range(nsc) for c in range(4)]
    w1_i = [0]; w2_i = [0]
    def issue_w1():
        if w1_i[0] >= len(w1_seq): return
        k = w1_i[0]; e, s, c = w1_seq[k]
        w1_tok[(e, s, c)] = (P.dma('pool', w1b[k % 2][:], w1v[e, :, :, c * 256:(c + 1) * 256], deps=[w1_free[k % 2]], slot='w1_%d' % (k % 2)), k % 2)
        w1_i[0] += 1
    def issue_w2():
        if w2_i[0] >= len(w2_seq): return
        k = w2_i[0]; e, s, c = w2_seq[k]
        w2_tok[(e, s, c)] = (P.dma('pool', w2b[k % 2][:], w2v[e, :, :, c * 512:(c + 1) * 512], deps=[w2_free[k % 2]], slot='w2_%d' % (k % 2)), k % 2)
        w2_i[0] += 1
    a = P.memset(start[:, 0, :], 0.0, deps=[t_cnt])
    for s in range(1, 8):
        a = P.tt(start[:, s, :], start[:, s - 1, :], cnt[:, s - 1, :], ALU.add, deps=[a, t_cnt])
    t_start = a
    tv = P.ts(valid[:], pm[:], 0.0, None, ALU.is_ge, deps=[t_pm])
    last = tv
    for s in range(8):
        for e in range(NE):
            last = P.ts(posg[:, 8 * s:8 * s + 8, e], pm[:, 8 * s:8 * s + 8, e], start[:, s, e:e + 1], None, ALU.add, deps=[t_start, t_pm])
    a = P.tt(posg[:], posg[:], valid[:], ALU.mult, deps=[last, tv])
    b = P.ts(inv[:], valid[:], -1.0, -BIG, ALU.add, ALU.mult, deps=[tv])
    a = P.tt(posg[:], posg[:], inv[:], ALU.add, deps=[a, b])
    t_posi = P.cp(posi[:], posg[:], deps=[a])
    tb_ = P.memset(bigt[:], int(BIG))
    t_li = [P.dma('sp', lists[e].rearrange("(p b) o -> p (b o)", p=128), bigt[:], deps=[tb_]) for e in range(NE)]
    t_sc = []
    for e in range(NE):
        lastsc = None
        hist = []
        for G in range(64):
            lastsc = _ind(P, 's', lists[e], tid[:, G:G + 1], posi[:, G, e:e + 1], [t_posi, t_tid, t_li[e]] + ([hist[-8]] if len(hist) >= 8 else []), 'ls%d' % e, bounds_check=CAPT - 1, oob_is_err=False)
            hist.append(lastsc)
        t_sc.append(lastsc)
    t_idx = [P.dma('sp', idxt[:, e, :], lists[e].rearrange("(p b) o -> p (b o)", p=128), deps=[t_sc[e]]) for e in range(NE)]
    a = P.cp(idxf[:], idxt[:], deps=t_idx)
    a2 = P.ts(v2[:], idxf[:], 8192.0, None, ALU.is_lt, deps=[a])
    a3 = P.tt(idxf[:], idxf[:], v2[:], ALU.mult, deps=[a2])
    a4 = P.ts(v2[:], v2[:], -1.0, -1.0, ALU.add, ALU.mult, deps=[a3])
    a5 = P.ts(v2[:], v2[:], dum[:, 0:1], None, ALU.mult, deps=[a4, t_c[2]])
    a6 = P.tt(idxf[:], idxf[:], v2[:], ALU.add, deps=[a5])
    t_isc_l = []
    for c4 in range(4):
        a7 = P.ts(iscf[:], idxf[:], 4.0, float(c4), ALU.mult, ALU.add, deps=[a6] + t_isc_l)
        t_isc_l.append(P.cp(isc[:, c4, :, :], iscf[:], deps=[a7]))
    t_isc = t_isc_l[-1]
    y4 = y_o.rearrange("t (c n) -> (t c) n", n=512)
    issue_w1(); issue_w1(); issue_w2(); issue_w2()

    Xs_free = [[] for _ in range(3)]; Yst_free = [list(t_zero) if False else [] for _ in range(4)]
    XeT_free = []; actT_free = []; tmp_free = []; b2_free = []; gsl_free = []
    prev_scatter = list(t_zero_last) + t_zero
    tfree = [[], []]
    for e in range(NE):
        t_b2 = P.dma('sp', b2[:], b2_d[0, e, :].partition_broadcast(128), deps=b2_free, slot='b2')
        exp_scatter = []
        for sc in range(nsc):
            blk0 = sc * NBS
            t_g = None
            for bl in range(NBS):
                t_g = _ind(P, 'g', gsl[:, blk0 + bl, :], G_d[:, :], idxt[:, e, blk0 + bl:blk0 + bl + 1], [t_idx[e]] + gsl_free, 'gg', bounds_check=8191, oob_is_err=False)
            gsl_free = []
            xe_t = [[None] * NBS for _ in range(16)]
            lasttr = None
            for bl in range(NBS):
                r = bl % 3
                tg = _ind(P, 'g', Xs[r][:], h_d[:, :], idxt[:, e, blk0 + bl:blk0 + bl + 1], [t_idx[e]] + Xs_free[r], 'xg%d' % r, bounds_check=8191, oob_is_err=False)
                for q in range(4):
                    ti = (bl * 4 + q) % 2
                    for k4 in range(4):
                        kc = q * 4 + k4
                        lasttr = P.tr(tbank[ti][:, k4 * 128:(k4 + 1) * 128], Xs[r][:, kc * 128:(kc + 1) * 128], ident[:], deps=[tg, t_c[3]] + (tfree[ti] if k4 == 0 else []))
                    ev = P.cp(XeT[:, q * 4:(q + 1) * 4, bl * 128:(bl + 1) * 128], tbank[ti][:, 0:512].rearrange("p (k n) -> p k n", k=4), deps=[lasttr] + XeT_free,
                              eng=('act' if q % 2 == 0 else 'dve'))
                    tfree[ti] = [ev]
                    for k4 in range(4):
                        xe_t[q * 4 + k4][bl] = ev
                Xs_free[r] = [lasttr]
            XeT_free = []
            act_t = [[None] * len(SUBS) for _ in range(16)]
            lastw1 = None
            for ic in range(16):
                tw, wi = w1_tok[(e, sc, ic)]
                for si, (s0, sn) in enumerate(SUBS):
                    bG, dG = alloc(); bL, dL = alloc()
                    xdeps = [xe_t[kc_][bb] for kc_ in range(16) for bb in range(s0 // 128, (s0 + sn) // 128)]
                    for kc in range(16):
                        P.mm(banks[bG][:, 0:sn], w1b[wi][:, kc, 0:256:2], XeT[:, kc, s0:s0 + sn], start=(kc == 0), stop=(kc == 15), deps=(dG + [tw] + xdeps if kc == 0 else []))
                    mg = ('c', 'pe', len(P.ops['pe']) - 1)
                    for kc in range(16):
                        lastw1 = P.mm(banks[bL][:, 0:sn], w1b[wi][:, kc, 1:256:2], XeT[:, kc, s0:s0 + sn], start=(kc == 0), stop=(kc == 15), deps=(dL if kc == 0 else []))
                    e1 = P.ts(gc[:, 0:sn], banks[bG][:, 0:sn], b1g[:, e, ic:ic + 1], 7.0, ALU.add, ALU.min, deps=[mg] + tmp_free + t_c)
                    e2 = P.ts(lc[:, 0:sn], banks[bL][:, 0:sn], b1l[:, e, ic:ic + 1], 7.0, ALU.add, ALU.min, deps=[lastw1] + tmp_free)
                    bfree[bG] = [e1]; bfree[bL] = [e2]
                    e3 = P.act(sgm[:, 0:sn], gc[:, 0:sn], AF.Sigmoid, scale=1.702, deps=[e1] + tmp_free)
                    e4 = P.ts(lc[:, 0:sn], lc[:, 0:sn], -7.0, 1.0, ALU.max, ALU.add, deps=[e2])
                    e5 = P.tt(gc[:, 0:sn], gc[:, 0:sn], sgm[:, 0:sn], ALU.mult, deps=[e3, e1])
                    e6 = P.tt(actT[:, ic, s0:s0 + sn], gc[:, 0:sn], lc[:, 0:sn], ALU.mult, deps=[e5, e4] + actT_free)
                    tmp_free = [e6]
                    act_t[ic][si] = e6
                w1_free[wi] = lastw1
                issue_w1()
            actT_free = []
            XeT_free = [lastw1]
            lastw2 = None
            yk = 0
            for dmc in range(4):
                tw, wi = w2_tok[(e, sc, dmc)]
                for bl in range(NBS):
                    si = [i for i, (s0, sn) in enumerate(SUBS) if s0 <= bl * 128 < s0 + sn][0]
                    bk, dps = alloc()
                    for ic in range(16):
                        lastw2 = P.mm(banks[bk][:, 0:512], actT[:, ic, bl * 128:(bl + 1) * 128], w2b[wi][:, ic, :], start=(ic == 0), stop=(ic == 15),
                                      deps=(dps + [tw] if ic == 0 else []) + [act_t[ic][si]])
                    r = yk % 4; yk += 1
                    ev0 = P.tt(Yst[r][:], banks[bk][:, 0:512], b2[:, dmc * 512:(dmc + 1) * 512], ALU.add, deps=[lastw2, t_b2] + Yst_free[r] + zt)
                    bfree[bk] = [ev0]
                    ev = P.act(Yst[r][:], Yst[r][:], AF.Copy, scale=gsl[:, blk0 + bl, e:e + 1], deps=[ev0, t_g])
                    sct = _ind(P, 's', y4, Yst[r][:], isc[:, dmc, e, blk0 + bl:blk0 + bl + 1], [ev, t_isc] + prev_scatter, 'ys%d' % r, compute_op=ALU.add)
                    Yst_free[r] = [sct]
                    exp_scatter.append(sct)
                w2_free[wi] = lastw2
                issue_w2()
            actT_free = [lastw2]
            gsl_free = [lastw2]
            b2_free = [lastw2]
            prev_scatter = prev_scatter if sc < nsc - 1 else []
        prev_scatter = exp_scatter[-4:] + [t for t in exp_scatter]
    for t in prev_scatter:
        P.out_toks.append(t)
    return P

def prep_C3(core, h2all, pm_all, cnt_all, G_all, w1_l, b1_l, w2_l, b2_l, NE=4):
    e0 = core * NE
    pm = np.concatenate([pm_all[s][:, :, e0:e0 + NE] for s in range(8)], axis=1)
    cnt = np.stack([np.tile(cnt_all[s][None, e0:e0 + NE], (128, 1)) for s in range(8)], axis=1)
    Grows = np.concatenate([G_all[s][:, :, e0:e0 + NE].transpose(1, 0, 2).reshape(1024, NE) for s in range(8)], axis=0)
    tid = (np.arange(64, dtype=np.int32)[None, :] * 128 + np.arange(128, dtype=np.int32)[:, None]).astype(np.int32)
    b1 = b1_l[e0:e0 + NE]
    b1g = b1[:, 0::2].reshape(NE, 16, 128).transpose(2, 0, 1); b1l = b1[:, 1::2].reshape(NE, 16, 128).transpose(2, 0, 1)
    return dict(h2all=h2all, pm=np.ascontiguousarray(pm), cnt=np.ascontiguousarray(cnt.astype(np.float32)), Grows=np.ascontiguousarray(Grows),
                tid=tid, dumrow=(8192 + np.arange(128, dtype=np.float32))[:, None], ident=np.eye(128, dtype=np.float32).astype(ml_dtypes.bfloat16),
                w1=np.ascontiguousarray(w1_l[e0:e0 + NE]), w2=np.ascontiguousarray(w2_l[e0:e0 + NE]),
                b1g=np.ascontiguousarray(b1g), b1l=np.ascontiguousarray(b1l), b2=np.ascontiguousarray(b2_l[None, e0:e0 + NE]))


def build_D(final=False):
    P = Prog()
    x_d = P.din('x1T', [2048, 1024]); y_d = P.din('yparts', [8, 2048, 1024]); modv_d = P.din('modv', [128, 6, 16]); fg_d = P.din('fg', [128, 16])
    o_d = P.dout('x2T', [2048, 1024])
    x = P.sb('x', [128, 16, 1024]); mods = P.sb('mods', [128, 6, 16]); fg = P.sb('fg', [128, 16])
    yb = [[P.sb('yb%d_%d' % (i, c), [128, 1024]) for c in range(8)] for i in range(2)]
    sq = [P.sb('sq%d' % i, [128, 1024], BF16) for i in range(2)]; ones = P.sb('ones', [128, 128], BF16); rstd = P.sb('rstd', [128, 1024])
    ps = [P.ps('b%d' % i) for i in range(2)]
    t_m = P.dma('act', mods[:], modv_d[:, :, :]); t_fg = P.dma('act', fg[:], fg_d[:, :])
    t_x = [P.dma('act', x[:, kc * 4:(kc + 1) * 4, :], x_d.rearrange("(kc p) t -> p kc t", p=128)[:, kc * 4:(kc + 1) * 4, :]) for kc in range(4)]
    t_ones = P.memset(ones[:], 1.0)
    yfree = [[None] * 8, [None] * 8]
    sq_free = [None, None]
    t_x2 = []
    lastN = [None, None]
    for kc in range(16):
        i = kc % 2
        lt = [P.dma('sp', yb[i][c][:], y_d[c, kc * 128:(kc + 1) * 128, :], deps=[yfree[i][c]], slot='y%d_%d' % (i, c)) for c in range(8)]
        a01 = P.tt(yb[i][0][:], yb[i][0][:], yb[i][1][:], ALU.add, deps=[lt[0], lt[1]])
        a23 = P.tt(yb[i][2][:], yb[i][2][:], yb[i][3][:], ALU.add, deps=[lt[2], lt[3]], eng='pool')
        a45 = P.tt(yb[i][4][:], yb[i][4][:], yb[i][5][:], ALU.add, deps=[lt[4], lt[5]])
        a67 = P.tt(yb[i][6][:], yb[i][6][:], yb[i][7][:], ALU.add, deps=[lt[6], lt[7]], eng='pool')
        b0 = P.tt(yb[i][0][:], yb[i][0][:], yb[i][2][:], ALU.add, deps=[a01, a23])
        b1 = P.tt(yb[i][4][:], yb[i][4][:], yb[i][6][:], ALU.add, deps=[a45, a67], eng='pool')
        c0 = P.tt(yb[i][0][:], yb[i][0][:], yb[i][4][:], ALU.add, deps=[b0, b1])
        xx = P.stt(x[:, kc, :], yb[i][0][:], mods[:, 5, kc:kc + 1], x[:, kc, :], ALU.mult, ALU.add, deps=[c0, t_m, t_x[kc // 4]])
        for c in range(8):
            yfree[i][c] = xx
        t_x2.append(xx)
        if final:
            s_ = P.act(sq[i][:], x[:, kc, :], AF.Square, deps=[xx, sq_free[i]])
            for hf in range(2):
                lastN[hf] = P.mm(ps[hf][:], ones[:], sq[i][:, hf * 512:(hf + 1) * 512], start=(kc == 0), stop=(kc == 15), deps=[s_, t_ones])
            sq_free[i] = lastN[1]
    if final:
        t_r = []
        for hf in range(2):
            cs = slice(hf * 512, (hf + 1) * 512)
            a = P.act(rstd[:, cs], ps[hf][:], AF.Sqrt, bias=1e-6, scale=1.0 / 2048, deps=[lastN[hf]])
            t_r.append(P.recip(rstd[:, cs], rstd[:, cs], deps=[a]))
        t_f = []
        for kc in range(16):
            t_f.append(P.stt(x[:, kc, :], x[:, kc, :], fg[:, kc:kc + 1], rstd[:], ALU.mult, ALU.mult, deps=t_r + [t_fg, t_x2[kc]]))
        t_x2 = t_f
    for kc in range(4):
        P.dma('sp', o_d.rearrange("(kc p) t -> p kc t", p=128)[:, kc * 4:(kc + 1) * 4, :], x[:, kc * 4:(kc + 1) * 4, :], deps=t_x2[kc * 4:(kc + 1) * 4], is_out=True)
    return P


def _np(a):
    return np.asarray(a)

def _to_bf16(a):
    a = np.asarray(a)
    if a.dtype == ml_dtypes.bfloat16:
        return a
    if a.dtype.itemsize == 2:
        return a.view(ml_dtypes.bfloat16)
    raise ValueError('unexpected dtype %s' % a.dtype)

def kernel(x, c, positions, ada_w, ada_b, norm1_g, norm2_g, w_in, conv_w, conv_b, lru_wa, lru_ba, lru_wx, lru_bx,
           lru_lambda, attn_out_g, lru_out_g, w_out, router_w, router_b, w1, b1, w2, b2, final_g):
    x = _np(x); c = _np(c); positions = _np(positions); ada_w = _np(ada_w); ada_b = _np(ada_b)
    norm1_g = _np(norm1_g); norm2_g = _np(norm2_g); w_in = _np(w_in); conv_w = _np(conv_w); conv_b = _np(conv_b)
    lru_wa = _np(lru_wa); lru_ba = _np(lru_ba); lru_wx = _np(lru_wx); lru_bx = _np(lru_bx); lru_lambda = _np(lru_lambda)
    attn_out_g = _np(attn_out_g); lru_out_g = _np(lru_out_g); w_out = _np(w_out); router_w = _np(router_w); router_b = _np(router_b)
    w1 = _np(w1); b1 = _np(b1); w2 = _np(w2); b2 = _np(b2); final_g = _np(final_g)
    NC = 8
    ims = []
    for core in range(NC):
        l = core // 4; cs = (core % 4) * 3072
        ims.append({'c': np.ascontiguousarray(c.reshape(16, 128).T), 'w': np.ascontiguousarray(ada_w[l][:, cs:cs + 3072]),
                    'b': np.ascontiguousarray(ada_b[l][None, cs:cs + 3072])})
    res = build_M().run(ims)
    mod = np.concatenate([np.asarray(r['mod'])[0] for r in res.results]).reshape(2, 12288)
    xT = [np.ascontiguousarray(x[0, k * 1024:(k + 1) * 1024, :].T) for k in range(NC)]
    fgl = np.ascontiguousarray(final_g.reshape(16, 128).T)
    for l in range(2):
        modv = np.ascontiguousarray(mod[l].reshape(6, 16, 128).transpose(2, 0, 1))
        w_in_l = np.ascontiguousarray(w_in[l])
        cA = consts_A()
        ims = []
        for k in range(NC):
            d = dict(xT=xT[k], modv=modv, gn=np.ascontiguousarray(norm1_g[l].reshape(16, 128).T), w=w_in_l,
                     pos=np.ascontiguousarray(positions[:, k * 1024:(k + 1) * 1024]))
            d.update(cA)
            ims.append(d)
        rA = build_A().run(ims).results
        qT = np.concatenate([_to_bf16(r['qT']) for r in rA], axis=1)
        kT = np.concatenate([_to_bf16(r['kT']) for r in rA], axis=1)
        vT = np.concatenate([_to_bf16(r['vT']) for r in rA], axis=1)
        xrT = np.concatenate([np.asarray(r['xrT']) for r in rA], axis=1)
        grT = [np.asarray(r['grT']) for r in rA]
        ims = []
        for k in range(NC):
            m = prep_B_attn(k, qT, kT, vT)
            m.update(prep_B_lru(k, xrT, conv_w[l], conv_b[l], lru_wa[l], lru_ba[l], lru_wx[l], lru_bx[l], lru_lambda[l], positions))
            ims.append(m)
        rB = build_B(True).run(ims).results
        hloc = [np.asarray(r['hloc']) for r in rB]; pprod = [np.asarray(r['pprod']) for r in rB]
        wo_l = np.ascontiguousarray(w_out[l])
        ims = [prep_C1(k, xT[k], np.asarray(rB[k]['attnT']), hloc, pprod, grT[k], mod[l], norm2_g[l], attn_out_g[l], lru_out_g[l],
                       wo_l, router_w[l], router_b[l]) for k in range(NC)]
        rC = build_C1().run(ims).results
        x1T = [np.asarray(r['x1T']) for r in rC]
        h2all = np.ascontiguousarray(np.concatenate([_to_bf16(r['h2T']).T for r in rC], axis=0))
        pm = [np.asarray(r['pm']) for r in rC]; G = [np.asarray(r['G']) for r in rC]
        cnt_all = [np.asarray(r['cnt'])[0] for r in rC]
        ims = [prep_C3(k, h2all, pm, cnt_all, G, w1[l], b1[l], w2[l], b2[l]) for k in range(NC)]
        rE = build_C3().run(ims).results
        yp = [np.asarray(r['ypart']) for r in rE]
        final = (l == 1)
        ims = [dict(x1T=x1T[k], yparts=np.ascontiguousarray(np.stack([yp[cc][k * 1024:(k + 1) * 1024].T for cc in range(NC)])), modv=modv, fg=fgl)
               for k in range(NC)]
        rD = build_D(final=final).run(ims).results
        xT = [np.asarray(r['x2T']) for r in rD]
    out = np.concatenate([t.T for t in xT], axis=0)[None].astype(np.float32)
    return out
```
